# Optimizing a Trainium2 kernel written in Bass

```python
import math
import jax
import jax.numpy as jnp
from jax import lax
import numpy as np

D_MODEL = 1024
BATCH = 2
SEQ = 8192
DEPTH = 2

GRID_W = 64
CTX_LEN = 256
EPS = 1e-6
CHUNK = 128
Q_BLOCK = 128

MLA_HEADS = 4
MLA_Q_LORA = 256
MLA_KV_LORA = 128
MLA_NOPE = 64
MLA_ROPE = 32
MLA_V = 64
MLA_SCALE = (MLA_NOPE + MLA_ROPE) ** -0.5
ROPE_AXIS_DIM = MLA_ROPE // 2
ROPE_BASE = 10000.0

RET_HEADS = 4
RET_QK = 64
RET_V = 64

SSD_HEADS = 8
SSD_HEADDIM = 64
SSD_GROUPS = 2
SSD_STATE = 64
SSD_CONV = 3
SSD_INNER = SSD_HEADS * SSD_HEADDIM
SSD_CONV_CH = SSD_INNER + 2 * SSD_GROUPS * SSD_STATE

IN_SIZES = (MLA_Q_LORA, MLA_KV_LORA, MLA_ROPE,
            RET_HEADS * RET_QK, RET_HEADS * RET_QK, RET_HEADS * RET_V, RET_HEADS * RET_V,
            SSD_INNER, SSD_CONV_CH, SSD_HEADS, SSD_HEADS)
IN_WIDTH = sum(IN_SIZES)
MIX_WIDTH = MLA_HEADS * MLA_V + RET_HEADS * RET_V + SSD_INNER

D_FF = 2816
N_EXPERTS = 8
TOP_K = 2
D_FF_EXPERT = 1408
N_DENSE = (DEPTH + 1) // 2
N_MOE = DEPTH // 2

kernel_name = "hybrid_mla_retention_ssd_dit_block"


def rmsnorm(x, w):
    xf = x.astype(jnp.float32)
    y = xf * lax.rsqrt(jnp.mean(xf * xf, axis=-1, keepdims=True) + EPS)
    return (y * w.astype(jnp.float32)).astype(x.dtype)


def modulate(h, shift, scale):
    return h * (1.0 + scale) + shift


def axial_rope_tables(rows, dtype):
    f32 = jnp.float32
    row = jnp.repeat(jnp.arange(rows, dtype=f32), GRID_W)
    col = jnp.tile(jnp.arange(GRID_W, dtype=f32), rows)
    inv_freq = ROPE_BASE ** (-jnp.arange(0, ROPE_AXIS_DIM, 2, dtype=f32) / ROPE_AXIS_DIM)
    ang_r = row[:, None] * inv_freq
    ang_c = col[:, None] * inv_freq
    ang = jnp.concatenate([ang_r, ang_r, ang_c, ang_c], axis=-1)
    return jnp.cos(ang).astype(dtype), jnp.sin(ang).astype(dtype)


def apply_axial_rope(x, cos, sin):
    x1, x2, x3, x4 = jnp.split(x, 4, axis=-1)
    rot = jnp.concatenate([-x2, x1, -x4, x3], axis=-1)
    return x * cos + rot * sin


def split_in(proj):
    offs = np.cumsum(IN_SIZES)[:-1].tolist()
    return jnp.split(proj, offs, axis=-1)


def block_attention(q, k, v):
    b, l, h, d = q.shape
    nb = l // Q_BLOCK
    qb = jnp.moveaxis(q.reshape(b, nb, Q_BLOCK, h, d), 1, 0)

    def one(q_blk):
        s = jnp.einsum('bqhd,bkhd->bhqk', q_blk, k).astype(jnp.float32) * MLA_SCALE
        p = jax.nn.softmax(s, axis=-1).astype(v.dtype)
        return jnp.einsum('bhqk,bkhd->bqhd', p, v)

    out = lax.map(one, qb)
    return jnp.moveaxis(out, 0, 1).reshape(b, l, h, v.shape[-1])


def mla_qkv(cq, ckv, kpe, q_norm_w, w_qb, kv_norm_w, w_kb, w_vb, cos, sin):
    b, l, _ = cq.shape
    q = (rmsnorm(cq, q_norm_w) @ w_qb).reshape(b, l, MLA_HEADS, MLA_NOPE + MLA_ROPE)
    q_nope, q_pe = q[..., :MLA_NOPE], q[..., MLA_NOPE:]
    ckv = rmsnorm(ckv, kv_norm_w)
    k_nope = (ckv @ w_kb).reshape(b, l, MLA_HEADS, MLA_NOPE)
    v = (ckv @ w_vb).reshape(b, l, MLA_HEADS, MLA_V)
    kpe = kpe[:, :, None, :]
    if cos is not None:
        q_pe = apply_axial_rope(q_pe, cos[:, None, :], sin[:, None, :])
        kpe = apply_axial_rope(kpe, cos[:, None, :], sin[:, None, :])
    q = jnp.concatenate([q_nope, q_pe], axis=-1)
    k = jnp.concatenate([k_nope, jnp.broadcast_to(kpe, (b, l, MLA_HEADS, MLA_ROPE))], axis=-1)
    return q, k, v


def chunked_scan(q, k, v, log_a, state0, include_diag):
    f32 = jnp.float32
    b, l, h, n = q.shape
    p = v.shape[-1]
    nc = l // CHUNK
    qc = q.astype(f32).reshape(b, nc, CHUNK, h, n)
    kc = k.astype(f32).reshape(b, nc, CHUNK, h, n)
    vc = v.astype(f32).reshape(b, nc, CHUNK, h, p)
    acum = jnp.cumsum(log_a.astype(f32).reshape(b, nc, CHUNK, h), axis=2)
    acum_h = jnp.swapaxes(acum, 2, 3)
    idx = jnp.arange(CHUNK)
    mask = (idx[:, None] >= idx[None, :]) if include_diag else (idx[:, None] > idx[None, :])
    seg = acum_h[..., :, None] - acum_h[..., None, :]
    decay = jnp.exp(jnp.where(mask, seg, -jnp.inf))
    scores = jnp.einsum('bcihn,bcjhn->bchij', qc, kc) * decay
    y = jnp.einsum('bchij,bcjhp->bcihp', scores, vc)
    w_end = jnp.exp(acum[:, :, -1:, :] - acum)
    chunk_states = jnp.einsum('bcjhn,bcjhp->bchnp', kc * w_end[..., None], vc)
    chunk_decay = jnp.exp(acum[:, :, -1, :])

    def step(state, inp):
        s_c, d_c = inp
        return state * d_c[:, :, None, None] + s_c, state

    final, states_in = lax.scan(step, state0,
                                (jnp.moveaxis(chunk_states, 1, 0), jnp.moveaxis(chunk_decay, 1, 0)))
    states_in = jnp.moveaxis(states_in, 0, 1)
    y = y + jnp.einsum('bcihn,bchnp->bcihp', qc * jnp.exp(acum)[..., None], states_in)
    return y.reshape(b, l, h, p), final


def bidir_scan(ctx_in, lat_in, bwd_diag):
    q_c, k_c, v_cf, v_cb, la_cf, la_cb = ctx_in
    q_x, k_x, v_xf, v_xb, la_xf, la_xb = lat_in
    b, _, h, n = q_c.shape
    s0 = jnp.zeros((b, h, n, v_cf.shape[-1]), jnp.float32)
    flip = lambda t: jnp.flip(t, axis=1)
    y_cf, s_cf = chunked_scan(q_c, k_c, v_cf, la_cf, s0, True)
    y_cb, s_cb = chunked_scan(flip(q_c), flip(k_c), flip(v_cb), flip(la_cb), s0, bwd_diag)
    y_xf, _ = chunked_scan(q_x, k_x, v_xf, la_xf, s_cf, True)
    y_xb, _ = chunked_scan(flip(q_x), flip(k_x), flip(v_xb), flip(la_xb), s_cb, bwd_diag)
    return y_cf + flip(y_cb), y_xf + flip(y_xb)


def retention_inputs(rq, rk, rv, log_gamma):
    b, l, _ = rq.shape
    q = rq.reshape(b, l, RET_HEADS, RET_QK) * (RET_QK ** -0.5)
    k = rk.reshape(b, l, RET_HEADS, RET_QK)
    v = rv.reshape(b, l, RET_HEADS, RET_V)
    la_f = jnp.broadcast_to(log_gamma[0], (b, l, RET_HEADS))
    la_b = jnp.broadcast_to(log_gamma[1], (b, l, RET_HEADS))
    return (q, k, v, v, la_f, la_b)


def retention_output(y, g):
    b, l = y.shape[:2]
    mu = jnp.mean(y, axis=-1, keepdims=True)
    var = jnp.mean(jnp.square(y - mu), axis=-1, keepdims=True)
    yn = ((y - mu) * lax.rsqrt(var + EPS)).reshape(b, l, RET_HEADS * RET_V)
    return yn.astype(g.dtype) * jax.nn.silu(g)


def depthwise_conv(x, w, bias):
    y = lax.conv_general_dilated(x, w[:, None, :].astype(x.dtype), window_strides=(1,),
                                 padding=[(SSD_CONV // 2, SSD_CONV // 2)],
                                 dimension_numbers=('NWC', 'WIO', 'NWC'),
                                 feature_group_count=x.shape[-1])
    return y + bias


def ssd_inputs(xbc, dt_f_raw, dt_b_raw, conv_w, conv_b, dt_bias, a_log):
    f32 = jnp.float32
    b, l, _ = xbc.shape
    xbc = jax.nn.silu(depthwise_conv(xbc, conv_w, conv_b))
    xs, bm, cm = jnp.split(xbc, [SSD_INNER, SSD_INNER + SSD_GROUPS * SSD_STATE], axis=-1)
    xs = xs.reshape(b, l, SSD_HEADS, SSD_HEADDIM)
    rep = SSD_HEADS // SSD_GROUPS
    bm = jnp.repeat(bm.reshape(b, l, SSD_GROUPS, SSD_STATE), rep, axis=2)
    cm = jnp.repeat(cm.reshape(b, l, SSD_GROUPS, SSD_STATE), rep, axis=2)
    a = -jnp.exp(a_log.astype(f32))
    dt_bias = dt_bias.astype(f32)
    dt_f = jax.nn.softplus(dt_f_raw.astype(f32) + dt_bias[0])
    dt_b = jax.nn.softplus(dt_b_raw.astype(f32) + dt_bias[1])
    scan_in = (cm, bm, xs * dt_f[..., None], xs * dt_b[..., None], dt_f * a[0], dt_b * a[1])
    return scan_in, xs


def ssd_output(y, xs, z, d_skip, norm_w):
    b, l = y.shape[:2]
    y = y + xs.astype(jnp.float32) * d_skip.astype(jnp.float32)[:, None]
    y = y.reshape(b, l, SSD_INNER) * jax.nn.silu(z.astype(jnp.float32))
    return rmsnorm(y, norm_w).astype(z.dtype)


def hybrid_mixer(hx, hc, cos, sin, w_in, w_out, mla_q_norm_w, mla_w_qb, mla_kv_norm_w, mla_w_kb,
                 mla_w_vb, ret_decay_logit, ssd_conv_w, ssd_conv_b, ssd_dt_bias, ssd_a_log, ssd_d,
                 ssd_norm_w, with_ctx_out):
    px = split_in(hx @ w_in)
    pc = split_in(hc @ w_in)

    q_x, k_x, v_x = mla_qkv(px[0], px[1], px[2], mla_q_norm_w, mla_w_qb, mla_kv_norm_w, mla_w_kb, mla_w_vb, cos, sin)
    q_c, k_c, v_c = mla_qkv(pc[0], pc[1], pc[2], mla_q_norm_w, mla_w_qb, mla_kv_norm_w, mla_w_kb, mla_w_vb, None, None)
    k_all = jnp.concatenate([k_c, k_x], axis=1)
    v_all = jnp.concatenate([v_c, v_x], axis=1)
    b, l = hx.shape[:2]
    att_x = block_attention(q_x, k_all, v_all).reshape(b, l, MLA_HEADS * MLA_V)

    log_gamma = jax.nn.log_sigmoid(ret_decay_logit.astype(jnp.float32))
    ret_yc, ret_yx = bidir_scan(retention_inputs(pc[3], pc[4], pc[5], log_gamma),
                                retention_inputs(px[3], px[4], px[5], log_gamma), False)
    ret_x = retention_output(ret_yx, px[6])

    ssd_in_c, xs_c = ssd_inputs(pc[8], pc[9], pc[10], ssd_conv_w, ssd_conv_b, ssd_dt_bias, ssd_a_log)
    ssd_in_x, xs_x = ssd_inputs(px[8], px[9], px[10], ssd_conv_w, ssd_conv_b, ssd_dt_bias, ssd_a_log)
    ssd_yc, ssd_yx = bidir_scan(ssd_in_c, ssd_in_x, True)
    ssd_x = ssd_output(ssd_yx, xs_x, px[7], ssd_d, ssd_norm_w)

    out_x = jnp.concatenate([att_x, ret_x, ssd_x], axis=-1) @ w_out
    if not with_ctx_out:
        return out_x, None
    lc = hc.shape[1]
    att_c = block_attention(q_c, k_c, v_c).reshape(b, lc, MLA_HEADS * MLA_V)
    ret_c = retention_output(ret_yc, pc[6])
    ssd_c = ssd_output(ssd_yc, xs_c, pc[7], ssd_d, ssd_norm_w)
    out_c = jnp.concatenate([att_c, ret_c, ssd_c], axis=-1) @ w_out
    return out_x, out_c


def swiglu(h, w_gate, w_up, w_down):
    return (jax.nn.silu(h @ w_gate) * (h @ w_up)) @ w_down


def moe_swiglu(h, router, w_gate, w_up, w_down):
    b, l, d = h.shape
    t = h.reshape(b * l, d)
    logits = (t @ router).astype(jnp.float32)
    top_v, top_i = lax.top_k(logits, TOP_K)
    gates = jax.nn.softmax(top_v, axis=-1)
    dense_gate = jnp.einsum('tk,tke->te', gates,
                            jax.nn.one_hot(top_i, N_EXPERTS, dtype=jnp.float32)).astype(t.dtype)
    out = jnp.zeros_like(t)
    for e in range(N_EXPERTS):
        out = out + dense_gate[:, e:e + 1] * swiglu(t, w_gate[e], w_up[e], w_down[e])
    return out.reshape(b, l, d)


def channel_mix(h, layer, ffn_w_gate, ffn_w_up, ffn_w_down, moe_router, moe_w_gate, moe_w_up, moe_w_down):
    j = layer // 2
    if layer % 2 == 0:
        return swiglu(h, ffn_w_gate[j], ffn_w_up[j], ffn_w_down[j])
    return moe_swiglu(h, moe_router[j], moe_w_gate[j], moe_w_up[j], moe_w_down[j])


def setup_inputs(seed: int = 0) -> dict:
    key = jax.random.key(seed)
    ks = iter(jax.random.split(key, 40))
    f32 = jnp.float32
    D = D_MODEL

    def nrm(shape, scale):
        return jax.random.normal(next(ks), shape, f32) * scale

    def gain(shape):
        return 1.0 + 0.05 * jax.random.normal(next(ks), shape, f32)

    gamma0 = 1.0 - 2.0 ** (-5.0 - np.arange(RET_HEADS))
    ret_base = jnp.asarray(np.log(gamma0 / (1.0 - gamma0)).astype(np.float32))
    u = jax.random.uniform(next(ks), (DEPTH, 2, SSD_HEADS), f32)
    dt0 = jnp.exp(u * (math.log(0.1) - math.log(0.001)) + math.log(0.001))

    return {
        "x": nrm((BATCH, SEQ, D), 1.0),
        "c": nrm((BATCH, D), 1.0),
        "ctx": nrm((BATCH, CTX_LEN, D), 1.0),
        "c_ctx": nrm((D,), 1.0),
        "mod_w": nrm((DEPTH, D, 6 * D), 0.5 * D ** -0.5),
        "mod_b": nrm((DEPTH, 6 * D), 0.01),
        "norm1_w": gain((DEPTH, D)),
        "norm2_w": gain((DEPTH, D)),
        "w_in": nrm((DEPTH, D, IN_WIDTH), D ** -0.5),
        "w_out": nrm((DEPTH, MIX_WIDTH, D), MIX_WIDTH ** -0.5),
        "mla_q_norm_w": gain((DEPTH, MLA_Q_LORA)),
        "mla_w_qb": nrm((DEPTH, MLA_Q_LORA, MLA_HEADS * (MLA_NOPE + MLA_ROPE)), MLA_Q_LORA ** -0.5),
        "mla_kv_norm_w": gain((DEPTH, MLA_KV_LORA)),
        "mla_w_kb": nrm((DEPTH, MLA_KV_LORA, MLA_HEADS * MLA_NOPE), MLA_KV_LORA ** -0.5),
        "mla_w_vb": nrm((DEPTH, MLA_KV_LORA, MLA_HEADS * MLA_V), MLA_KV_LORA ** -0.5),
        "ret_decay_logit": ret_base + nrm((DEPTH, 2, RET_HEADS), 0.1),
        "ssd_conv_w": nrm((DEPTH, SSD_CONV, SSD_CONV_CH), SSD_CONV ** -0.5),
        "ssd_conv_b": nrm((DEPTH, SSD_CONV_CH), 0.01),
        "ssd_dt_bias": dt0 + jnp.log(-jnp.expm1(-dt0)),
        "ssd_a_log": jnp.log(jax.random.uniform(next(ks), (DEPTH, 2, SSD_HEADS), f32, 1.0, 16.0)),
        "ssd_d": gain((DEPTH, SSD_HEADS)),
        "ssd_norm_w": gain((DEPTH, SSD_INNER)),
        "ffn_w_gate": nrm((N_DENSE, D, D_FF), D ** -0.5),
        "ffn_w_up": nrm((N_DENSE, D, D_FF), D ** -0.5),
        "ffn_w_down": nrm((N_DENSE, D_FF, D), D_FF ** -0.5),
        "moe_router": nrm((N_MOE, D, N_EXPERTS), D ** -0.5),
        "moe_w_gate": nrm((N_MOE, N_EXPERTS, D, D_FF_EXPERT), D ** -0.5),
        "moe_w_up": nrm((N_MOE, N_EXPERTS, D, D_FF_EXPERT), D ** -0.5),
        "moe_w_down": nrm((N_MOE, N_EXPERTS, D_FF_EXPERT, D), D_FF_EXPERT ** -0.5),
        "final_norm_w": gain((D,)),
    }


def reference(x, c, ctx, c_ctx, mod_w, mod_b, norm1_w, norm2_w, w_in, w_out, mla_q_norm_w, mla_w_qb,
              mla_kv_norm_w, mla_w_kb, mla_w_vb, ret_decay_logit, ssd_conv_w, ssd_conv_b, ssd_dt_bias,
              ssd_a_log, ssd_d, ssd_norm_w, ffn_w_gate, ffn_w_up, ffn_w_down, moe_router, moe_w_gate,
              moe_w_up, moe_w_down, final_norm_w):
    n_lat = x.shape[1]
    rows = n_lat // GRID_W
    cos, sin = axial_rope_tables(rows, x.dtype)
    silu_c = jax.nn.silu(c)
    silu_cc = jax.nn.silu(c_ctx)
    h_ctx = ctx
    for i in range(DEPTH):
        last = i == DEPTH - 1
        mx = jnp.split((silu_c @ mod_w[i] + mod_b[i])[:, None, :], 6, axis=-1)
        mc = jnp.split(silu_cc @ mod_w[i] + mod_b[i], 6, axis=-1)
        hx = modulate(rmsnorm(x, norm1_w[i]), mx[0], mx[1])
        hc = modulate(rmsnorm(h_ctx, norm1_w[i]), mc[0], mc[1])
        out_x, out_c = hybrid_mixer(hx, hc, cos, sin, w_in[i], w_out[i], mla_q_norm_w[i], mla_w_qb[i],
                                    mla_kv_norm_w[i], mla_w_kb[i], mla_w_vb[i], ret_decay_logit[i],
                                    ssd_conv_w[i], ssd_conv_b[i], ssd_dt_bias[i], ssd_a_log[i], ssd_d[i],
                                    ssd_norm_w[i], not last)
        x = x + mx[2] * out_x
        hx = modulate(rmsnorm(x, norm2_w[i]), mx[3], mx[4])
        x = x + mx[5] * channel_mix(hx, i, ffn_w_gate, ffn_w_up, ffn_w_down,
                                    moe_router, moe_w_gate, moe_w_up, moe_w_down)
        if not last:
            h_ctx = h_ctx + mc[2] * out_c
            hc = modulate(rmsnorm(h_ctx, norm2_w[i]), mc[3], mc[4])
            h_ctx = h_ctx + mc[5] * channel_mix(hc, i, ffn_w_gate, ffn_w_up, ffn_w_down,
                                                moe_router, moe_w_gate, moe_w_up, moe_w_down)
    return rmsnorm(x, final_norm_w)
```

```python
import numpy as np
import ml_dtypes
from contextlib import ExitStack
import concourse.bass as bass
import concourse.mybir as mybir
from concourse.bass_utils import run_bass_kernel_spmd

F32 = mybir.dt.float32
BF16 = mybir.dt.bfloat16
AF = mybir.ActivationFunctionType
ALU = mybir.AluOpType
AX = mybir.AxisListType

D = 1024
TC = 256
TL = 2048
NT = TC + TL
NCH = NT // 128
BLOCKS = [(0, 256, 1), (256, 512, 0), (768, 512, 0), (1280, 512, 0), (1792, 512, 0)]
EPS = 1e-6
PI8 = [0, 4, 1, 5, 2, 6, 3, 7]
MLA_SCALE = 96 ** -0.5
CD_QT, CD_KT, CD_CT, CD_BT = 0, 256, 512, 640
CD_KTOK, CD_BTOK, CD_VTOK = 768, 1024, 1152
CD_XF, CD_XB, CD_XS, CD_SG, CD_SZ = 1408, 1920, 2432, 2944, 3200
CDW = 3712


STRICT = False


class Buf:
    __slots__ = ("w", "r", "chan")

    def __init__(self):
        self.w = None
        self.r = {}
        self.chan = None


class FW:
    def __init__(self, nc, es):
        self.nc = nc
        self.es = es
        self.E = {"pe": nc.tensor, "act": nc.scalar, "dve": nc.vector, "pool": nc.gpsimd, "sp": nc.sync}
        self.sems = {}
        self.cnt = {}
        self.waited = {e: {} for e in self.E}
        for e in self.E:
            self._newsem(e)
        self.nchan = 0
        self.ninstr = 0

    def _newsem(self, key):
        h = self.es.enter_context(self.nc.semaphore("s_%s" % (str(key).replace(" ", ""),)))
        self.sems[key] = h
        self.cnt[key] = 0
        return h

    def _wait(self, eng, key, val):
        if val <= 0:
            return
        w = self.waited[eng]
        if w.get(key, 0) >= val:
            return
        w[key] = val
        self.E[eng].wait_ge(self.sems[key], val)

    def deps(self, eng, reads, writes, is_dma=False):
        for b in reads:
            if b.w is not None:
                k, v = b.w
                if k != "pe" or eng != "pe" or is_dma:
                    self._wait(eng, k, v)
        for b in writes:
            if b.w is not None:
                k, v = b.w
                if k != eng or is_dma or (STRICT and eng != "pe"):
                    self._wait(eng, k, v)
            for k, v in b.r.items():
                if k != eng or is_dma or (STRICT and eng != "pe"):
                    self._wait(eng, k, v)

    def record(self, key, val, reads, writes):
        for b in reads:
            b.r[key] = val
        for b in writes:
            b.w = (key, val)
            b.r = {}

    def op(self, eng, fn, reads=(), writes=()):
        self.deps(eng, reads, writes)
        ins = fn(self.E[eng])
        self.cnt[eng] += 1
        ins.then_inc(self.sems[eng], 1)
        self.record(eng, self.cnt[eng], reads, writes)
        self.ninstr += 1
        return ins

    def group(self, eng, fns, reads=(), writes=()):
        self.deps(eng, reads, writes)
        ins = None
        for f in fns:
            ins = f(self.E[eng])
            self.ninstr += 1
        self.cnt[eng] += 1
        ins.then_inc(self.sems[eng], 1)
        self.record(eng, self.cnt[eng], reads, writes)

    def dma(self, q, out, in_, reads=(), writes=(), chanbuf=None, **kw):
        cb = chanbuf if chanbuf is not None else (writes[0] if writes else reads[0])
        if cb.chan is None:
            cb.chan = ("ch", self.nchan)
            self.nchan += 1
            self._newsem(cb.chan)
        ck = cb.chan
        self.deps(q, reads, writes, is_dma=True)
        self._wait(q, ck, self.cnt[ck])
        ins = self.E[q].dma_start(out=out, in_=in_, **kw)
        self.cnt[ck] += 16
        ins.then_inc(self.sems[ck], 16)
        self.record(ck, self.cnt[ck], reads, writes)
        self.ninstr += 1
        return ins

    def cc(self, kind, ins_ap, outs_ap, groups, reads, writes, chanbuf):
        if chanbuf.chan is None:
            chanbuf.chan = ("ch", self.nchan)
            self.nchan += 1
            self._newsem(chanbuf.chan)
        ck = chanbuf.chan
        self.deps("pool", reads, writes, is_dma=True)
        self._wait("pool", ck, self.cnt[ck])
        ins = self.nc.gpsimd.collective_compute(kind, ALU.bypass, replica_groups=groups, ins=[ins_ap], outs=[outs_ap])
        self.cnt[ck] += 1
        ins.then_inc(self.sems[ck], 1)
        self.record(ck, self.cnt[ck], reads, writes)

    def barrier(self):
        snap = dict(self.cnt)
        for eng in self.E:
            for key, val in snap.items():
                if key != eng:
                    self._wait(eng, key, val)

    def finish(self, bufs, eng="sp"):
        for b in bufs:
            if b.w is not None:
                self._wait(eng, *b.w)
            for k, v in b.r.items():
                self._wait(eng, k, v)


class Ring:
    def __init__(self, items):
        self.items = items
        self.i = 0

    def get(self):
        it = self.items[self.i % len(self.items)]
        self.i += 1
        return it


class _Stop(Exception):
    pass


class _StopOuter(Exception):
    pass


def build_program(n_layers=2, dbg=None, stop=None, groups=None):
    box = {}
    try:
        _build_program(box, n_layers, dbg, stop, groups)
    except _StopOuter:
        pass
    return box["nc"]


def _build_program(box, n_layers, dbg, stop, groups):
    nc = bass.Bass("TRN2", target_bir_lowering=False)
    box["nc"] = nc
    dbg = dbg or {}

    def din(name, shape, dt=F32):
        return nc.dram_tensor(name, list(shape), dt, kind="ExternalInput").ap()

    xT_in = din("xT", [D, NT])
    cc_in = din("cc", [128, 8, 2])
    mod_w = din("mod_w", [2, D, 6 * D])
    mod_b = din("mod_b", [2, 1, 6 * D])
    w_in = din("w_in", [2, D, 2736])
    w_out = din("w_out", [2, D, D])
    w_qb = din("w_qb", [2, 256, 384])
    w_qbrot = din("w_qbrot", [2, 256, 384])
    w_kb = din("w_kb", [2, 128, 256])
    w_vb = din("w_vb", [2, 128, 256])
    nw_in = din("nw", [2, 128, 16])
    fnw_in = din("fnw", [128, 8])
    qnw_in = din("qnw_b", [2, 128, 256])
    kvnw_in = din("kvnw_b", [2, 128, 128])
    convw_in = din("convw", [2, 128, 18])
    convb_in = din("convb", [2, 128, 6])
    dtb_in = din("dtb_b", [2, 128, 16])
    alog_in = din("alog_b", [2, 128, 16])
    rdl_in = din("rdl_b", [2, 128, 8])
    dskip_in = din("dskip_b", [2, 128, 512])
    snw_in = din("snw_b", [2, 128, 512])
    router_in = din("router", [D, 8])
    ffn_wg = din("ffn_wg", [D, 2816])
    ffn_wu = din("ffn_wu", [D, 2816])
    ffn_wd = din("ffn_wd", [2816, D])
    moe_wg = din("moe_wg", [8, D, 1408])
    moe_wu = din("moe_wu", [8, D, 1408])
    moe_wd = din("moe_wd", [8, 1408, D])
    cf_in = din("cf32", [128, 6, 128])
    cb_in = din("cbf16", [128, 3, 128], BF16)
    rope_in = din("ropeT", [128, 2, NT])
    sel_in = din("sel", [128, 16])
    outT = nc.dram_tensor("outT", [D, TL], F32, kind="ExternalOutput").ap()
    dbg_out = {k: nc.dram_tensor("dbg_" + k, list(s), F32, kind="ExternalOutput").ap() for k, s in dbg.items()}

    xres = nc.dram_tensor("xres", [D, NT], F32).ap()
    kv_send = nc.dram_tensor("kv_send", [162, NT], BF16)
    kv_all = nc.dram_tensor("kv_all", [4 * 162, NT], BF16)
    st_send = nc.dram_tensor("st_send", [128, 1560], F32)
    st_all = nc.dram_tensor("st_all", [4 * 128, 1560], F32)
    cd_dram = nc.dram_tensor("cd_dram", [NCH, 128, CDW], BF16).ap()
    yf_dram = nc.dram_tensor("yf_dram", [NCH, 128, 768], F32).ap()
    modrow_d = nc.dram_tensor("modrow_d", [2, 6 * D], F32).ap()
    xbc_dram = nc.dram_tensor("xbc_dram", [128, 6, NT + 4], BF16).ap()
    GROUPS = groups or [[0, 1, 2, 3], [4, 5, 6, 7]]

    with ExitStack() as es:
        fw = FW(nc, es)
        op = fw.op

        sbn = [0]

        def sb(es_, name, shape, dt):
            sbn[0] += 1
            return es_.enter_context(nc.sbuf_tensor("%s_%d" % (name, sbn[0]), list(shape), dt))

        PS = [es.enter_context(nc.psum_tensor("ps%d" % i, [128, 512], F32)) for i in range(8)]
        PSB = [Buf() for _ in range(8)]
        psr = Ring(list(zip(PS, PSB)))

        cf = sb(es, "cf_sb", [128, 6, 128], F32); b_cf = Buf()
        cb = sb(es, "cb_sb", [128, 3, 128], BF16); b_cb = Buf()
        ident_f, triF, triB, triBs, ones_f = (cf[:, i, :] for i in range(5))
        selm = cf[:, 5, 0:64]
        ident_b, ones_b = cb[:, 0, :], cb[:, 1, :]
        perm32 = cb[0:32, 2, 0:32]
        sel = sb(es, "sel_sb", [128, 16], F32); b_sel = Buf()
        ccs = sb(es, "ccs", [128, 8, 2], F32); b_ccs = Buf()
        ccb = sb(es, "ccb", [128, 8, 2], BF16)
        mv = sb(es, "mv", [128, 48, 2], F32); b_mv = Buf()
        g12 = sb(es, "g12", [128, 16, 2], F32); b_g12 = Buf()
        nws = sb(es, "nws", [128, 16], F32); b_nws = Buf()
        fnw = sb(es, "fnw_sb", [128, 8], F32); b_fnw = Buf()
        smallv = sb(es, "smallv", [128, 256 + 128 + 18 + 6 + 16 + 16 + 8 + 512 + 512], F32); b_small = Buf()
        o_ = [0]

        def carve(n):
            a = smallv[:, o_[0]:o_[0] + n]
            o_[0] += n
            return a
        qnw_b, kvnw_b, convw, convb, dtb_b, alog_b, rdl_b, dskip_b, snw_b = (carve(n) for n in (256, 128, 18, 6, 16, 16, 8, 512, 512))
        a_b = sb(es, "a_b", [128, 16], F32); b_ab = Buf()
        laret = sb(es, "laret", [128, 8], F32); b_laret = Buf()
        la_all = sb(es, "la_all", [128, NCH, 24], F32)
        b_la = [Buf() for _ in range(NCH)]
        hxT = sb(es, "hxT", [128, 8, NT], BF16)
        b_hx = [Buf() for _ in BLOCKS]
        b_mix = [Buf() for _ in range(NCH)]
        rope = None

        fw.dma("sp", cf[:, :, :], cf_in, writes=[b_cf])
        fw.dma("sp", cb[:, :, :], cb_in, writes=[b_cb])
        fw.dma("sp", sel[:, :], sel_in, writes=[b_sel])
        fw.dma("sp", ccs[:, :, :], cc_in, writes=[b_ccs])
        fw.dma("sp", fnw[:, :], fnw_in, writes=[b_fnw])
        op("act", lambda e: e.activation(out=ccb[:, :, :], in_=ccs[:, :, :], func=AF.Silu), reads=[b_ccs], writes=[b_ccs])

        dbg_bufs = []

        def dump(name, ap, bufs):
            if name in dbg_out:
                b_ = Buf()
                dbg_bufs.append(b_)
                ncol = ap.shape[-1]
                for c0 in range(0, ncol, 1024):
                    c1 = min(ncol, c0 + 1024)
                    fw.dma("pool", dbg_out[name][:, c0:c1], ap[:, c0:c1], reads=list(bufs), writes=[b_])

        def stop_at(tag):
            if stop == tag:
                raise _Stop()

        def blk_of_chunk(c):
            return 0 if c < 2 else 1 + (c - 2) // 4

        lm = None
        stopped = False
        try:
          for L in range(n_layers):
              last = L == n_layers - 1
              x_src = xT_in if L == 0 else xres
              fw.barrier()
              offs = 0
              for src, n in ((qnw_in, 256), (kvnw_in, 128), (convw_in, 18), (convb_in, 6), (dtb_in, 16), (alog_in, 16),
                             (rdl_in, 8), (dskip_in, 512), (snw_in, 512)):
                  fw.dma("sp", smallv[:, offs:offs + n], src[L], writes=[b_small])
                  offs += n
              fw.dma("sp", nws[:, :], nw_in[L], writes=[b_nws])
              op("act", lambda e: e.activation(out=a_b[:, :], in_=alog_b, func=AF.Exp), reads=[b_small], writes=[b_ab])
              op("dve", lambda e: e.tensor_scalar(out=a_b[:, :], in0=a_b[:, :], scalar1=-1.0, scalar2=None, op0=ALU.mult), reads=[b_ab], writes=[b_ab])
              op("act", lambda e: e.activation(out=laret[:, :], in_=rdl_b, func=AF.Exp, scale=-1.0), reads=[b_small], writes=[b_laret])
              op("act", lambda e: e.activation(out=laret[:, :], in_=laret[:, :], func=AF.Ln, bias=1.0), reads=[b_laret], writes=[b_laret])
              op("dve", lambda e: e.tensor_scalar(out=laret[:, :], in0=laret[:, :], scalar1=-1.0, scalar2=None, op0=ALU.mult), reads=[b_laret], writes=[b_laret])

              with ExitStack() as ph:
                  mw = [sb(ph, "mw%d" % i, [128, 8, 512], BF16) for i in range(2)]
                  b_mw = [Buf(), Buf()]
                  mrow = sb(ph, "mrow", [2, 6 * D], F32); b_mrow = Buf()
                  mbias = sb(ph, "mbias", [2, 6 * D], F32); b_mbias = Buf()
                  for v in range(2):
                      fw.dma("sp", mbias[v:v + 1, :], mod_b[L], writes=[b_mbias])
                  for j in range(12):
                      t, bt = mw[j % 2], b_mw[j % 2]
                      fw.dma("pool", t[:, :, :], mod_w[L, :, j * 512:(j + 1) * 512].rearrange("(k p) n -> p k n", p=128), writes=[bt])
                      pt, pb = psr.get()
                      fw.group("pe", [(lambda e, k=k: e.matmul(pt[0:2, :], ccb[:, k, :], t[:, k, :], start=(k == 0), stop=(k == 7))) for k in range(8)],
                               reads=[b_ccs, bt], writes=[pb])
                      op("dve", lambda e: e.tensor_tensor(out=mrow[:, j * 512:(j + 1) * 512], in0=pt[0:2, :], in1=mbias[:, j * 512:(j + 1) * 512], op=ALU.add),
                         reads=[pb, b_mbias], writes=[b_mrow])
                  pt, pb = psr.get()
                  fw.group("pe", [(lambda e, j=j: e.transpose(pt[:, 2 * j:2 * j + 2], mrow[0:2, j * 128:(j + 1) * 128], ident_f[0:2, 0:2])) for j in range(48)],
                           reads=[b_mrow, b_cf], writes=[pb])
                  op("dve", lambda e: e.tensor_copy(mv[:, :, :].rearrange("p j v -> p (j v)"), pt[:, 0:96]), reads=[pb], writes=[b_mv])
                  for n_, (sc0, nw0) in enumerate(((8, 0), (32, 8))):
                      op("dve", lambda e: e.tensor_scalar(out=g12[:, n_ * 8:(n_ + 1) * 8, :], in0=mv[:, sc0:sc0 + 8, :], scalar1=1.0, scalar2=None, op0=ALU.add),
                         reads=[b_mv], writes=[b_g12])
                      op("dve", lambda e: e.tensor_tensor(out=g12[:, n_ * 8:(n_ + 1) * 8, :], in0=g12[:, n_ * 8:(n_ + 1) * 8, :],
                                                           in1=nws[:, nw0:nw0 + 8].unsqueeze(2).to_broadcast([128, 8, 2]), op=ALU.mult),
                         reads=[b_g12, b_nws], writes=[b_g12])

              if L == 0:
                  dump("mv", mv[:, :, :].rearrange("p j v -> p (j v)"), [b_mv])
                  dump("g12", g12[:, :, :].rearrange("p j v -> p (j v)"), [b_g12])
                  stop_at("mod")
              fw.barrier()
              def norm_mod(xb_ap, xb_buf, t0, W, v, gofs, shofs, wk, out_buf):
                  sq, b_sq = wk["sq"].get()
                  op("act", lambda e: e.activation(out=sq[:, :, 0:W], in_=xb_ap, func=AF.Square), reads=[xb_buf], writes=[b_sq])
                  pt, pb = psr.get()
                  fw.group("pe", [(lambda e, k=k: e.matmul(pt[:, 0:W], ones_b, sq[:, k, 0:W], start=(k == 0), stop=(k == 7))) for k in range(8)],
                           reads=[b_sq, b_cb], writes=[pb])
                  rs, b_rs = wk["rs"].get()
                  op("act", lambda e: e.activation(out=rs[:, 0:W], in_=pt[:, 0:W], func=AF.Sqrt, scale=1.0 / D, bias=EPS), reads=[pb], writes=[b_rs])
                  op("dve", lambda e: e.reciprocal(out=rs[:, 0:W], in_=rs[:, 0:W]), reads=[b_rs], writes=[b_rs])
                  for k in range(8):
                      tmp, b_tmp = wk["tmp"].get()
                      op("dve", lambda e: e.tensor_tensor(out=tmp[:, 0:W], in0=xb_ap[:, k, :], in1=rs[:, 0:W], op=ALU.mult), reads=[xb_buf, b_rs], writes=[b_tmp])
                      op("act", lambda e: e.activation(out=hxT[:, k, t0:t0 + W], in_=tmp[:, 0:W], func=AF.Identity,
                                                        scale=g12[:, gofs + k, v:v + 1], bias=mv[:, shofs + k, v:v + 1]),
                         reads=[b_tmp, b_g12, b_mv], writes=[out_buf])

              lm = ExitStack()
              mixT = sb(lm, "mixT", [128, 8, NT], BF16)
              b_xres = Buf()
              with ExitStack() as mx:
                  xbcT = xbc_dram
                  qT = sb(mx, "qT", [96, 4, NT], BF16); b_qT = [Buf() for _ in BLOCKS]
                  hal = sb(mx, "hal", [128, 6, 4], BF16); b_hal = Buf()
                  b_xbc = [Buf() for _ in BLOCKS]
                  b_halo = Buf()
                  XO = lambda t: t + 1 if t < TC else t + 3
                  with ExitStack() as s1:
                      W1a = sb(s1, "W1a", [128, 8, 416], BF16); b_W1a = Buf()
                      W1x = sb(s1, "W1x", [128, 8, 768], BF16); b_W1x = Buf()
                      wqb = sb(s1, "wqb", [128, 2, 384], BF16); b_wqb = Buf()
                      wqr = sb(s1, "wqr", [128, 2, 384], BF16); b_wqr = Buf()
                      ropes_t = sb(s1, "ropes", [128, 2, 512], F32); b_rope = Buf()
                      xst = sb(s1, "xst", [128, 6, 512], BF16); b_xst = Buf()
                      bcol_r = Ring([(sb(s1, "bcol%d" % i, [128, 136], BF16), Buf()) for i in range(2)])
                      kvl = sb(s1, "kvl", [128, 2, NT], BF16); b_kvl = Buf()
                      xb_r = Ring([(sb(s1, "xblk%d" % i, [128, 8, 512], F32), Buf()) for i in range(1)])
                      wk = {"sq": Ring([(sb(s1, "sq%d" % i, [128, 8, 512], BF16), Buf()) for i in range(1)]),
                            "rs": Ring([(sb(s1, "rs%d" % i, [128, 512], F32), Buf()) for i in range(1)]),
                            "tmp": Ring([(sb(s1, "tmp%d" % i, [128, 512], F32), Buf()) for i in range(2)])}
                      tk_r = Ring([(sb(s1, "tk%d" % i, [128, 416], BF16), Buf()) for i in range(2)])
                      st_r = Ring([(sb(s1, "st%d" % i, [128, 4], F32), Buf()) for i in range(2)])
                      cqT = sb(s1, "cqT", [128, 2, 512], BF16); b_cqT = Buf()
                      kpT = sb(s1, "kpT", [32, 512], BF16); b_kpT = Buf()
                      junk = sb(s1, "junk", [128, 256], F32); b_junk = Buf()
                      rt_r = Ring([(sb(s1, "rt%d" % i, [128, 512], F32), Buf()) for i in range(2)])
                      fw.dma("pool", W1a[:, :, :], w_in[L, :, 0:416].rearrange("(k p) n -> p k n", p=128), writes=[b_W1a])
                      fw.dma("pool", W1x[:, :, :], w_in[L, :, 1952:2720].rearrange("(k p) n -> p k n", p=128), writes=[b_W1x])
                      fw.dma("pool", wqb[:, :, :], w_qb[L].rearrange("(k p) n -> p k n", p=128), writes=[b_wqb])
                      fw.dma("pool", wqr[:, :, :], w_qbrot[L].rearrange("(k p) n -> p k n", p=128), writes=[b_wqr])
                      b_send = Buf()
                      zrow = sb(s1, "zrow", [2, 1536], BF16); b_zrow = Buf()
                      op("pool", lambda e: e.memset(zrow[:, :], 0.0), writes=[b_zrow])
                      fw.dma("sp", kv_send[160:162, 768:NT], zrow[:, :], reads=[b_zrow], writes=[b_send], chanbuf=b_zrow)
                      for bi, (t0, W, v) in enumerate(BLOCKS):
                          fw.dma("sp", ropes_t[:, :, 0:W], rope_in[:, :, t0:t0 + W], writes=[b_rope])
                          ropes = ropes_t[:, :, :]
                          xb, b_xb = xb_r.get()
                          fw.dma("sp", xb[:, :, 0:W], x_src[:, t0:t0 + W].rearrange("(k p) t -> p k t", p=128), writes=[b_xb])
                          norm_mod(xb[:, :, 0:W], b_xb, t0, W, v, 0, 0, wk, b_hx[bi])
                          if bi == 0:
                              stop_at("s1a")
                          for oc in range(6):
                              pt, pb = psr.get()
                              fw.group("pe", [(lambda e, k=k: e.matmul(pt[:, 0:W], W1x[:, k, oc * 128:(oc + 1) * 128], hxT[:, k, t0:t0 + W], start=(k == 0), stop=(k == 7))) for k in range(8)],
                                       reads=[b_W1x, b_hx[bi]], writes=[pb])
                              op("act", lambda e: e.activation(out=xst[:, oc, 0:W], in_=pt[:, 0:W], func=AF.Copy), reads=[pb], writes=[b_xst])
                          fw.dma("sp", xbcT[:, :, XO(t0):XO(t0) + W], xst[:, :, 0:W], reads=[b_xst], writes=[b_xbc[bi]], chanbuf=b_xst)
                          if bi in (1, 4):
                              ccol = 0 if bi == 1 else W - 1
                              rrow = 160 if bi == 1 else 161
                              bcol, b_bcol = bcol_r.get()
                              op("dve", lambda e: e.tensor_copy(bcol[:, 0:6], xst[:, :, ccol]), reads=[b_xst], writes=[b_bcol])
                              p2, pb2 = psr.get()
                              pbf = p2[:, 0:64].bitcast(BF16)
                              op("pe", lambda e: e.transpose(pbf[0:6, 0:128], bcol[:, 0:6], ident_b), reads=[b_bcol, b_cb], writes=[pb2])
                              op("dve", lambda e: e.tensor_copy(bcol[0:6, 8:136], pbf[0:6, 0:128]), reads=[pb2], writes=[b_bcol])
                              fw.dma("sp", kv_send[rrow:rrow + 1, 0:768].rearrange("o (c p) -> (o c) p", p=128), bcol[0:6, 8:136], reads=[b_bcol], writes=[b_send], chanbuf=b_bcol)
                          if bi == 0:
                              stop_at("s1b")
                          for ti in range(W // 128):
                              tt = t0 + ti * 128
                              pt, pb = psr.get()
                              fw.group("pe", [(lambda e, k=k: e.matmul(pt[:, 0:416], hxT[:, k, tt:tt + 128], W1a[:, k, :], start=(k == 0), stop=(k == 7))) for k in range(8)],
                                       reads=[b_W1a, b_hx[bi]], writes=[pb])
                              if bi == 0 and ti == 0:
                                  stop_at("t1")
                              stt_, b_st = st_r.get()
                              op("act", lambda e: e.activation(out=junk[:, 0:256], in_=pt[:, 0:256], func=AF.Square, accum_out=stt_[:, 0:1]), reads=[pb], writes=[b_junk, b_st])
                              op("act", lambda e: e.activation(out=junk[:, 0:128], in_=pt[:, 256:384], func=AF.Square, accum_out=stt_[:, 1:2]), reads=[pb], writes=[b_junk, b_st])
                              op("act", lambda e: e.activation(out=stt_[:, 2:3], in_=stt_[:, 0:1], func=AF.Sqrt, scale=1.0 / 256, bias=EPS), reads=[b_st], writes=[b_st])
                              op("act", lambda e: e.activation(out=stt_[:, 3:4], in_=stt_[:, 1:2], func=AF.Sqrt, scale=1.0 / 128, bias=EPS), reads=[b_st], writes=[b_st])
                              op("dve", lambda e: e.reciprocal(out=stt_[:, 2:4], in_=stt_[:, 2:4]), reads=[b_st], writes=[b_st])
                              if bi == 0 and ti == 0:
                                  stop_at("t2")
                              tk, b_tk = tk_r.get()
                              op("dve", lambda e: e.scalar_tensor_tensor(out=tk[:, 0:256], in0=pt[:, 0:256], scalar=stt_[:, 2:3], op0=ALU.mult, in1=qnw_b, op1=ALU.mult),
                                 reads=[pb, b_st, b_small], writes=[b_tk])
                              op("dve", lambda e: e.scalar_tensor_tensor(out=tk[:, 256:384], in0=pt[:, 256:384], scalar=stt_[:, 3:4], op0=ALU.mult, in1=kvnw_b, op1=ALU.mult),
                                 reads=[pb, b_st, b_small], writes=[b_tk])
                              op("act", lambda e: e.activation(out=tk[:, 384:416], in_=pt[:, 384:416], func=AF.Copy), reads=[pb], writes=[b_tk])
                              if bi == 0 and ti == 0:
                                  stop_at("t3")
                              p2, pb2 = psr.get()
                              pbf = p2[:, 0:256].bitcast(BF16)
                              fw.group("pe", [lambda e: e.transpose(pbf[:, 0:128], tk[:, 0:128], ident_b),
                                              lambda e: e.transpose(pbf[:, 128:256], tk[:, 128:256], ident_b),
                                              lambda e: e.transpose(pbf[:, 256:384], tk[:, 256:384], ident_b),
                                              lambda e: e.transpose(pbf[0:32, 384:512], tk[:, 384:416], ident_b)],
                                       reads=[b_tk, b_cb], writes=[pb2])
                              if bi == 0 and ti == 0:
                                  stop_at("t4")
                              op("dve", lambda e: e.tensor_copy(cqT[:, :, ti * 128:(ti + 1) * 128], pbf[:, 0:256].rearrange("p (c t) -> p c t", c=2)), reads=[pb2], writes=[b_cqT])
                              if bi == 0 and ti == 0:
                                  stop_at("t5")
                              op("dve", lambda e: e.tensor_copy(kvl[:, 0, tt:tt + 128], pbf[:, 256:384]), reads=[pb2], writes=[b_kvl])
                              if bi == 0 and ti == 0:
                                  stop_at("t6")
                              op("dve", lambda e: e.tensor_copy(kpT[:, ti * 128:(ti + 1) * 128], pbf[0:32, 384:512]), reads=[pb2], writes=[b_kpT])
                          if bi == 0:
                              stop_at("s1c")
                          pt, pb = psr.get()
                          op("pe", lambda e: e.matmul(pt[0:32, 0:W], perm32, kpT[:, 0:W], start=True, stop=True), reads=[b_cb, b_kpT], writes=[pb])
                          r1, b_r1 = rt_r.get()
                          r2, b_r2 = rt_r.get()
                          op("dve", lambda e: e.tensor_tensor(out=r1[0:32, 0:W], in0=pt[0:32, 0:W], in1=ropes_t[0:32, 1, 0:W], op=ALU.mult), reads=[pb, b_rope], writes=[b_r1])
                          op("pool", lambda e: e.tensor_tensor(out=r2[0:32, 0:W], in0=kpT[:, 0:W], in1=ropes_t[0:32, 0, 0:W], op=ALU.mult), reads=[b_kpT, b_rope], writes=[b_r2])
                          op("dve", lambda e: e.tensor_tensor(out=kvl[0:32, 1, t0:t0 + W], in0=r1[0:32, 0:W], in1=r2[0:32, 0:W], op=ALU.add), reads=[b_r1, b_r2], writes=[b_kvl])
                          if bi == 0:
                              stop_at("s1d")
                          for h in range(4):
                              pq, pbq = psr.get()
                              pr, pbr = psr.get()
                              fw.group("pe", [(lambda e, k=k: e.matmul(pq[0:96, 0:W], wqb[:, k, h * 96:(h + 1) * 96], cqT[:, k, 0:W], start=(k == 0), stop=(k == 1))) for k in range(2)],
                                       reads=[b_wqb, b_cqT], writes=[pbq])
                              fw.group("pe", [(lambda e, k=k: e.matmul(pr[0:96, 0:W], wqr[:, k, h * 96:(h + 1) * 96], cqT[:, k, 0:W], start=(k == 0), stop=(k == 1))) for k in range(2)],
                                       reads=[b_wqr, b_cqT], writes=[pbr])
                              op("act", lambda e: e.activation(out=qT[0:64, h, t0:t0 + W], in_=pq[0:64, 0:W], func=AF.Copy), reads=[pbq], writes=[b_qT[bi]])
                              r1, b_r1 = rt_r.get()
                              r2, b_r2 = rt_r.get()
                              op("dve", lambda e: e.tensor_tensor(out=r1[64:96, 0:W], in0=pr[64:96, 0:W], in1=ropes_t[64:96, 1, 0:W], op=ALU.mult), reads=[pbr, b_rope], writes=[b_r1])
                              op("dve", lambda e: e.tensor_tensor(out=r2[64:96, 0:W], in0=pq[64:96, 0:W], in1=ropes_t[64:96, 0, 0:W], op=ALU.mult), reads=[pbq, b_rope], writes=[b_r2])
                              if bi == 0 and h == 0:
                                  stop_at("q0")
                              op("pool", lambda e: e.tensor_tensor(out=qT[64:96, h, t0:t0 + W], in0=r1[64:96, 0:W], in1=r2[64:96, 0:W], op=ALU.add), reads=[b_r1, b_r2], writes=[b_qT[bi]])
                              if bi == 0 and h == 0:
                                  stop_at("q1")
                          if bi == 0:
                              stop_at("b0")
                          if bi == 1:
                              stop_at("b1")
                      fw.dma("sp", kv_send[0:128, :], kvl[:, 0, :], reads=[b_kvl], writes=[b_send])
                      fw.dma("sp", kv_send[128:160, :], kvl[0:32, 1, :], reads=[b_kvl], writes=[b_send])
                      if L == 0:
                          dump("hx", hxT[:, :, :].rearrange("p k t -> p (k t)"), b_hx)
                          dump("qT", qT[:, :, :].rearrange("p k t -> p (k t)"), b_qT)
                          dump("kvl", kvl[:, :, :].rearrange("p k t -> p (k t)"), [b_kvl])
                          dump("xbcA", xbc_dram[:, 0, 0:260], b_xbc)
                          stop_at("s1")
                      b_all = Buf()
                      fw.cc("AllGather", kv_send.ap().opt(), kv_all.ap().opt(), GROUPS, [b_send], [b_all], b_all)
                  fw.barrier()
                  with ExitStack() as hh:
                      bd = sb(hh, "bd", [8, 768], BF16); b_bd = Buf()
                      selb = sb(hh, "selb", [8, 2], BF16); b_selb = Buf()
                      for r in range(4):
                          fw.dma("sp", bd[2 * r:2 * r + 2, :], kv_all[r * 162 + 160:r * 162 + 162, 0:768], reads=[b_all], writes=[b_bd])
                      op("dve", lambda e: e.tensor_copy(selb[:, :], sel[0:8, 0:2]), reads=[b_sel], writes=[b_selb])
                      op("dve", lambda e: e.memset(hal[:, :, :], 0.0), writes=[b_hal])
                      for oc in range(6):
                          pt, pb = psr.get()
                          op("pe", lambda e: e.matmul(pt[:, 0:2], bd[:, oc * 128:(oc + 1) * 128], selb[:, :], start=True, stop=True), reads=[b_bd, b_selb], writes=[pb])
                          op("dve", lambda e: e.tensor_copy(hal[:, oc, 2:4], pt[:, 0:2]), reads=[pb], writes=[b_hal])

                  fw.barrier()
                  with ExitStack() as at:
                      NK = TC + 4 * TL
                      ckT = sb(at, "ckT", [128, NK], BF16); b_ck = Buf()
                      KT = sb(at, "KT", [96, NK], BF16); b_KTp = Buf(); b_KTn = Buf()
                      Va = sb(at, "Va", [128, NK // 128, 4, 65], BF16); b_Va = Buf()
                      wkb = sb(at, "wkb", [128, 256], BF16); b_wkb = Buf()
                      wvb = sb(at, "wvb", [128, 256], BF16); b_wvb = Buf()
                      pT_r = Ring([(sb(at, "pT%d" % i, [128, 512], BF16), Buf()) for i in range(5)])
                      s_ring = Ring(list(zip(PS[0:4], PSB[0:4])))
                      o_ring = Ring(list(zip(PS[4:6], PSB[4:6])))
                      psr_keep = psr
                      psr = Ring(list(zip(PS[6:8], PSB[6:8])))
                      osb = sb(at, "osb", [65, 512], F32); b_osb = Buf()
                      rb = sb(at, "rb", [64, 512], F32); b_rb = Buf()
                      ot = sb(at, "ot", [64, 512], BF16); b_ot = Buf()
                      fw.dma("pool", wkb[:, :], w_kb[L], writes=[b_wkb])
                      fw.dma("pool", wvb[:, :], w_vb[L], writes=[b_wvb])
                      fw.dma("sp", ckT[:, 0:TC], kv_all[0:128, 0:TC], reads=[b_all], writes=[b_ck])
                      fw.dma("sp", KT[64:96, 0:TC], kv_all[128:160, 0:TC], reads=[b_all], writes=[b_KTp])
                      for r in range(4):
                          fw.dma("sp", ckT[:, TC + r * TL:TC + (r + 1) * TL], kv_all[r * 162:r * 162 + 128, TC:NT], reads=[b_all], writes=[b_ck])
                          fw.dma("sp", KT[64:96, TC + r * TL:TC + (r + 1) * TL], kv_all[r * 162 + 128:r * 162 + 160, TC:NT], reads=[b_all], writes=[b_KTp])
                      op("pool", lambda e: e.memset(Va[:, :, :, 64:65], 1.0), writes=[b_Va])
                      for kc in range(NK // 128):
                          pt, pb = psr.get()
                          op("pe", lambda e: e.matmul(pt[:, 0:256], ckT[:, kc * 128:(kc + 1) * 128], wvb[:, :], start=True, stop=True), reads=[b_ck, b_wvb], writes=[pb])
                          eng = "act" if kc % 2 == 0 else "dve"
                          if eng == "act":
                              op("act", lambda e: e.activation(out=Va[:, kc, :, 0:64], in_=pt[:, 0:256].rearrange("p (h d) -> p h d", h=4), func=AF.Copy), reads=[pb], writes=[b_Va])
                          else:
                              op("dve", lambda e: e.tensor_copy(Va[:, kc, :, 0:64], pt[:, 0:256].rearrange("p (h d) -> p h d", h=4)), reads=[pb], writes=[b_Va])
                      for h in range(4):
                          for kb in range((NK + 511) // 512):
                              k0 = kb * 512
                              kw_ = min(512, NK - k0)
                              pt, pb = psr.get()
                              op("pe", lambda e: e.matmul(pt[0:64, 0:kw_], wkb[:, h * 64:(h + 1) * 64], ckT[:, k0:k0 + kw_], start=True, stop=True), reads=[b_wkb, b_ck], writes=[pb])
                              if kb % 2 == 0:
                                  op("act", lambda e: e.activation(out=KT[0:64, k0:k0 + kw_], in_=pt[0:64, 0:kw_], func=AF.Copy), reads=[pb], writes=[b_KTn])
                              else:
                                  op("dve", lambda e: e.tensor_copy(KT[0:64, k0:k0 + kw_], pt[0:64, 0:kw_]), reads=[pb], writes=[b_KTn])
                          for bi, (t0, W, v) in enumerate(BLOCKS):
                              if v == 1 and last:
                                  continue
                              nkc = 2 if v == 1 else NK // 128
                              po, pbo = o_ring.get()
                              LA = 2
                              pend = []
                              for kc in range(nkc + LA):
                                  if kc < nkc:
                                      ps_, pbs = s_ring.get()
                                      op("pe", lambda e: e.matmul(ps_[:, 0:W], KT[0:96, kc * 128:(kc + 1) * 128], qT[0:96, h, t0:t0 + W], start=True, stop=True),
                                         reads=[b_KTn, b_KTp, b_qT[bi]], writes=[pbs])
                                      pT, b_pT = pT_r.get()
                                      op("act", lambda e: e.activation(out=pT[:, 0:W], in_=ps_[:, 0:W], func=AF.Exp, scale=MLA_SCALE), reads=[pbs], writes=[b_pT])
                                      pend.append((pT, b_pT))
                                  if kc >= LA:
                                      kk = kc - LA
                                      pT2, b_pT2 = pend[kk]
                                      op("pe", lambda e: e.matmul(po[0:65, 0:W], Va[:, kk, h, :], pT2[:, 0:W], start=(kk == 0), stop=(kk == nkc - 1)),
                                         reads=[b_Va, b_pT2], writes=[pbo])
                              op("dve", lambda e: e.tensor_copy(osb[:, 0:W], po[0:65, 0:W]), reads=[pbo], writes=[b_osb])
                              op("dve", lambda e: e.reciprocal(out=osb[64:65, 0:W], in_=osb[64:65, 0:W]), reads=[b_osb], writes=[b_osb])
                              pt, pb = psr.get()
                              op("pe", lambda e: e.matmul(pt[0:64, 0:W], selm[0:65, :], osb[0:65, 0:W], start=True, stop=True), reads=[b_osb, b_cf], writes=[pb])
                              op("act", lambda e: e.activation(out=rb[:, 0:W], in_=pt[0:64, 0:W], func=AF.Copy), reads=[pb], writes=[b_rb])
                              mixw = [b_mix[c] for c in range(t0 // 128, (t0 + W) // 128)]
                              if h % 2 == 0:
                                  op("dve", lambda e: e.tensor_tensor(out=mixT[0:64, h // 2, t0:t0 + W], in0=osb[0:64, 0:W], in1=rb[:, 0:W], op=ALU.mult),
                                     reads=[b_osb, b_rb], writes=mixw)
                              else:
                                  op("dve", lambda e: e.tensor_tensor(out=ot[:, 0:W], in0=osb[0:64, 0:W], in1=rb[:, 0:W], op=ALU.mult), reads=[b_osb, b_rb], writes=[b_ot])
                                  op("dve", lambda e: e.tensor_copy(mixT[64:128, h // 2, t0:t0 + W], ot[:, 0:W]), reads=[b_ot], writes=mixw)

                  psr = psr_keep
                  if L == 0:
                      dump("att", mixT[:, 0:2, :].rearrange("p k t -> p (k t)"), b_mix)
                      stop_at("att")
                  fw.barrier()
                  with ExitStack() as sc:
                      W2 = sb(sc, "W2", [128, 8, 1552], BF16); b_W2 = Buf()
                      fw.dma("pool", W2[:, :, 0:1536], w_in[L, :, 416:1952].rearrange("(k p) n -> p k n", p=128), writes=[b_W2])
                      fw.dma("pool", W2[:, :, 1536:1552], w_in[L, :, 2720:2736].rearrange("(k p) n -> p k n", p=128), writes=[b_W2])
                      S_run = [sb(sc, "Srun%d" % d, [128, 6, 128], F32) for d in range(2)]; b_S = [Buf(), Buf()]
                      Sbd = [sb(sc, "Sbd%d" % d, [128, 6, 128], BF16) for d in range(2)]; b_Sbd = [Buf(), Buf()]
                      S_ctx = [sb(sc, "Sctx%d" % d, [128, 6, 128], F32) for d in range(2)]; b_Sctx = [Buf(), Buf()]
                      Lacc = sb(sc, "Lacc", [128, 24], F32); b_Lacc = Buf()
                      eL = sb(sc, "eL", [128, 12], F32); b_eL = Buf()
                      cd_r = Ring([(sb(sc, "cd%d" % i, [128, CDW], BF16), Buf()) for i in range(2)])
                      xin_pre = {}
                      xin_r = Ring([(sb(sc, "xin%d" % i, [128, 6, 130], BF16), Buf()) for i in range(2)])
                      xact_r = Ring([(sb(sc, "xact%d" % i, [128, 6, 128], BF16), Buf()) for i in range(2)])
                      cacc_r = Ring([(sb(sc, "cacc%d" % i, [128, 128], F32), Buf()) for i in range(3)])
                      dt_r = Ring([(sb(sc, "dtt%d" % i, [128, 16], F32), Buf()) for i in range(2)])
                      scl_r = Ring([(sb(sc, "scl%d" % i, [128, 64], F32), Buf()) for i in range(2)])
                      kw_r = Ring([(sb(sc, "kw%d" % i, [128, 128], BF16), Buf()) for i in range(3)])
                      dec_r = Ring([(sb(sc, "dec%d" % i, [128, 128], F32), Buf()) for i in range(4)])
                      qkm_r = Ring([(sb(sc, "qkm%d" % i, [128, 128], F32), Buf()) for i in range(6)])
                      scT_r = Ring([(sb(sc, "scT%d" % i, [128, 128], BF16), Buf()) for i in range(5)])
                      yA_r = Ring([(sb(sc, "yAs%d" % i, [128, 768], F32), Buf()) for i in range(2)])
                      yt_r = Ring([(sb(sc, "yts%d" % i, [128, 768], F32), Buf()) for i in range(2)])
                      yf_r = Ring([(sb(sc, "yfs%d" % i, [128, 768], F32), Buf()) for i in range(1)])
                      gt_r = Ring([(sb(sc, "gts%d" % i, [128, 768], F32), Buf()) for i in range(1)])
                      mo_r = Ring([(sb(sc, "mos%d" % i, [128, 768], BF16), Buf()) for i in range(2)])
                      gst_r = Ring([(sb(sc, "gst%d" % i, [128, 32], F32), Buf()) for i in range(2)])
                      sr_t = sb(sc, "sr_t", [128, 1560], F32); b_sr = Buf()
                      dm_t = sb(sc, "dm_t", [128, 12], F32); b_dm = Buf()
                      tri_d = [triF, triB]
                      b_cdd = [Buf() for _ in range(NCH)]
                      b_yfd = [Buf() for _ in range(NCH)]

                      def hx_bufs(c):
                          return [b_hx[blk_of_chunk(c)]]

                      def xbc_bufs(c):
                          bi = blk_of_chunk(c)
                          return [b_xbc[i] for i in range(max(0, bi - 1), min(len(BLOCKS), bi + 2))]

                      def small_stats(c, d):
                          pt, pb = psr.get()
                          la_c = la_all[:, c, d * 12:(d + 1) * 12]
                          op("pe", lambda e: e.matmul(pt[:, 0:12], tri_d[d], la_c, start=True, stop=True), reads=[b_cf, b_la[c]], writes=[pb])
                          pt2, pb2 = psr.get()
                          op("pe", lambda e: e.matmul(pt2[:, 0:12], ones_f, la_c, start=True, stop=True), reads=[b_cf, b_la[c]], writes=[pb2])
                          scl, b_scl = scl_r.get()
                          op("act", lambda e: e.activation(out=scl[:, 0:12], in_=pt[:, 0:12], func=AF.Copy, scale=-1.0), reads=[pb], writes=[b_scl])
                          op("act", lambda e: e.activation(out=scl[:, 12:24], in_=pt[:, 0:12], func=AF.Exp), reads=[pb], writes=[b_scl])
                          op("act", lambda e: e.activation(out=scl[:, 24:36], in_=pt2[:, 0:12], func=AF.Exp), reads=[pb2], writes=[b_scl])
                          op("dve", lambda e: e.tensor_tensor(out=scl[:, 36:48], in0=pt2[:, 0:12], in1=scl[:, 0:12], op=ALU.add), reads=[pb2, b_scl], writes=[b_scl])
                          op("act", lambda e: e.activation(out=scl[:, 36:48], in_=scl[:, 36:48], func=AF.Exp), reads=[b_scl], writes=[b_scl])
                          op("dve", lambda e: e.tensor_copy(scl[:, 48:60], pt2[:, 0:12]), reads=[pb2], writes=[b_scl])
                          return scl, b_scl

                      def kv_pair(cd, m, d):
                          if m < 2:
                              return cd[:, CD_KTOK + m * 128:CD_KTOK + (m + 1) * 128], cd[:, CD_VTOK + m * 128:CD_VTOK + (m + 1) * 128]
                          xo = (CD_XF if d == 0 else CD_XB) + (m - 2) * 128
                          return cd[:, CD_BTOK:CD_BTOK + 128], cd[:, xo:xo + 128]

                      def chunk_state(cd, b_cd, scl, b_scl, d, m):
                          k_ap, v_ap = kv_pair(cd, m, d)
                          kw, b_kw = kw_r.get()
                          op("dve", lambda e: e.tensor_tensor(out=kw[:, :].rearrange("p (h n) -> p h n", h=2), in0=k_ap.rearrange("p (h n) -> p h n", h=2),
                                                               in1=scl[:, 36 + 2 * m:38 + 2 * m].unsqueeze(2).to_broadcast([128, 2, 64]), op=ALU.mult),
                             reads=[b_cd, b_scl], writes=[b_kw])
                          pt, pb = psr.get()
                          op("pe", lambda e: e.matmul(pt[:, 0:128], kw[:, :], v_ap, start=True, stop=True), reads=[b_kw, b_cd], writes=[pb])
                          return pt, pb

                      kwb_r = Ring([(sb(sc, "kwb%d" % i, [128, 128], BF16), Buf()) for i in range(12)])

                      def states_batch(cd, b_cd, scl, b_scl, d, slots):
                          kws = []
                          for m in range(6):
                              k_ap, v_ap = kv_pair(cd, m, d)
                              kw, b_kw = kwb_r.get()
                              op("pool", lambda e: e.tensor_tensor(out=kw[:, :].rearrange("p (h n) -> p h n", h=2), in0=k_ap.rearrange("p (h n) -> p h n", h=2),
                                                                    in1=scl[:, 36 + 2 * m:38 + 2 * m].unsqueeze(2).to_broadcast([128, 2, 64]), op=ALU.mult),
                                 reads=[b_cd, b_scl], writes=[b_kw])
                              kws.append((kw, b_kw, v_ap))
                          outs = []
                          for m in range(6):
                              kw, b_kw, v_ap = kws[m]
                              pt, pb = slots[m]
                              op("pe", lambda e: e.matmul(pt, kw[:, :], v_ap, start=True, stop=True), reads=[b_kw, b_cd], writes=[pb])
                              outs.append((pt, pb))
                          return outs

                      def upd_state(S, b_S_, scal, b_scal, so, m, add_ap, add_buf):
                          for half in range(2):
                              r0, r1 = half * 64, half * 64 + 64
                              u = 2 * m + half
                              op("dve", lambda e: e.scalar_tensor_tensor(out=S[r0:r1, m, r0:r1], in0=S[r0:r1, m, r0:r1], scalar=scal[r0:r1, so + u:so + u + 1],
                                                                          op0=ALU.mult, in1=add_ap[r0:r1, r0:r1], op1=ALU.add),
                                 reads=[b_S_, b_scal, add_buf], writes=[b_S_])

                      for d in range(2):
                          op("pool", lambda e: e.memset(S_run[d][:, :, :], 0.0), writes=[b_S[d]])
                      op("pool", lambda e: e.memset(Lacc[:, :], 0.0), writes=[b_Lacc])
                      for c in range(NCH):
                          tt = c * 128
                          cd, b_cd = cd_r.get()
                          hb = hx_bufs(c)
                          for col0, dst, scale in ((0, CD_QT, 0.125), (128, CD_QT + 128, 0.125), (256, CD_KT, 1.0), (384, CD_KT + 128, 1.0)):
                              pt, pb = psr.get()
                              fw.group("pe", [(lambda e, k=k: e.matmul(pt[:, 0:128], W2[:, k, col0:col0 + 128], hxT[:, k, tt:tt + 128], start=(k == 0), stop=(k == 7))) for k in range(8)],
                                       reads=[b_W2] + hb, writes=[pb])
                              op("act", lambda e: e.activation(out=cd[:, dst:dst + 128], in_=pt[:, 0:128], func=AF.Copy, scale=scale), reads=[pb], writes=[b_cd])
                          pt, pb = psr.get()
                          fw.group("pe", [(lambda e, k=k: e.matmul(pt[:, 0:512], hxT[:, k, tt:tt + 128], W2[:, k, 256:768], start=(k == 0), stop=(k == 7))) for k in range(8)],
                                   reads=[b_W2] + hb, writes=[pb])
                          op("dve", lambda e: e.tensor_copy(cd[:, CD_KTOK:CD_KTOK + 256], pt[:, 0:256]), reads=[pb], writes=[b_cd])
                          op("act", lambda e: e.activation(out=cd[:, CD_VTOK:CD_VTOK + 256], in_=pt[:, 256:512], func=AF.Copy), reads=[pb], writes=[b_cd])
                          pt, pb = psr.get()
                          fw.group("pe", [(lambda e, k=k: e.matmul(pt[:, 0:256], hxT[:, k, tt:tt + 128], W2[:, k, 768:1024], start=(k == 0), stop=(k == 7))) for k in range(8)],
                                   reads=[b_W2] + hb, writes=[pb])
                          op("act", lambda e: e.activation(out=cd[:, CD_SG:CD_SG + 256], in_=pt[:, 0:256], func=AF.Silu), reads=[pb], writes=[b_cd])
                          pt, pb = psr.get()
                          fw.group("pe", [(lambda e, k=k: e.matmul(pt[:, 0:512], hxT[:, k, tt:tt + 128], W2[:, k, 1024:1536], start=(k == 0), stop=(k == 7))) for k in range(8)],
                                   reads=[b_W2] + hb, writes=[pb])
                          op("act", lambda e: e.activation(out=cd[:, CD_SZ:CD_SZ + 512], in_=pt[:, 0:512], func=AF.Silu), reads=[pb], writes=[b_cd])
                          pt, pb = psr.get()
                          fw.group("pe", [(lambda e, k=k: e.matmul(pt[:, 0:16], hxT[:, k, tt:tt + 128], W2[:, k, 1536:1552], start=(k == 0), stop=(k == 7))) for k in range(8)],
                                   reads=[b_W2] + hb, writes=[pb])
                          dtt, b_dt = dt_r.get()
                          op("dve", lambda e: e.tensor_tensor(out=dtt[:, :], in0=pt[:, 0:16], in1=dtb_b, op=ALU.add), reads=[pb, b_small], writes=[b_dt])
                          op("act", lambda e: e.activation(out=dtt[:, :], in_=dtt[:, :], func=AF.Exp), reads=[b_dt], writes=[b_dt])
                          op("act", lambda e: e.activation(out=dtt[:, :], in_=dtt[:, :], func=AF.Ln, bias=1.0), reads=[b_dt], writes=[b_dt])
                          for d in range(2):
                              op("dve", lambda e: e.tensor_copy(la_all[:, c, d * 12:d * 12 + 4], laret[:, d * 4:d * 4 + 4]), reads=[b_laret], writes=[b_la[c]])
                              op("dve", lambda e: e.tensor_tensor(out=la_all[:, c, d * 12 + 4:d * 12 + 12], in0=dtt[:, d * 8:d * 8 + 8], in1=a_b[:, d * 8:d * 8 + 8], op=ALU.mult),
                                 reads=[b_dt, b_ab], writes=[b_la[c]])
                          xact, b_xa = xact_r.get()
                          xc = (tt + 1) if tt < TC else (tt + 3)
                          xb_ = xbc_bufs(c)
                          def load_xin(c_):
                              tt_ = c_ * 128
                              xc_ = (tt_ + 1) if tt_ < TC else (tt_ + 3)
                              xin_, b_xin_ = xin_r.get()
                              lo = 1 if c_ in (0, 2) else 0
                              hi = 129 if c_ in (1, NCH - 1) else 130
                              fw.dma("sp", xin_[:, :, lo:hi], xbcT[:, :, xc_ - 1 + lo:xc_ - 1 + hi], reads=xbc_bufs(c_), writes=[b_xin_])
                              return xin_, b_xin_
                          if c not in xin_pre:
                              xin_pre[c] = load_xin(c)
                          xin, b_xin = xin_pre.pop(c)
                          if c + 1 < NCH:
                              xin_pre[c + 1] = load_xin(c + 1)
                          xb_ = [b_xin]
                          if L == 0 and c in (0, 1):
                              dump("xin%d" % c, xin[:, :, :].rearrange("p k t -> p (k t)"), [b_xin])
                          for cc_, xcol, hcol in ((0, 0, 0), (1, 129, 1), (2, 0, 2), (NCH - 1, 129, 3)):
                              if c == cc_:
                                  op("dve", lambda e: e.tensor_copy(xin[:, :, xcol], hal[:, :, hcol]), reads=[b_hal, b_xin], writes=[b_xin])
                          for oc in range(6):
                              ca, b_ca = cacc_r.get()
                              op("dve", lambda e: e.tensor_scalar(out=ca[:, :], in0=xin[:, oc, 0:128], scalar1=convw[:, oc * 3:oc * 3 + 1], scalar2=None, op0=ALU.mult),
                                 reads=xb_ + [b_small], writes=[b_ca])
                              op("dve", lambda e: e.scalar_tensor_tensor(out=ca[:, :], in0=xin[:, oc, 1:129], scalar=convw[:, oc * 3 + 1:oc * 3 + 2], op0=ALU.mult, in1=ca[:, :], op1=ALU.add),
                                 reads=xb_ + [b_small, b_ca], writes=[b_ca])
                              op("dve", lambda e: e.scalar_tensor_tensor(out=ca[:, :], in0=xin[:, oc, 2:130], scalar=convw[:, oc * 3 + 2:oc * 3 + 3], op0=ALU.mult, in1=ca[:, :], op1=ALU.add),
                                 reads=xb_ + [b_small, b_ca], writes=[b_ca])
                              if oc < 4:
                                  op("act", lambda e: e.activation(out=xact[:, oc, :], in_=ca[:, :], func=AF.Silu, bias=convb[:, oc:oc + 1]), reads=[b_ca, b_small], writes=[b_xa])
                              else:
                                  dst = CD_BT if oc == 4 else CD_CT
                                  op("act", lambda e: e.activation(out=cd[:, dst:dst + 128], in_=ca[:, :], func=AF.Silu, bias=convb[:, oc:oc + 1]), reads=[b_ca, b_small], writes=[b_cd])
                          p2, pb2 = psr.get()
                          pbf = p2[:, 0:320].bitcast(BF16)
                          fw.group("pe", [(lambda e, oc=oc: e.transpose(pbf[:, oc * 128:(oc + 1) * 128], xact[:, oc, :], ident_b)) for oc in range(4)]
                                   + [lambda e: e.transpose(pbf[:, 512:640], cd[:, CD_BT:CD_BT + 128], ident_b)],
                                   reads=[b_xa, b_cd, b_cb], writes=[pb2])
                          op("dve", lambda e: e.tensor_copy(cd[:, CD_XS:CD_XS + 512], pbf[:, 0:512]), reads=[pb2], writes=[b_cd])
                          op("dve", lambda e: e.tensor_copy(cd[:, CD_BTOK:CD_BTOK + 128], pbf[:, 512:640]), reads=[pb2], writes=[b_cd])
                          for d in range(2):
                              xo = CD_XF if d == 0 else CD_XB
                              op("dve" if d == 0 else "pool", lambda e: e.tensor_tensor(out=cd[:, xo:xo + 512].rearrange("p (h n) -> p h n", h=8),
                                                                                       in0=cd[:, CD_XS:CD_XS + 512].rearrange("p (h n) -> p h n", h=8),
                                                                                       in1=dtt[:, d * 8:d * 8 + 8].unsqueeze(2).to_broadcast([128, 8, 64]), op=ALU.mult),
                                 reads=[b_cd, b_dt], writes=[b_cd])
                          if c == 2:
                              for d in range(2):
                                  op("dve", lambda e: e.tensor_copy(S_ctx[d][:, :, :], S_run[d][:, :, :]), reads=[b_S[d]], writes=[b_Sctx[d]])
                                  op("pool", lambda e: e.memset(S_run[d][:, :, :], 0.0), writes=[b_S[d]])
                              op("pool", lambda e: e.memset(Lacc[:, :], 0.0), writes=[b_Lacc])
                          for d in range(2):
                              scl, b_scl = small_stats(c, d)
                              if d == 1:
                                  op("act", lambda e: e.activation(out=eL[:, :], in_=Lacc[:, 12:24], func=AF.Exp), reads=[b_Lacc], writes=[b_eL])
                              slots1 = []
                              for m in range(6):
                                  p_, pb_ = psr.get()
                                  slots1.append((p_[:, 0:128], pb_))
                              st_out = states_batch(cd, b_cd, scl, b_scl, d, slots1)
                              for m in range(6):
                                  pt, pb = st_out[m]
                                  if d == 0:
                                      upd_state(S_run[0], b_S[0], scl, b_scl, 24, m, pt, pb)
                                  else:
                                      for half in range(2):
                                          r0, r1 = half * 64, half * 64 + 64
                                          u = 2 * m + half
                                          op("dve", lambda e: e.scalar_tensor_tensor(out=S_run[1][r0:r1, m, r0:r1], in0=pt[r0:r1, r0:r1], scalar=eL[r0:r1, u:u + 1],
                                                                                      op0=ALU.mult, in1=S_run[1][r0:r1, m, r0:r1], op1=ALU.add),
                                             reads=[pb, b_eL, b_S[1]], writes=[b_S[1]])
                              op("dve", lambda e: e.tensor_tensor(out=Lacc[:, d * 12:(d + 1) * 12], in0=Lacc[:, d * 12:(d + 1) * 12], in1=scl[:, 48:60], op=ALU.add),
                                 reads=[b_Lacc, b_scl], writes=[b_Lacc])
                          fw.dma("sp", cd_dram[c], cd[:, :], reads=[b_cd], writes=[b_cdd[c]], chanbuf=b_cd)
                          if L == 0 and c in (0, 2):
                              dump("cd%d" % c, cd[:, :], [b_cd])

                      if L == 0:
                          dump("la", la_all[:, :, :].rearrange("p c n -> p (c n)"), b_la)
                          dump("xbcB", xbc_dram[:, 0, 0:260], b_xbc)
                          dump("sagg0", S_run[0][:, :, :].rearrange("p c n -> p (c n)"), [b_S[0]])
                          dump("sagg1", S_run[1][:, :, :].rearrange("p c n -> p (c n)"), [b_S[1]])
                          dump("sctx0", S_ctx[0][:, :, :].rearrange("p c n -> p (c n)"), [b_Sctx[0]])
                          dump("sctx1", S_ctx[1][:, :, :].rearrange("p c n -> p (c n)"), [b_Sctx[1]])
                          stop_at("sw1")
                      b_ss = Buf(); b_sa = Buf()
                      for d in range(2):
                          fw.dma("sp", st_send[:, d * 768:(d + 1) * 768], S_run[d][:, :, :].rearrange("p m n -> p (m n)"), reads=[b_S[d]], writes=[b_ss])
                      fw.dma("sp", st_send[:, 1536:1560], Lacc[:, :], reads=[b_Lacc], writes=[b_ss])
                      fw.cc("AllGather", st_send.ap().opt(), st_all.ap().opt(), GROUPS, [b_ss], [b_sa], b_sa)
                      S0 = S_ctx
                      for d in range(2):
                          order = range(4) if d == 0 else range(3, -1, -1)
                          for r in order:
                              mcol = (2 + r) if d == 0 else (6 + r)
                              fw.dma("sp", sr_t[:, :], st_all[r * 128:(r + 1) * 128, :], reads=[b_sa], writes=[b_sr])
                              op("act", lambda e: e.activation(out=dm_t[:, :], in_=sr_t[:, 1536 + d * 12:1548 + d * 12], func=AF.Exp), reads=[b_sr], writes=[b_dm])
                              op("dve", lambda e: e.tensor_scalar(out=dm_t[:, :], in0=dm_t[:, :], scalar1=-1.0, scalar2=sel[:, mcol:mcol + 1], op0=ALU.add, op1=ALU.mult),
                                 reads=[b_dm, b_sel], writes=[b_dm])
                              op("dve", lambda e: e.tensor_scalar(out=dm_t[:, :], in0=dm_t[:, :], scalar1=1.0, scalar2=None, op0=ALU.add), reads=[b_dm], writes=[b_dm])
                              op("dve", lambda e: e.tensor_scalar(out=sr_t[:, d * 768:(d + 1) * 768], in0=sr_t[:, d * 768:(d + 1) * 768], scalar1=sel[:, mcol:mcol + 1], scalar2=None, op0=ALU.mult),
                                 reads=[b_sr, b_sel], writes=[b_sr])
                              for m in range(6):
                                  upd_state(S0[d], b_Sctx[d], dm_t, b_dm, 0, m, sr_t[:, d * 768 + m * 128:d * 768 + (m + 1) * 128], b_sr)

                      SCS = [(PS[4][:, 256:384], Buf()), (PS[4][:, 384:512], Buf()), (PS[6][:, 256:384], Buf()), (PS[6][:, 384:512], Buf()),
                             (PS[0][:, 256:384], Buf()), (PS[1][:, 256:384], Buf())]

                      def load_cd(c):
                          cd, b_cd = cd_r.get()
                          fw.dma("sp", cd[:, :], cd_dram[c], reads=[b_cdd[c]], writes=[b_cd])
                          return cd, b_cd

                      def y_step(c, d, pre, c_next):
                          cd, b_cd = pre
                          nxt = load_cd(c_next) if c_next is not None else None
                          scl, b_scl = small_stats(c, d)
                          la_c = la_all[:, c, :]
                          yA = (PS[6], PS[7]); yAb = (PSB[6], PSB[7])
                          yB = (PS[4], PS[5]); yBb = (PSB[4], PSB[5])

                          def ycols(ps2, u0, n):
                              c0 = u0 * 64
                              if c0 < 256:
                                  return ps2[0][:, c0:c0 + n]
                              return ps2[1][:, c0 - 256:c0 - 256 + n]
                          qk_cache = {}
                          pendA = []

                          def emitA(u_, scT_, b_scT_):
                              m_, half_ = u_ // 2, u_ % 2
                              _, v_pair = kv_pair(cd, m_, d)
                              yb_ = yAb[0] if u_ < 4 else yAb[1]
                              op("pe", lambda e: e.matmul(ycols(yA, u_, 64), scT_[:, :], v_pair[:, half_ * 64:half_ * 64 + 64], start=True, stop=True), reads=[b_scT_, b_cd], writes=[yb_])
                          for u in range(12):
                              m, half = u // 2, u % 2
                              r0, r1 = half * 64, half * 64 + 64
                              pm, pbm = Ring(list(zip(PS[0:4], PSB[0:4]))).items[u % 2]
                              op("pe", lambda e: e.matmul(pm[:, 0:128], la_c[:, d * 12 + u:d * 12 + u + 1].to_broadcast([128, 128]), tri_d[d], start=True, stop=True),
                                 reads=[b_la[c], b_cf], writes=[pbm])
                              dec, b_dec = dec_r.get()
                              op("act", lambda e: e.activation(out=dec[:, :], in_=pm[:, 0:128], func=AF.Exp, bias=scl[:, u:u + 1]), reads=[pbm, b_scl], writes=[b_dec])
                              qkey = u if u < 4 else 4 + half
                              if qkey not in qk_cache:
                                  pq, pbq = (PS[2], PSB[2]) if (len(qk_cache) % 2 == 0) else (PS[3], PSB[3])
                                  if u < 4:
                                      k_ap = cd[r0:r1, CD_KT + m * 128:CD_KT + (m + 1) * 128]
                                      q_ap = cd[r0:r1, CD_QT + m * 128:CD_QT + (m + 1) * 128]
                                  else:
                                      k_ap = cd[r0:r1, CD_BT:CD_BT + 128]
                                      q_ap = cd[r0:r1, CD_CT:CD_CT + 128]
                                  op("pe", lambda e: e.matmul(pq[:, 0:128], k_ap, q_ap, start=True, stop=True), reads=[b_cd], writes=[pbq])
                                  qkm, b_qkm = qkm_r.get()
                                  msk = triF if d == 0 else (triBs if u < 4 else triB)
                                  op("dve", lambda e: e.tensor_tensor(out=qkm[:, :], in0=pq[:, 0:128], in1=msk, op=ALU.mult), reads=[pbq, b_cf], writes=[b_qkm])
                                  qk_cache[qkey] = (qkm, b_qkm)
                              qkm, b_qkm = qk_cache[qkey]
                              scT, b_scT = scT_r.get()
                              op("dve", lambda e: e.scalar_tensor_tensor(out=scT[:, :], in0=dec[:, :], scalar=1.0, op0=ALU.min, in1=qkm[:, :], op1=ALU.mult),
                                 reads=[b_dec, b_qkm], writes=[b_scT])
                              pendA.append((u, scT, b_scT))
                              if len(pendA) > 2:
                                  emitA(*pendA.pop(0))
                          while pendA:
                              emitA(*pendA.pop(0))
                          for m in range(6):
                              q_ap = cd[:, CD_QT + m * 128:CD_QT + (m + 1) * 128] if m < 2 else cd[:, CD_CT:CD_CT + 128]
                              yb_ = yBb[0] if m < 2 else yBb[1]
                              op("pe", lambda e: e.matmul(ycols(yB, 2 * m, 128), q_ap, Sbd[d][:, m, :], start=True, stop=True), reads=[b_cd, b_Sbd[d]], writes=[yb_])
                          st_out = states_batch(cd, b_cd, scl, b_scl, d, SCS)
                          for m in range(6):
                              pt, pb = st_out[m]
                              upd_state(S_run[d], b_S[d], scl, b_scl, 24, m, pt, pb)
                          op("act", lambda e: e.activation(out=Sbd[d][:, :, :], in_=S_run[d][:, :, :], func=AF.Copy), reads=[b_S[d]], writes=[b_Sbd[d]])
                          yAs, b_yAs = yA_r.get()
                          yts, b_yts = yt_r.get()
                          op("act", lambda e: e.activation(out=yAs[:, 0:256], in_=yA[0][:, 0:256], func=AF.Copy), reads=[yAb[0]], writes=[b_yAs])
                          op("act", lambda e: e.activation(out=yAs[:, 256:768], in_=yA[1][:, 0:512], func=AF.Copy), reads=[yAb[1]], writes=[b_yAs])
                          op("dve", lambda e: e.tensor_tensor(out=yts[:, 0:256].rearrange("p (h n) -> p h n", h=4), in0=yB[0][:, 0:256].rearrange("p (h n) -> p h n", h=4),
                                                               in1=scl[:, 12:16].unsqueeze(2).to_broadcast([128, 4, 64]), op=ALU.mult), reads=[yBb[0], b_scl], writes=[b_yts])
                          op("dve", lambda e: e.tensor_tensor(out=yts[:, 256:768].rearrange("p (h n) -> p h n", h=8), in0=yB[1][:, 0:512].rearrange("p (h n) -> p h n", h=8),
                                                               in1=scl[:, 16:24].unsqueeze(2).to_broadcast([128, 8, 64]), op=ALU.mult), reads=[yBb[1], b_scl], writes=[b_yts])
                          op("pool", lambda e: e.tensor_tensor(out=yts[:, :], in0=yts[:, :], in1=yAs[:, :], op=ALU.add), reads=[b_yts, b_yAs], writes=[b_yts])
                          return cd, b_cd, yts, b_yts, nxt

                      psr_full = psr
                      psr = Ring(list(zip(PS[0:2], PSB[0:2])))
                      for d in range(2):
                          seqs = [[0, 1], list(range(2, NCH))] if d == 0 else [[1, 0], list(range(NCH - 1, 1, -1))]
                          flat = seqs[0] + seqs[1]
                          pre = load_cd(flat[0])
                          for si, seq in enumerate(seqs):
                              if si == 0:
                                  op("pool", lambda e: e.memset(S_run[d][:, :, :], 0.0), writes=[b_S[d]])
                              else:
                                  op("dve", lambda e: e.tensor_copy(S_run[d][:, :, :], S0[d][:, :, :]), reads=[b_Sctx[d]], writes=[b_S[d]])
                              op("act", lambda e: e.activation(out=Sbd[d][:, :, :], in_=S_run[d][:, :, :], func=AF.Copy), reads=[b_S[d]], writes=[b_Sbd[d]])
                              for c in seq:
                                  fi = flat.index(c)
                                  if d == 1 and not (last and c < 2):
                                      yfs, b_yfs = yf_r.get()
                                      fw.dma("sp", yfs[:, :], yf_dram[c], reads=[b_yfd[c]], writes=[b_yfs])
                                  cd, b_cd, yts, b_yts, pre = y_step(c, d, pre, flat[fi + 1] if fi + 1 < len(flat) else None)
                                  if d == 0:
                                      fw.dma("pool", yf_dram[c], yts[:, :], reads=[b_yts], writes=[b_yfd[c]], chanbuf=b_yts)
                                      continue
                                  if last and c < 2:
                                      continue
                                  op("pool", lambda e: e.tensor_tensor(out=yts[:, :], in0=yts[:, :], in1=yfs[:, :], op=ALU.add), reads=[b_yts, b_yfs], writes=[b_yts])
                                  gt, b_gt = gt_r.get()
                                  gst, b_gst = gst_r.get()
                                  mo, b_mo = mo_r.get()
                                  yr = yts[:, 0:256].rearrange("p (h n) -> p h n", h=4)
                                  gr = gt[:, 0:256].rearrange("p (h n) -> p h n", h=4)
                                  op("dve", lambda e: e.tensor_reduce(out=gst[:, 0:4], in_=yr, op=ALU.add, axis=AX.X), reads=[b_yts], writes=[b_gst])
                                  op("act", lambda e: e.activation(out=gt[:, 0:256], in_=yts[:, 0:256], func=AF.Square), reads=[b_yts], writes=[b_gt])
                                  op("dve", lambda e: e.tensor_reduce(out=gst[:, 4:8], in_=gr, op=ALU.add, axis=AX.X), reads=[b_gt], writes=[b_gst])
                                  op("dve", lambda e: e.tensor_scalar(out=gst[:, 0:8], in0=gst[:, 0:8], scalar1=1.0 / 64, scalar2=None, op0=ALU.mult), reads=[b_gst], writes=[b_gst])
                                  op("dve", lambda e: e.tensor_tensor(out=gst[:, 8:12], in0=gst[:, 0:4], in1=gst[:, 0:4], op=ALU.mult), reads=[b_gst], writes=[b_gst])
                                  op("dve", lambda e: e.tensor_tensor(out=gst[:, 8:12], in0=gst[:, 4:8], in1=gst[:, 8:12], op=ALU.subtract), reads=[b_gst], writes=[b_gst])
                                  op("act", lambda e: e.activation(out=gst[:, 8:12], in_=gst[:, 8:12], func=AF.Sqrt, bias=EPS), reads=[b_gst], writes=[b_gst])
                                  op("dve", lambda e: e.reciprocal(out=gst[:, 8:12], in_=gst[:, 8:12]), reads=[b_gst], writes=[b_gst])
                                  op("dve", lambda e: e.tensor_tensor(out=gr, in0=yr, in1=gst[:, 0:4].unsqueeze(2).to_broadcast([128, 4, 64]), op=ALU.subtract), reads=[b_yts, b_gst], writes=[b_gt])
                                  op("dve", lambda e: e.tensor_tensor(out=gr, in0=gr, in1=gst[:, 8:12].unsqueeze(2).to_broadcast([128, 4, 64]), op=ALU.mult), reads=[b_gt, b_gst], writes=[b_gt])
                                  op("dve", lambda e: e.tensor_tensor(out=mo[:, 0:256], in0=gt[:, 0:256], in1=cd[:, CD_SG:CD_SG + 256], op=ALU.mult), reads=[b_gt, b_cd], writes=[b_mo])
                                  op("pool", lambda e: e.tensor_tensor(out=gt[:, 256:768], in0=cd[:, CD_XS:CD_XS + 512], in1=dskip_b, op=ALU.mult), reads=[b_cd, b_small], writes=[b_gt])
                                  op("pool", lambda e: e.tensor_tensor(out=gt[:, 256:768], in0=gt[:, 256:768], in1=yts[:, 256:768], op=ALU.add), reads=[b_gt, b_yts], writes=[b_gt])
                                  op("dve", lambda e: e.tensor_tensor(out=gt[:, 256:768], in0=gt[:, 256:768], in1=cd[:, CD_SZ:CD_SZ + 512], op=ALU.mult), reads=[b_gt, b_cd], writes=[b_gt])
                                  op("act", lambda e: e.activation(out=yts[:, 256:768], in_=gt[:, 256:768], func=AF.Square, accum_out=gst[:, 16:17]), reads=[b_gt], writes=[b_yts, b_gst])
                                  op("act", lambda e: e.activation(out=gst[:, 17:18], in_=gst[:, 16:17], func=AF.Sqrt, scale=1.0 / 512, bias=EPS), reads=[b_gst], writes=[b_gst])
                                  op("dve", lambda e: e.reciprocal(out=gst[:, 17:18], in_=gst[:, 17:18]), reads=[b_gst], writes=[b_gst])
                                  op("dve", lambda e: e.scalar_tensor_tensor(out=mo[:, 256:768], in0=gt[:, 256:768], scalar=gst[:, 17:18], op0=ALU.mult, in1=snw_b, op1=ALU.mult),
                                     reads=[b_gt, b_gst, b_small], writes=[b_mo])
                                  for half3 in range(2):
                                      p2, pb2 = psr.get()
                                      pbf = p2[:, 0:192].bitcast(BF16)
                                      fw.group("pe", [(lambda e, i=i: e.transpose(pbf[:, i * 128:(i + 1) * 128], mo[:, (half3 * 3 + i) * 128:(half3 * 3 + i + 1) * 128], ident_b)) for i in range(3)],
                                               reads=[b_mo, b_cb], writes=[pb2])
                                      eng = "dve"
                                      if eng == "act":
                                          op("act", lambda e: e.activation(out=mixT[:, 2 + half3 * 3:5 + half3 * 3, c * 128:(c + 1) * 128], in_=pbf[:, 0:384].rearrange("p (c t) -> p c t", c=3), func=AF.Copy),
                                             reads=[pb2], writes=[b_mix[c]])
                                      else:
                                          op("dve", lambda e: e.tensor_copy(mixT[:, 2 + half3 * 3:5 + half3 * 3, c * 128:(c + 1) * 128], pbf[:, 0:384].rearrange("p (c t) -> p c t", c=3)),
                                             reads=[pb2], writes=[b_mix[c]])
                      psr = psr_full
              if L == 0:
                  dump("mix", mixT[:, :, :].rearrange("p k t -> p (k t)"), b_mix)
                  stop_at("scan")
              fw.barrier()
              blks = [(bi, t0, W, v) for bi, (t0, W, v) in enumerate(BLOCKS) if not (last and v == 1)]
              with ExitStack() as s3:
                  xb3 = sb(s3, "xb3", [128, 8, 512], F32); b_xb3 = Buf()
                  wo = sb(s3, "wo", [128, 8, D], BF16); b_wo = Buf()
                  fw.dma("pool", wo[:, :, :], w_out[L].rearrange("(k p) n -> p k n", p=128), writes=[b_wo])
                  wk = {"sq": Ring([(sb(s3, "sq3_%d" % i, [128, 8, 512], BF16), Buf()) for i in range(1)]),
                        "rs": Ring([(sb(s3, "rs3_%d" % i, [128, 512], F32), Buf()) for i in range(2)]),
                        "tmp": Ring([(sb(s3, "tmp3_%d" % i, [128, 512], F32), Buf()) for i in range(2)])}
                  for bi, t0, W, v in blks:
                      fw.dma("sp", xb3[:, :, 0:W], x_src[:, t0:t0 + W].rearrange("(k p) t -> p k t", p=128), writes=[b_xb3])
                      mb = [b_mix[c] for c in range(t0 // 128, (t0 + W) // 128)]
                      for oc in range(8):
                          pt, pb = psr.get()
                          fw.group("pe", [(lambda e, k=k: e.matmul(pt[:, 0:W], wo[:, k, oc * 128:(oc + 1) * 128], mixT[:, k, t0:t0 + W], start=(k == 0), stop=(k == 7))) for k in range(8)],
                                   reads=[b_wo] + mb, writes=[pb])
                          op("dve", lambda e: e.scalar_tensor_tensor(out=xb3[:, oc, 0:W], in0=pt[:, 0:W], scalar=mv[:, 16 + oc, v:v + 1], op0=ALU.mult, in1=xb3[:, oc, 0:W], op1=ALU.add),
                             reads=[pb, b_mv, b_xb3], writes=[b_xb3])
                      norm_mod(xb3[:, :, 0:W], b_xb3, t0, W, v, 8, 24, wk, b_hx[bi])
                      fw.dma("sp", xres[:, t0:t0 + W].rearrange("(k p) t -> p k t", p=128), xb3[:, :, 0:W], reads=[b_xb3], writes=[b_xres], chanbuf=b_xb3)
              if L == 0:
                  dump("xmid", xres, [b_xres])
                  dump("hx2", hxT[:, :, :].rearrange("p k t -> p (k t)"), b_hx)
                  stop_at("s3a")
              lm.close()
              fw.barrier()
              with ExitStack() as s3:
                  xr = sb(s3, "xr", [128, 8, NT], F32); b_xr = [Buf() for _ in BLOCKS]
                  wk = {"sq": Ring([(sb(s3, "sq4_%d" % i, [128, 8, 512], BF16), Buf()) for i in range(1)]),
                        "rs": Ring([(sb(s3, "rs4_%d" % i, [128, 512], F32), Buf()) for i in range(1)])}
                  for bi, t0, W, v in blks:
                      fw.dma("sp", xr[:, :, t0:t0 + W], xres[:, t0:t0 + W].rearrange("(k p) t -> p k t", p=128), reads=[b_xres], writes=[b_xr[bi]])
                  is_moe = (L % 2 == 1)
                  if not is_moe:
                      units = [(None, f0, min(4, 22 - f0)) for f0 in range(0, 22, 4)]
                      wg_src, wu_src, wd_src = ffn_wg, ffn_wu, ffn_wd
                  else:
                      units = [(e_, f0, min(4, 11 - f0)) for e_ in range(8) for f0 in (0, 4, 8)]
                  wgu_r = Ring([(sb(s3, "wgu%d" % i, [128, 2, 8, 512], BF16), Buf()) for i in range(2)])
                  wd_r = Ring([(sb(s3, "wdn%d" % i, [128, 4, D], BF16), Buf()) for i in range(2)])
                  hid = sb(s3, "hid", [128, 4, 512], BF16); b_hid = Buf()
                  sg_r = Ring([(sb(s3, "sg%d" % i, [128, 512], BF16), Buf()) for i in range(2)])
                  ug_r = Ring([(sb(s3, "ug%d" % i, [128, 512], BF16), Buf()) for i in range(2)])
                  Gt = None
                  if is_moe:
                      rt = sb(s3, "rt", [128, 8, 8], BF16); b_rt = Buf()
                      fw.dma("pool", rt[:, :, :], router_in.rearrange("(k p) n -> p k n", p=128), writes=[b_rt])
                      gates = sb(s3, "gates", [128, NCH, 8], F32); b_gates = Buf()
                      G = sb(s3, "G", [128, 512], F32); b_G = Buf()
                      gb_r = Ring([(sb(s3, "gb%d" % i, [128, 128], F32), Buf()) for i in range(2)])
                      gs_r = Ring([(sb(s3, "gs%d" % i, [128, 32], F32), Buf()) for i in range(2)])
                      for bi, t0, W, v in blks:
                          for ti in range(W // 128):
                              c = (t0 // 128) + ti
                              tt = c * 128
                              pt, pb = psr.get()
                              fw.group("pe", [(lambda e, k=k: e.matmul(pt[:, 0:8], hxT[:, k, tt:tt + 128], rt[:, k, :], start=(k == 0), stop=(k == 7))) for k in range(8)],
                                       reads=[b_rt, b_hx[bi]], writes=[pb])
                              gs, b_gs = gs_r.get()
                              op("dve", lambda e: e.tensor_copy(gs[:, 0:8], pt[:, 0:8]), reads=[pb], writes=[b_gs])
                              op("dve", lambda e: e.max(out=gs[:, 8:16], in_=gs[:, 0:8]), reads=[b_gs], writes=[b_gs])
                              op("dve", lambda e: e.tensor_scalar(out=gs[:, 16:17], in0=gs[:, 8:9], scalar1=-1.0, scalar2=None, op0=ALU.mult), reads=[b_gs], writes=[b_gs])
                              op("act", lambda e: e.activation(out=gs[:, 24:32], in_=gs[:, 0:8], func=AF.Exp, bias=gs[:, 16:17]), reads=[b_gs], writes=[b_gs])
                              op("dve", lambda e: e.scalar_tensor_tensor(out=gs[:, 24:32], in0=gs[:, 0:8], scalar=gs[:, 9:10], op0=ALU.is_ge, in1=gs[:, 24:32], op1=ALU.mult), reads=[b_gs], writes=[b_gs])
                              op("dve", lambda e: e.tensor_reduce(out=gs[:, 17:18], in_=gs[:, 24:32], op=ALU.add, axis=AX.X), reads=[b_gs], writes=[b_gs])
                              op("dve", lambda e: e.reciprocal(out=gs[:, 17:18], in_=gs[:, 17:18]), reads=[b_gs], writes=[b_gs])
                              op("dve", lambda e: e.tensor_scalar(out=gates[:, c, :], in0=gs[:, 24:32], scalar1=gs[:, 17:18], scalar2=None, op0=ALU.mult), reads=[b_gs], writes=[b_gates])
                  for (e_, f0, F) in units:
                      wgu, b_wgu = wgu_r.get()
                      wdn, b_wdn = wd_r.get()
                      if e_ is None:
                          gsrc = ffn_wg[:, f0 * 128:(f0 + F) * 128]; usrc = ffn_wu[:, f0 * 128:(f0 + F) * 128]; dsrc = ffn_wd[f0 * 128:(f0 + F) * 128, :]
                      else:
                          gsrc = moe_wg[e_, :, f0 * 128:(f0 + F) * 128]; usrc = moe_wu[e_, :, f0 * 128:(f0 + F) * 128]; dsrc = moe_wd[e_, f0 * 128:(f0 + F) * 128, :]
                      fw.dma("pool", wgu[:, 0, :, 0:F * 128], gsrc.rearrange("(k p) n -> p k n", p=128), writes=[b_wgu])
                      fw.dma("pool", wgu[:, 1, :, 0:F * 128], usrc.rearrange("(k p) n -> p k n", p=128), writes=[b_wgu])
                      fw.dma("pool", wdn[:, 0:F, :], dsrc.rearrange("(f p) n -> p f n", p=128), writes=[b_wdn])
                      for bi, t0, W, v in blks:
                          if e_ is not None:
                              pt, pb = psr.get()
                              for ti in range(W // 128):
                                  c = (t0 // 128) + ti
                                  gb, b_gb = gb_r.get()
                                  op("dve", lambda e: e.tensor_scalar(out=gb[:, :], in0=ones_f, scalar1=gates[:, c, e_:e_ + 1], scalar2=None, op0=ALU.mult), reads=[b_cf, b_gates], writes=[b_gb])
                                  op("pe", lambda e: e.matmul(pt[:, ti * 128:(ti + 1) * 128], gb[:, :], ident_f, start=True, stop=True), reads=[b_gb, b_cf], writes=[pb])
                              op("act", lambda e: e.activation(out=G[:, 0:W], in_=pt[:, 0:W], func=AF.Copy), reads=[pb], writes=[b_G])
                          for f in range(F):
                              pg, pbg = psr.get()
                              pu, pbu = psr.get()
                              fw.group("pe", [(lambda e, k=k: e.matmul(pg[:, 0:W], wgu[:, 0, k, f * 128:(f + 1) * 128], hxT[:, k, t0:t0 + W], start=(k == 0), stop=(k == 7))) for k in range(8)],
                                       reads=[b_wgu, b_hx[bi]], writes=[pbg])
                              fw.group("pe", [(lambda e, k=k: e.matmul(pu[:, 0:W], wgu[:, 1, k, f * 128:(f + 1) * 128], hxT[:, k, t0:t0 + W], start=(k == 0), stop=(k == 7))) for k in range(8)],
                                       reads=[b_wgu, b_hx[bi]], writes=[pbu])
                              sg, b_sg = sg_r.get()
                              op("act", lambda e: e.activation(out=sg[:, 0:W], in_=pg[:, 0:W], func=AF.Silu), reads=[pbg], writes=[b_sg])
                              if e_ is None:
                                  op("dve", lambda e: e.tensor_tensor(out=hid[:, f, 0:W], in0=pu[:, 0:W], in1=sg[:, 0:W], op=ALU.mult), reads=[pbu, b_sg], writes=[b_hid])
                              else:
                                  ug, b_ug = ug_r.get()
                                  op("dve", lambda e: e.tensor_tensor(out=ug[:, 0:W], in0=pu[:, 0:W], in1=G[:, 0:W], op=ALU.mult), reads=[pbu, b_G], writes=[b_ug])
                                  op("pool", lambda e: e.tensor_tensor(out=hid[:, f, 0:W], in0=ug[:, 0:W], in1=sg[:, 0:W], op=ALU.mult), reads=[b_ug, b_sg], writes=[b_hid])
                          for oc in range(8):
                              pt, pb = psr.get()
                              fw.group("pe", [(lambda e, f=f: e.matmul(pt[:, 0:W], wdn[:, f, oc * 128:(oc + 1) * 128], hid[:, f, 0:W], start=(f == 0), stop=(f == F - 1))) for f in range(F)],
                                       reads=[b_wdn, b_hid], writes=[pb])
                              op("dve", lambda e: e.scalar_tensor_tensor(out=xr[:, oc, t0:t0 + W], in0=pt[:, 0:W], scalar=mv[:, 40 + oc, v:v + 1], op0=ALU.mult, in1=xr[:, oc, t0:t0 + W], op1=ALU.add),
                                 reads=[pb, b_mv, b_xr[bi]], writes=[b_xr[bi]])
                  b_out = Buf()
                  if not last:
                      for bi, t0, W, v in blks:
                          fw.dma("sp", xres[:, t0:t0 + W].rearrange("(k p) t -> p k t", p=128), xr[:, :, t0:t0 + W], reads=[b_xr[bi]], writes=[b_xres], chanbuf=b_xr[bi])
                      fw.finish([b_xres], eng="sp")
                  else:
                      for bi, t0, W, v in blks:
                          sq, b_sq = wk["sq"].get()
                          op("act", lambda e: e.activation(out=sq[:, :, 0:W], in_=xr[:, :, t0:t0 + W], func=AF.Square), reads=[b_xr[bi]], writes=[b_sq])
                          pt, pb = psr.get()
                          fw.group("pe", [(lambda e, k=k: e.matmul(pt[:, 0:W], ones_b, sq[:, k, 0:W], start=(k == 0), stop=(k == 7))) for k in range(8)], reads=[b_sq, b_cb], writes=[pb])
                          rs, b_rs = wk["rs"].get()
                          op("act", lambda e: e.activation(out=rs[:, 0:W], in_=pt[:, 0:W], func=AF.Sqrt, scale=1.0 / D, bias=EPS), reads=[pb], writes=[b_rs])
                          op("dve", lambda e: e.reciprocal(out=rs[:, 0:W], in_=rs[:, 0:W]), reads=[b_rs], writes=[b_rs])
                          for k in range(8):
                              op("dve", lambda e: e.scalar_tensor_tensor(out=xr[:, k, t0:t0 + W], in0=xr[:, k, t0:t0 + W], scalar=fnw[:, k:k + 1], op0=ALU.mult, in1=rs[:, 0:W], op1=ALU.mult),
                                 reads=[b_xr[bi], b_fnw, b_rs], writes=[b_xr[bi]])
                          fw.dma("sp", outT[:, t0 - TC:t0 - TC + W].rearrange("(k p) t -> p k t", p=128), xr[:, :, t0:t0 + W], reads=[b_xr[bi]], writes=[b_out], chanbuf=b_xr[bi])
                      fw.finish([b_out], eng="sp")
        except _Stop:
            stopped = True
        fw.finish(dbg_bufs, eng="sp")
        print("instructions:", fw.ninstr, "dma channels:", fw.nchan)
        if stopped:
            raise _StopOuter()
    return nc


def _perm_heads(a, axis):
    a = np.moveaxis(a, axis, -1)
    sh = a.shape
    a = a.reshape(sh[:-1] + (8, sh[-1] // 8))[..., PI8, :].reshape(sh)
    return np.ascontiguousarray(np.moveaxis(a, -1, axis))


def _host_inputs(inp):
    f32 = np.float32
    g = {k: np.asarray(v) for k, v in inp.items()}
    x, c, ctx, c_ctx = g["x"], g["c"], g["ctx"], g["c_ctx"]
    w_in = g["w_in"].copy()
    w_in[:, :, 1440:1952] = _perm_heads(g["w_in"][:, :, 1440:1952], 2)
    w_in[:, :, 1952:2464] = _perm_heads(g["w_in"][:, :, 1952:2464], 2)
    w_in[:, :, 2720:2728] = g["w_in"][:, :, 2720:2728][:, :, PI8]
    w_in[:, :, 2728:2736] = g["w_in"][:, :, 2728:2736][:, :, PI8]
    w_out = g["w_out"].copy()
    w_out[:, 512:1024, :] = _perm_heads(g["w_out"][:, 512:1024, :], 1)
    perm = np.concatenate([np.arange(8, 16), np.arange(0, 8), np.arange(24, 32), np.arange(16, 24)])
    w_qb = g["mla_w_qb"]
    w_qbrot = np.zeros_like(w_qb)
    for h in range(4):
        w_qbrot[:, :, h * 96 + 64:h * 96 + 96] = w_qb[:, :, h * 96 + 64 + perm]
    conv_w = g["ssd_conv_w"].copy()
    conv_w[:, :, 0:512] = _perm_heads(g["ssd_conv_w"][:, :, 0:512], 2)
    conv_b = g["ssd_conv_b"].copy()
    conv_b[:, 0:512] = _perm_heads(g["ssd_conv_b"][:, 0:512], 1)
    bc = lambda v: np.ascontiguousarray(np.broadcast_to(v[:, None, :], (v.shape[0], 128, v.shape[1]))).astype(f32)
    fm = lambda v: np.ascontiguousarray(v.reshape(-1, 128).T)
    nw = np.stack([np.concatenate([fm(g["norm1_w"][l]), fm(g["norm2_w"][l])], axis=1) for l in range(2)]).astype(f32)
    convw = np.stack([np.stack([fm(conv_w[l, t]) for t in range(3)], axis=2).reshape(128, 18) for l in range(2)]).astype(f32)
    convb = np.stack([fm(conv_b[l]) for l in range(2)]).astype(f32)
    dtb = np.concatenate([g["ssd_dt_bias"][:, 0][:, PI8], g["ssd_dt_bias"][:, 1][:, PI8]], axis=1)
    alog = np.concatenate([g["ssd_a_log"][:, 0][:, PI8], g["ssd_a_log"][:, 1][:, PI8]], axis=1)
    rdl = np.concatenate([g["ret_decay_logit"][:, 0], g["ret_decay_logit"][:, 1]], axis=1)
    dskip = np.repeat(g["ssd_d"][:, PI8], 64, axis=1)
    snw = _perm_heads(g["ssd_norm_w"], 1)
    k = np.arange(128)
    cf = np.zeros((128, 6, 128), f32)
    cf[:, 0] = np.eye(128)
    cf[:, 1] = (k[:, None] <= k[None, :])
    cf[:, 2] = (k[:, None] >= k[None, :])
    cf[:, 3] = (k[:, None] > k[None, :])
    cf[:, 4] = 1.0
    cf[64, 5, 0:64] = 1.0
    cbf = np.zeros((128, 3, 128), f32)
    cbf[:, 0] = np.eye(128)
    cbf[:, 1] = 1.0
    for m in range(32):
        cbf[perm[m], 2, m] = 1.0
    cbf = cbf.astype(ml_dtypes.bfloat16)
    inv_freq = (10000.0 ** (-np.arange(0, 16, 2, dtype=f32) / 16)).astype(f32)
    sign = np.concatenate([-np.ones(8), np.ones(8), -np.ones(8), np.ones(8)]).astype(f32)
    shared = dict(mod_w=g["mod_w"], mod_b=g["mod_b"][:, None, :], w_in=w_in, w_out=w_out, w_qb=w_qb, w_qbrot=w_qbrot,
                  w_kb=g["mla_w_kb"], w_vb=g["mla_w_vb"], nw=nw, fnw=fm(g["final_norm_w"]).astype(f32),
                  qnw_b=bc(g["mla_q_norm_w"]), kvnw_b=bc(g["mla_kv_norm_w"]), convw=convw, convb=convb,
                  dtb_b=bc(dtb), alog_b=bc(alog), rdl_b=bc(rdl), dskip_b=bc(dskip), snw_b=bc(snw),
                  router=g["moe_router"][0], ffn_wg=g["ffn_w_gate"][0], ffn_wu=g["ffn_w_up"][0], ffn_wd=g["ffn_w_down"][0],
                  moe_wg=g["moe_w_gate"][0], moe_wu=g["moe_w_up"][0], moe_wd=g["moe_w_down"][0], cf32=cf, cbf16=cbf)
    shared = {k_: np.ascontiguousarray(v, dtype=(v.dtype if v.dtype == ml_dtypes.bfloat16 else f32)) for k_, v in shared.items()}
    maps = []
    for r in range(8):
        b, j = r // 4, r % 4
        xt = np.concatenate([ctx[b], x[b, j * TL:(j + 1) * TL]], axis=0)
        ccv = np.stack([fm(c[b]), fm(c_ctx)], axis=2).astype(f32)
        tg = (j * TL + np.arange(TL)).astype(f32)
        row = np.floor(tg / 64).astype(f32)
        col = (tg - row * 64).astype(f32)
        ang_r = row[:, None] * inv_freq
        ang_c = col[:, None] * inv_freq
        ang = np.concatenate([ang_r, ang_r, ang_c, ang_c], axis=1).astype(f32)
        cos32 = np.concatenate([np.ones((TC, 32), f32), np.cos(ang).astype(f32)], axis=0)
        sin32 = np.concatenate([np.zeros((TC, 32), f32), np.sin(ang).astype(f32) * sign], axis=0)
        ropeT = np.zeros((128, 2, NT), f32)
        for base in (0, 64):
            ropeT[base:base + 32, 0] = cos32.T
            ropeT[base:base + 32, 1] = sin32.T
        sel = np.zeros((128, 16), f32)
        if j > 0:
            sel[2 * (j - 1) + 1, 0] = 1.0
        if j < 3:
            sel[2 * (j + 1), 1] = 1.0
        for r2 in range(4):
            sel[:, 2 + r2] = 1.0 if r2 < j else 0.0
            sel[:, 6 + r2] = 1.0 if r2 > j else 0.0
        m = dict(shared)
        m.update(xT=np.ascontiguousarray(xt.T.astype(f32)), cc=np.ascontiguousarray(ccv), ropeT=ropeT, sel=sel)
        maps.append(m)
    return maps


_NC_CACHE = {}


def kernel(**inputs):
    maps = _host_inputs(inputs)
    if "nc" not in _NC_CACHE:
        _NC_CACHE["nc"] = build_program()
    res = run_bass_kernel_spmd(_NC_CACHE["nc"], maps, core_ids=list(range(8)))
    out = np.zeros((2, 4 * TL, D), np.float32)
    for r in range(8):
        b, j = r // 4, r % 4
        out[b, j * TL:(j + 1) * TL, :] = np.asarray(res.results[r]["outT"]).T
    return out
```

```python
import numpy as np
import ml_dtypes
from contextlib import ExitStack
import concourse.bass as bass
import concourse.mybir as mybir
from concourse.bass_utils import run_bass_kernel_spmd

F32 = mybir.dt.float32
BF16 = mybir.dt.bfloat16
AF = mybir.ActivationFunctionType
ALU = mybir.AluOpType
AX = mybir.AxisListType

D = 1024
TC = 256
TL = 2048
NT = TC + TL
NCH = NT // 128
BLOCKS = [(0, 256, 1), (256, 512, 0), (768, 512, 0), (1280, 512, 0), (1792, 512, 0)]
EPS = 1e-6
PI8 = [0, 4, 1, 5, 2, 6, 3, 7]
MLA_SCALE = 96 ** -0.5
CD_QT, CD_KT, CD_CT, CD_BT = 0, 256, 512, 640
CD_KTOK, CD_BTOK, CD_VTOK = 768, 1024, 1152
CD_XF, CD_XB, CD_XS, CD_SG, CD_SZ = 1408, 1920, 2432, 2944, 3200
CDW = 3712


STRICT = False


class Buf:
    __slots__ = ("w", "r", "chan")

    def __init__(self):
        self.w = None
        self.r = {}
        self.chan = None


class FW:
    def __init__(self, nc, es):
        self.nc = nc
        self.es = es
        self.E = {"pe": nc.tensor, "act": nc.scalar, "dve": nc.vector, "pool": nc.gpsimd, "sp": nc.sync}
        self.sems = {}
        self.cnt = {}
        self.waited = {e: {} for e in self.E}
        for e in self.E:
            self._newsem(e)
        self.nchan = 0
        self.ninstr = 0

    def _newsem(self, key):
        h = self.es.enter_context(self.nc.semaphore("s_%s" % (str(key).replace(" ", ""),)))
        self.sems[key] = h
        self.cnt[key] = 0
        return h

    def _wait(self, eng, key, val):
        if val <= 0:
            return
        w = self.waited[eng]
        if w.get(key, 0) >= val:
            return
        w[key] = val
        self.E[eng].wait_ge(self.sems[key], val)

    def deps(self, eng, reads, writes, is_dma=False):
        for b in reads:
            if b.w is not None:
                k, v = b.w
                if k != "pe" or eng != "pe" or is_dma:
                    self._wait(eng, k, v)
        for b in writes:
            if b.w is not None:
                k, v = b.w
                if k != eng or is_dma or (STRICT and eng != "pe"):
                    self._wait(eng, k, v)
            for k, v in b.r.items():
                if k != eng or is_dma or (STRICT and eng != "pe"):
                    self._wait(eng, k, v)

    def record(self, key, val, reads, writes):
        for b in reads:
            b.r[key] = val
        for b in writes:
            b.w = (key, val)
            b.r = {}

    def op(self, eng, fn, reads=(), writes=()):
        self.deps(eng, reads, writes)
        ins = fn(self.E[eng])
        self.cnt[eng] += 1
        ins.then_inc(self.sems[eng], 1)
        self.record(eng, self.cnt[eng], reads, writes)
        self.ninstr += 1
        return ins

    def group(self, eng, fns, reads=(), writes=()):
        self.deps(eng, reads, writes)
        ins = None
        for f in fns:
            ins = f(self.E[eng])
            self.ninstr += 1
        self.cnt[eng] += 1
        ins.then_inc(self.sems[eng], 1)
        self.record(eng, self.cnt[eng], reads, writes)

    def dma(self, q, out, in_, reads=(), writes=(), chanbuf=None, **kw):
        cb = chanbuf if chanbuf is not None else (writes[0] if writes else reads[0])
        if cb.chan is None:
            cb.chan = ("ch", self.nchan)
            self.nchan += 1
            self._newsem(cb.chan)
        ck = cb.chan
        self.deps(q, reads, writes, is_dma=True)
        self._wait(q, ck, self.cnt[ck])
        ins = self.E[q].dma_start(out=out, in_=in_, **kw)
        self.cnt[ck] += 16
        ins.then_inc(self.sems[ck], 16)
        self.record(ck, self.cnt[ck], reads, writes)
        self.ninstr += 1
        return ins

    def cc(self, kind, ins_ap, outs_ap, groups, reads, writes, chanbuf):
        if chanbuf.chan is None:
            chanbuf.chan = ("ch", self.nchan)
            self.nchan += 1
            self._newsem(chanbuf.chan)
        ck = chanbuf.chan
        self.deps("pool", reads, writes, is_dma=True)
        self._wait("pool", ck, self.cnt[ck])
        ins = self.nc.gpsimd.collective_compute(kind, ALU.bypass, replica_groups=groups, ins=[ins_ap], outs=[outs_ap])
        self.cnt[ck] += 1
        ins.then_inc(self.sems[ck], 1)
        self.record(ck, self.cnt[ck], reads, writes)

    def barrier(self):
        snap = dict(self.cnt)
        for eng in self.E:
            for key, val in snap.items():
                if key != eng:
                    self._wait(eng, key, val)

    def finish(self, bufs, eng="sp"):
        for b in bufs:
            if b.w is not None:
                self._wait(eng, *b.w)
            for k, v in b.r.items():
                self._wait(eng, k, v)


class Ring:
    def __init__(self, items):
        self.items = items
        self.i = 0

    def get(self):
        it = self.items[self.i % len(self.items)]
        self.i += 1
        return it


class _Stop(Exception):
    pass


class _StopOuter(Exception):
    pass


def build_program(n_layers=2, dbg=None, stop=None, groups=None):
    box = {}
    try:
        _build_program(box, n_layers, dbg, stop, groups)
    except _StopOuter:
        pass
    return box["nc"]


def _build_program(box, n_layers, dbg, stop, groups):
    nc = bass.Bass("TRN2", target_bir_lowering=False)
    box["nc"] = nc
    dbg = dbg or {}

    def din(name, shape, dt=F32):
        return nc.dram_tensor(name, list(shape), dt, kind="ExternalInput").ap()

    xT_in = din("xT", [D, NT])
    cc_in = din("cc", [128, 8, 2])
    mod_w = din("mod_w", [2, D, 6 * D])
    mod_b = din("mod_b", [2, 1, 6 * D])
    w_in = din("w_in", [2, D, 2736])
    w_out = din("w_out", [2, D, D])
    w_qb = din("w_qb", [2, 256, 384])
    w_qbrot = din("w_qbrot", [2, 256, 384])
    w_kb = din("w_kb", [2, 128, 256])
    w_vb = din("w_vb", [2, 128, 256])
    nw_in = din("nw", [2, 128, 16])
    fnw_in = din("fnw", [128, 8])
    qnw_in = din("qnw_b", [2, 128, 256])
    kvnw_in = din("kvnw_b", [2, 128, 128])
    convw_in = din("convw", [2, 128, 18])
    convb_in = din("convb", [2, 128, 6])
    dtb_in = din("dtb_b", [2, 128, 16])
    alog_in = din("alog_b", [2, 128, 16])
    rdl_in = din("rdl_b", [2, 128, 8])
    dskip_in = din("dskip_b", [2, 128, 512])
    snw_in = din("snw_b", [2, 128, 512])
    router_in = din("router", [D, 8])
    ffn_wg = din("ffn_wg", [D, 2816])
    ffn_wu = din("ffn_wu", [D, 2816])
    ffn_wd = din("ffn_wd", [2816, D])
    moe_wg = din("moe_wg", [8, D, 1408])
    moe_wu = din("moe_wu", [8, D, 1408])
    moe_wd = din("moe_wd", [8, 1408, D])
    cf_in = din("cf32", [128, 6, 128])
    cb_in = din("cbf16", [128, 5, 128], BF16)
    rope_in = din("ropeT", [128, 2, NT])
    sel_in = din("sel", [128, 16])
    outT = nc.dram_tensor("outT", [D, TL], F32, kind="ExternalOutput").ap()
    dbg_out = {k: nc.dram_tensor("dbg_" + k, list(s), F32, kind="ExternalOutput").ap() for k, s in dbg.items()}

    xres = nc.dram_tensor("xres", [D, NT], F32).ap()
    kv_send = nc.dram_tensor("kv_send", [162, NT], BF16)
    kv_all = nc.dram_tensor("kv_all", [4 * 162, NT], BF16)
    st_send = nc.dram_tensor("st_send", [128, 1560], F32)
    st_all = nc.dram_tensor("st_all", [4 * 128, 1560], F32)
    cd_dram = nc.dram_tensor("cd_dram", [NCH, 128, CDW], BF16).ap()
    yf_dram = nc.dram_tensor("yf_dram", [NCH, 128, 768], F32).ap()
    modrow_d = nc.dram_tensor("modrow_d", [2, 6 * D], F32).ap()
    xbc_dram = nc.dram_tensor("xbc_dram", [128, 6, NT + 4], BF16).ap()
    GROUPS = groups or [[0, 1, 2, 3], [4, 5, 6, 7]]

    with ExitStack() as es:
        fw = FW(nc, es)
        op = fw.op

        sbn = [0]

        def sb(es_, name, shape, dt):
            sbn[0] += 1
            return es_.enter_context(nc.sbuf_tensor("%s_%d" % (name, sbn[0]), list(shape), dt))

        PS = [es.enter_context(nc.psum_tensor("ps%d" % i, [128, 512], F32)) for i in range(8)]
        PSB = [Buf() for _ in range(8)]
        psr = Ring(list(zip(PS, PSB)))

        cf = sb(es, "cf_sb", [128, 6, 128], F32); b_cf = Buf()
        cb = sb(es, "cb_sb", [128, 5, 128], BF16); b_cb = Buf()
        ident_f, triF, triB, triBs, ones_f = (cf[:, i, :] for i in range(5))
        selm = cf[:, 5, 0:64]
        tri_bf = [cb[:, 3, :], cb[:, 4, :]]
        ident_b, ones_b = cb[:, 0, :], cb[:, 1, :]
        perm32 = cb[0:32, 2, 0:32]
        sel = sb(es, "sel_sb", [128, 16], F32); b_sel = Buf()
        ccs = sb(es, "ccs", [128, 8, 2], F32); b_ccs = Buf()
        ccb = sb(es, "ccb", [128, 8, 2], BF16)
        mv = sb(es, "mv", [128, 48, 2], F32); b_mv = Buf()
        g12 = sb(es, "g12", [128, 16, 2], F32); b_g12 = Buf()
        nws = sb(es, "nws", [128, 16], F32); b_nws = Buf()
        fnw = sb(es, "fnw_sb", [128, 8], F32); b_fnw = Buf()
        smallv = sb(es, "smallv", [128, 256 + 128 + 18 + 6 + 16 + 16 + 8 + 512 + 512], F32); b_small = Buf()
        o_ = [0]

        def carve(n):
            a = smallv[:, o_[0]:o_[0] + n]
            o_[0] += n
            return a
        qnw_b, kvnw_b, convw, convb, dtb_b, alog_b, rdl_b, dskip_b, snw_b = (carve(n) for n in (256, 128, 18, 6, 16, 16, 8, 512, 512))
        a_b = sb(es, "a_b", [128, 16], F32); b_ab = Buf()
        laret = sb(es, "laret", [128, 8], F32); b_laret = Buf()
        la_all = sb(es, "la_all", [128, NCH, 24], F32)
        b_la = [Buf() for _ in range(NCH)]
        hxT = sb(es, "hxT", [128, 8, NT], BF16)
        b_hx = [Buf() for _ in BLOCKS]
        b_mix = [Buf() for _ in range(NCH)]
        rope = None

        fw.dma("sp", cf[:, :, :], cf_in, writes=[b_cf])
        fw.dma("sp", cb[:, :, :], cb_in, writes=[b_cb])
        fw.dma("sp", sel[:, :], sel_in, writes=[b_sel])
        fw.dma("sp", ccs[:, :, :], cc_in, writes=[b_ccs])
        fw.dma("sp", fnw[:, :], fnw_in, writes=[b_fnw])
        op("act", lambda e: e.activation(out=ccb[:, :, :], in_=ccs[:, :, :], func=AF.Silu), reads=[b_ccs], writes=[b_ccs])

        dbg_bufs = []

        def dump(name, ap, bufs):
            if name in dbg_out:
                b_ = Buf()
                dbg_bufs.append(b_)
                ncol = ap.shape[-1]
                for c0 in range(0, ncol, 1024):
                    c1 = min(ncol, c0 + 1024)
                    fw.dma("pool", dbg_out[name][:, c0:c1], ap[:, c0:c1], reads=list(bufs), writes=[b_])

        def stop_at(tag):
            if stop == tag:
                raise _Stop()

        def blk_of_chunk(c):
            return 0 if c < 2 else 1 + (c - 2) // 4

        lm = None
        stopped = False
        try:
          for L in range(n_layers):
              last = L == n_layers - 1
              x_src = xT_in if L == 0 else xres
              fw.barrier()
              offs = 0
              for src, n in ((qnw_in, 256), (kvnw_in, 128), (convw_in, 18), (convb_in, 6), (dtb_in, 16), (alog_in, 16),
                             (rdl_in, 8), (dskip_in, 512), (snw_in, 512)):
                  fw.dma("sp", smallv[:, offs:offs + n], src[L], writes=[b_small])
                  offs += n
              fw.dma("sp", nws[:, :], nw_in[L], writes=[b_nws])
              op("act", lambda e: e.activation(out=a_b[:, :], in_=alog_b, func=AF.Exp), reads=[b_small], writes=[b_ab])
              op("dve", lambda e: e.tensor_scalar(out=a_b[:, :], in0=a_b[:, :], scalar1=-1.0, scalar2=None, op0=ALU.mult), reads=[b_ab], writes=[b_ab])
              op("act", lambda e: e.activation(out=laret[:, :], in_=rdl_b, func=AF.Exp, scale=-1.0), reads=[b_small], writes=[b_laret])
              op("act", lambda e: e.activation(out=laret[:, :], in_=laret[:, :], func=AF.Ln, bias=1.0), reads=[b_laret], writes=[b_laret])
              op("dve", lambda e: e.tensor_scalar(out=laret[:, :], in0=laret[:, :], scalar1=-1.0, scalar2=None, op0=ALU.mult), reads=[b_laret], writes=[b_laret])

              with ExitStack() as ph:
                  mw = [sb(ph, "mw%d" % i, [128, 8, 512], BF16) for i in range(2)]
                  b_mw = [Buf(), Buf()]
                  mrow = sb(ph, "mrow", [2, 6 * D], F32); b_mrow = Buf()
                  mbias = sb(ph, "mbias", [2, 6 * D], F32); b_mbias = Buf()
                  for v in range(2):
                      fw.dma("sp", mbias[v:v + 1, :], mod_b[L], writes=[b_mbias])
                  for j in range(12):
                      t, bt = mw[j % 2], b_mw[j % 2]
                      fw.dma("pool", t[:, :, :], mod_w[L, :, j * 512:(j + 1) * 512].rearrange("(k p) n -> p k n", p=128), writes=[bt])
                      pt, pb = psr.get()
                      fw.group("pe", [(lambda e, k=k: e.matmul(pt[0:2, :], ccb[:, k, :], t[:, k, :], start=(k == 0), stop=(k == 7))) for k in range(8)],
                               reads=[b_ccs, bt], writes=[pb])
                      op("dve", lambda e: e.tensor_tensor(out=mrow[:, j * 512:(j + 1) * 512], in0=pt[0:2, :], in1=mbias[:, j * 512:(j + 1) * 512], op=ALU.add),
                         reads=[pb, b_mbias], writes=[b_mrow])
                  pt, pb = psr.get()
                  fw.group("pe", [(lambda e, j=j: e.transpose(pt[:, 2 * j:2 * j + 2], mrow[0:2, j * 128:(j + 1) * 128], ident_f[0:2, 0:2])) for j in range(48)],
                           reads=[b_mrow, b_cf], writes=[pb])
                  op("dve", lambda e: e.tensor_copy(mv[:, :, :].rearrange("p j v -> p (j v)"), pt[:, 0:96]), reads=[pb], writes=[b_mv])
                  for n_, (sc0, nw0) in enumerate(((8, 0), (32, 8))):
                      op("dve", lambda e: e.tensor_scalar(out=g12[:, n_ * 8:(n_ + 1) * 8, :], in0=mv[:, sc0:sc0 + 8, :], scalar1=1.0, scalar2=None, op0=ALU.add),
                         reads=[b_mv], writes=[b_g12])
                      op("dve", lambda e: e.tensor_tensor(out=g12[:, n_ * 8:(n_ + 1) * 8, :], in0=g12[:, n_ * 8:(n_ + 1) * 8, :],
                                                           in1=nws[:, nw0:nw0 + 8].unsqueeze(2).to_broadcast([128, 8, 2]), op=ALU.mult),
                         reads=[b_g12, b_nws], writes=[b_g12])

              if L == 0:
                  dump("mv", mv[:, :, :].rearrange("p j v -> p (j v)"), [b_mv])
                  dump("g12", g12[:, :, :].rearrange("p j v -> p (j v)"), [b_g12])
                  stop_at("mod")
              fw.barrier()
              def norm_mod(xb_ap, xb_buf, t0, W, v, gofs, shofs, wk, out_buf):
                  sq, b_sq = wk["sq"].get()
                  op("act", lambda e: e.activation(out=sq[:, :, 0:W], in_=xb_ap, func=AF.Square), reads=[xb_buf], writes=[b_sq])
                  pt, pb = psr.get()
                  fw.group("pe", [(lambda e, k=k: e.matmul(pt[:, 0:W], ones_b, sq[:, k, 0:W], start=(k == 0), stop=(k == 7))) for k in range(8)],
                           reads=[b_sq, b_cb], writes=[pb])
                  rs, b_rs = wk["rs"].get()
                  op("act", lambda e: e.activation(out=rs[:, 0:W], in_=pt[:, 0:W], func=AF.Sqrt, scale=1.0 / D, bias=EPS), reads=[pb], writes=[b_rs])
                  op("dve", lambda e: e.reciprocal(out=rs[:, 0:W], in_=rs[:, 0:W]), reads=[b_rs], writes=[b_rs])
                  for k in range(8):
                      tmp, b_tmp = wk["tmp"].get()
                      op("dve", lambda e: e.tensor_tensor(out=tmp[:, 0:W], in0=xb_ap[:, k, :], in1=rs[:, 0:W], op=ALU.mult), reads=[xb_buf, b_rs], writes=[b_tmp])
                      op("act", lambda e: e.activation(out=hxT[:, k, t0:t0 + W], in_=tmp[:, 0:W], func=AF.Identity,
                                                        scale=g12[:, gofs + k, v:v + 1], bias=mv[:, shofs + k, v:v + 1]),
                         reads=[b_tmp, b_g12, b_mv], writes=[out_buf])

              lm = ExitStack()
              mixT = sb(lm, "mixT", [128, 8, NT], BF16)
              b_xres = Buf()
              with ExitStack() as mx:
                  xbcT = xbc_dram
                  qT = sb(mx, "qT", [96, 4, NT], BF16); b_qT = [Buf() for _ in BLOCKS]
                  hal = sb(mx, "hal", [128, 6, 4], BF16); b_hal = Buf()
                  b_xbc = [Buf() for _ in BLOCKS]
                  b_halo = Buf()
                  XO = lambda t: t + 1 if t < TC else t + 3
                  with ExitStack() as s1:
                      W1a = sb(s1, "W1a", [128, 8, 416], BF16); b_W1a = Buf()
                      W1x = sb(s1, "W1x", [128, 8, 768], BF16); b_W1x = Buf()
                      wqb = sb(s1, "wqb", [128, 2, 384], BF16); b_wqb = Buf()
                      wqr = sb(s1, "wqr", [128, 2, 384], BF16); b_wqr = Buf()
                      ropes_t = sb(s1, "ropes", [128, 2, 512], F32); b_rope = Buf()
                      xst = sb(s1, "xst", [128, 6, 512], BF16); b_xst = Buf()
                      bcol_r = Ring([(sb(s1, "bcol%d" % i, [128, 136], BF16), Buf()) for i in range(2)])
                      kvl = sb(s1, "kvl", [128, 2, NT], BF16); b_kvl = Buf()
                      xb_r = Ring([(sb(s1, "xblk%d" % i, [128, 8, 512], F32), Buf()) for i in range(1)])
                      wk = {"sq": Ring([(sb(s1, "sq%d" % i, [128, 8, 512], BF16), Buf()) for i in range(1)]),
                            "rs": Ring([(sb(s1, "rs%d" % i, [128, 512], F32), Buf()) for i in range(1)]),
                            "tmp": Ring([(sb(s1, "tmp%d" % i, [128, 512], F32), Buf()) for i in range(2)])}
                      tk_r = Ring([(sb(s1, "tk%d" % i, [128, 416], BF16), Buf()) for i in range(2)])
                      st_r = Ring([(sb(s1, "st%d" % i, [128, 4], F32), Buf()) for i in range(2)])
                      cqT = sb(s1, "cqT", [128, 2, 512], BF16); b_cqT = Buf()
                      kpT = sb(s1, "kpT", [32, 512], BF16); b_kpT = Buf()
                      junk = sb(s1, "junk", [128, 256], F32); b_junk = Buf()
                      rt_r = Ring([(sb(s1, "rt%d" % i, [128, 512], F32), Buf()) for i in range(2)])
                      fw.dma("pool", W1a[:, :, :], w_in[L, :, 0:416].rearrange("(k p) n -> p k n", p=128), writes=[b_W1a])
                      fw.dma("pool", W1x[:, :, :], w_in[L, :, 1952:2720].rearrange("(k p) n -> p k n", p=128), writes=[b_W1x])
                      fw.dma("pool", wqb[:, :, :], w_qb[L].rearrange("(k p) n -> p k n", p=128), writes=[b_wqb])
                      fw.dma("pool", wqr[:, :, :], w_qbrot[L].rearrange("(k p) n -> p k n", p=128), writes=[b_wqr])
                      b_send = Buf()
                      zrow = sb(s1, "zrow", [2, 1536], BF16); b_zrow = Buf()
                      op("pool", lambda e: e.memset(zrow[:, :], 0.0), writes=[b_zrow])
                      fw.dma("sp", kv_send[160:162, 768:NT], zrow[:, :], reads=[b_zrow], writes=[b_send], chanbuf=b_zrow)
                      for bi, (t0, W, v) in enumerate(BLOCKS):
                          fw.dma("sp", ropes_t[:, :, 0:W], rope_in[:, :, t0:t0 + W], writes=[b_rope])
                          ropes = ropes_t[:, :, :]
                          xb, b_xb = xb_r.get()
                          fw.dma("sp", xb[:, :, 0:W], x_src[:, t0:t0 + W].rearrange("(k p) t -> p k t", p=128), writes=[b_xb])
                          norm_mod(xb[:, :, 0:W], b_xb, t0, W, v, 0, 0, wk, b_hx[bi])
                          if bi == 0:
                              stop_at("s1a")
                          for oc in range(6):
                              pt, pb = psr.get()
                              fw.group("pe", [(lambda e, k=k: e.matmul(pt[:, 0:W], W1x[:, k, oc * 128:(oc + 1) * 128], hxT[:, k, t0:t0 + W], start=(k == 0), stop=(k == 7))) for k in range(8)],
                                       reads=[b_W1x, b_hx[bi]], writes=[pb])
                              op("act", lambda e: e.activation(out=xst[:, oc, 0:W], in_=pt[:, 0:W], func=AF.Copy), reads=[pb], writes=[b_xst])
                          fw.dma("sp", xbcT[:, :, XO(t0):XO(t0) + W], xst[:, :, 0:W], reads=[b_xst], writes=[b_xbc[bi]], chanbuf=b_xst)
                          if bi in (1, 4):
                              ccol = 0 if bi == 1 else W - 1
                              rrow = 160 if bi == 1 else 161
                              bcol, b_bcol = bcol_r.get()
                              op("dve", lambda e: e.tensor_copy(bcol[:, 0:6], xst[:, :, ccol]), reads=[b_xst], writes=[b_bcol])
                              p2, pb2 = psr.get()
                              pbf = p2[:, 0:64].bitcast(BF16)
                              op("pe", lambda e: e.transpose(pbf[0:6, 0:128], bcol[:, 0:6], ident_b), reads=[b_bcol, b_cb], writes=[pb2])
                              op("dve", lambda e: e.tensor_copy(bcol[0:6, 8:136], pbf[0:6, 0:128]), reads=[pb2], writes=[b_bcol])
                              fw.dma("sp", kv_send[rrow:rrow + 1, 0:768].rearrange("o (c p) -> (o c) p", p=128), bcol[0:6, 8:136], reads=[b_bcol], writes=[b_send], chanbuf=b_bcol)
                          if bi == 0:
                              stop_at("s1b")
                          for ti in range(W // 128):
                              tt = t0 + ti * 128
                              pt, pb = psr.get()
                              fw.group("pe", [(lambda e, k=k: e.matmul(pt[:, 0:416], hxT[:, k, tt:tt + 128], W1a[:, k, :], start=(k == 0), stop=(k == 7))) for k in range(8)],
                                       reads=[b_W1a, b_hx[bi]], writes=[pb])
                              if bi == 0 and ti == 0:
                                  stop_at("t1")
                              stt_, b_st = st_r.get()
                              op("act", lambda e: e.activation(out=junk[:, 0:256], in_=pt[:, 0:256], func=AF.Square, accum_out=stt_[:, 0:1]), reads=[pb], writes=[b_junk, b_st])
                              op("act", lambda e: e.activation(out=junk[:, 0:128], in_=pt[:, 256:384], func=AF.Square, accum_out=stt_[:, 1:2]), reads=[pb], writes=[b_junk, b_st])
                              op("act", lambda e: e.activation(out=stt_[:, 2:3], in_=stt_[:, 0:1], func=AF.Sqrt, scale=1.0 / 256, bias=EPS), reads=[b_st], writes=[b_st])
                              op("act", lambda e: e.activation(out=stt_[:, 3:4], in_=stt_[:, 1:2], func=AF.Sqrt, scale=1.0 / 128, bias=EPS), reads=[b_st], writes=[b_st])
                              op("dve", lambda e: e.reciprocal(out=stt_[:, 2:4], in_=stt_[:, 2:4]), reads=[b_st], writes=[b_st])
                              if bi == 0 and ti == 0:
                                  stop_at("t2")
                              tk, b_tk = tk_r.get()
                              op("dve", lambda e: e.scalar_tensor_tensor(out=tk[:, 0:256], in0=pt[:, 0:256], scalar=stt_[:, 2:3], op0=ALU.mult, in1=qnw_b, op1=ALU.mult),
                                 reads=[pb, b_st, b_small], writes=[b_tk])
                              op("dve", lambda e: e.scalar_tensor_tensor(out=tk[:, 256:384], in0=pt[:, 256:384], scalar=stt_[:, 3:4], op0=ALU.mult, in1=kvnw_b, op1=ALU.mult),
                                 reads=[pb, b_st, b_small], writes=[b_tk])
                              op("act", lambda e: e.activation(out=tk[:, 384:416], in_=pt[:, 384:416], func=AF.Copy), reads=[pb], writes=[b_tk])
                              if bi == 0 and ti == 0:
                                  stop_at("t3")
                              p2, pb2 = psr.get()
                              pbf = p2[:, 0:256].bitcast(BF16)
                              fw.group("pe", [lambda e: e.transpose(pbf[:, 0:128], tk[:, 0:128], ident_b),
                                              lambda e: e.transpose(pbf[:, 128:256], tk[:, 128:256], ident_b),
                                              lambda e: e.transpose(pbf[:, 256:384], tk[:, 256:384], ident_b),
                                              lambda e: e.transpose(pbf[0:32, 384:512], tk[:, 384:416], ident_b)],
                                       reads=[b_tk, b_cb], writes=[pb2])
                              if bi == 0 and ti == 0:
                                  stop_at("t4")
                              op("dve", lambda e: e.tensor_copy(cqT[:, :, ti * 128:(ti + 1) * 128], pbf[:, 0:256].rearrange("p (c t) -> p c t", c=2)), reads=[pb2], writes=[b_cqT])
                              if bi == 0 and ti == 0:
                                  stop_at("t5")
                              op("dve", lambda e: e.tensor_copy(kvl[:, 0, tt:tt + 128], pbf[:, 256:384]), reads=[pb2], writes=[b_kvl])
                              if bi == 0 and ti == 0:
                                  stop_at("t6")
                              op("dve", lambda e: e.tensor_copy(kpT[:, ti * 128:(ti + 1) * 128], pbf[0:32, 384:512]), reads=[pb2], writes=[b_kpT])
                          if bi == 0:
                              stop_at("s1c")
                          pt, pb = psr.get()
                          op("pe", lambda e: e.matmul(pt[0:32, 0:W], perm32, kpT[:, 0:W], start=True, stop=True), reads=[b_cb, b_kpT], writes=[pb])
                          r1, b_r1 = rt_r.get()
                          r2, b_r2 = rt_r.get()
                          op("dve", lambda e: e.tensor_tensor(out=r1[0:32, 0:W], in0=pt[0:32, 0:W], in1=ropes_t[0:32, 1, 0:W], op=ALU.mult), reads=[pb, b_rope], writes=[b_r1])
                          op("pool", lambda e: e.tensor_tensor(out=r2[0:32, 0:W], in0=kpT[:, 0:W], in1=ropes_t[0:32, 0, 0:W], op=ALU.mult), reads=[b_kpT, b_rope], writes=[b_r2])
                          op("dve", lambda e: e.tensor_tensor(out=kvl[0:32, 1, t0:t0 + W], in0=r1[0:32, 0:W], in1=r2[0:32, 0:W], op=ALU.add), reads=[b_r1, b_r2], writes=[b_kvl])
                          if bi == 0:
                              stop_at("s1d")
                          for h in range(4):
                              pq, pbq = psr.get()
                              pr, pbr = psr.get()
                              fw.group("pe", [(lambda e, k=k: e.matmul(pq[0:96, 0:W], wqb[:, k, h * 96:(h + 1) * 96], cqT[:, k, 0:W], start=(k == 0), stop=(k == 1))) for k in range(2)],
                                       reads=[b_wqb, b_cqT], writes=[pbq])
                              fw.group("pe", [(lambda e, k=k: e.matmul(pr[0:96, 0:W], wqr[:, k, h * 96:(h + 1) * 96], cqT[:, k, 0:W], start=(k == 0), stop=(k == 1))) for k in range(2)],
                                       reads=[b_wqr, b_cqT], writes=[pbr])
                              op("act", lambda e: e.activation(out=qT[0:64, h, t0:t0 + W], in_=pq[0:64, 0:W], func=AF.Copy), reads=[pbq], writes=[b_qT[bi]])
                              r1, b_r1 = rt_r.get()
                              r2, b_r2 = rt_r.get()
                              op("dve", lambda e: e.tensor_tensor(out=r1[64:96, 0:W], in0=pr[64:96, 0:W], in1=ropes_t[64:96, 1, 0:W], op=ALU.mult), reads=[pbr, b_rope], writes=[b_r1])
                              op("dve", lambda e: e.tensor_tensor(out=r2[64:96, 0:W], in0=pq[64:96, 0:W], in1=ropes_t[64:96, 0, 0:W], op=ALU.mult), reads=[pbq, b_rope], writes=[b_r2])
                              if bi == 0 and h == 0:
                                  stop_at("q0")
                              op("pool", lambda e: e.tensor_tensor(out=qT[64:96, h, t0:t0 + W], in0=r1[64:96, 0:W], in1=r2[64:96, 0:W], op=ALU.add), reads=[b_r1, b_r2], writes=[b_qT[bi]])
                              if bi == 0 and h == 0:
                                  stop_at("q1")
                          if bi == 0:
                              stop_at("b0")
                          if bi == 1:
                              stop_at("b1")
                      fw.dma("sp", kv_send[0:128, :], kvl[:, 0, :], reads=[b_kvl], writes=[b_send])
                      fw.dma("sp", kv_send[128:160, :], kvl[0:32, 1, :], reads=[b_kvl], writes=[b_send])
                      if L == 0:
                          dump("hx", hxT[:, :, :].rearrange("p k t -> p (k t)"), b_hx)
                          dump("qT", qT[:, :, :].rearrange("p k t -> p (k t)"), b_qT)
                          dump("kvl", kvl[:, :, :].rearrange("p k t -> p (k t)"), [b_kvl])
                          dump("xbcA", xbc_dram[:, 0, 0:260], b_xbc)
                          stop_at("s1")
                      b_all = Buf()
                      fw.cc("AllGather", kv_send.ap().opt(), kv_all.ap().opt(), GROUPS, [b_send], [b_all], b_all)
                  fw.barrier()
                  with ExitStack() as hh:
                      bd = sb(hh, "bd", [8, 768], BF16); b_bd = Buf()
                      selb = sb(hh, "selb", [8, 2], BF16); b_selb = Buf()
                      for r in range(4):
                          fw.dma("sp", bd[2 * r:2 * r + 2, :], kv_all[r * 162 + 160:r * 162 + 162, 0:768], reads=[b_all], writes=[b_bd])
                      op("dve", lambda e: e.tensor_copy(selb[:, :], sel[0:8, 0:2]), reads=[b_sel], writes=[b_selb])
                      op("dve", lambda e: e.memset(hal[:, :, :], 0.0), writes=[b_hal])
                      for oc in range(6):
                          pt, pb = psr.get()
                          op("pe", lambda e: e.matmul(pt[:, 0:2], bd[:, oc * 128:(oc + 1) * 128], selb[:, :], start=True, stop=True), reads=[b_bd, b_selb], writes=[pb])
                          op("dve", lambda e: e.tensor_copy(hal[:, oc, 2:4], pt[:, 0:2]), reads=[pb], writes=[b_hal])

                  fw.barrier()
                  with ExitStack() as at:
                      NK = TC + 4 * TL
                      ckT = sb(at, "ckT", [128, NK], BF16); b_ck = Buf()
                      KT = sb(at, "KT", [96, NK], BF16); b_KTp = Buf(); b_KTn = Buf()
                      Va = sb(at, "Va", [128, NK // 128, 4, 65], BF16); b_Va = Buf()
                      wkb = sb(at, "wkb", [128, 256], BF16); b_wkb = Buf()
                      wvb = sb(at, "wvb", [128, 256], BF16); b_wvb = Buf()
                      pT_r = Ring([(sb(at, "pT%d" % i, [128, 512], BF16), Buf()) for i in range(5)])
                      s_ring = Ring(list(zip(PS[0:4], PSB[0:4])))
                      o_ring = Ring(list(zip(PS[4:6], PSB[4:6])))
                      psr_keep = psr
                      psr = Ring(list(zip(PS[6:8], PSB[6:8])))
                      osb = sb(at, "osb", [65, 512], F32); b_osb = Buf()
                      rb = sb(at, "rb", [64, 512], F32); b_rb = Buf()
                      ot = sb(at, "ot", [64, 512], BF16); b_ot = Buf()
                      fw.dma("pool", wkb[:, :], w_kb[L], writes=[b_wkb])
                      fw.dma("pool", wvb[:, :], w_vb[L], writes=[b_wvb])
                      fw.dma("sp", ckT[:, 0:TC], kv_all[0:128, 0:TC], reads=[b_all], writes=[b_ck])
                      fw.dma("sp", KT[64:96, 0:TC], kv_all[128:160, 0:TC], reads=[b_all], writes=[b_KTp])
                      for r in range(4):
                          fw.dma("sp", ckT[:, TC + r * TL:TC + (r + 1) * TL], kv_all[r * 162:r * 162 + 128, TC:NT], reads=[b_all], writes=[b_ck])
                          fw.dma("sp", KT[64:96, TC + r * TL:TC + (r + 1) * TL], kv_all[r * 162 + 128:r * 162 + 160, TC:NT], reads=[b_all], writes=[b_KTp])
                      op("pool", lambda e: e.memset(Va[:, :, :, 64:65], 1.0), writes=[b_Va])
                      for kc in range(NK // 128):
                          pt, pb = psr.get()
                          op("pe", lambda e: e.matmul(pt[:, 0:256], ckT[:, kc * 128:(kc + 1) * 128], wvb[:, :], start=True, stop=True), reads=[b_ck, b_wvb], writes=[pb])
                          eng = "act" if kc % 2 == 0 else "dve"
                          if eng == "act":
                              op("act", lambda e: e.activation(out=Va[:, kc, :, 0:64], in_=pt[:, 0:256].rearrange("p (h d) -> p h d", h=4), func=AF.Copy), reads=[pb], writes=[b_Va])
                          else:
                              op("dve", lambda e: e.tensor_copy(Va[:, kc, :, 0:64], pt[:, 0:256].rearrange("p (h d) -> p h d", h=4)), reads=[pb], writes=[b_Va])
                      for h in range(4):
                          for kb in range((NK + 511) // 512):
                              k0 = kb * 512
                              kw_ = min(512, NK - k0)
                              pt, pb = psr.get()
                              op("pe", lambda e: e.matmul(pt[0:64, 0:kw_], wkb[:, h * 64:(h + 1) * 64], ckT[:, k0:k0 + kw_], start=True, stop=True), reads=[b_wkb, b_ck], writes=[pb])
                              if kb % 2 == 0:
                                  op("act", lambda e: e.activation(out=KT[0:64, k0:k0 + kw_], in_=pt[0:64, 0:kw_], func=AF.Copy), reads=[pb], writes=[b_KTn])
                              else:
                                  op("dve", lambda e: e.tensor_copy(KT[0:64, k0:k0 + kw_], pt[0:64, 0:kw_]), reads=[pb], writes=[b_KTn])
                          for bi, (t0, W, v) in enumerate(BLOCKS):
                              if v == 1 and last:
                                  continue
                              nkc = 2 if v == 1 else NK // 128
                              po, pbo = o_ring.get()
                              LA = 2
                              pend = []
                              for kc in range(nkc + LA):
                                  if kc < nkc:
                                      ps_, pbs = s_ring.get()
                                      op("pe", lambda e: e.matmul(ps_[:, 0:W], KT[0:96, kc * 128:(kc + 1) * 128], qT[0:96, h, t0:t0 + W], start=True, stop=True),
                                         reads=[b_KTn, b_KTp, b_qT[bi]], writes=[pbs])
                                      pT, b_pT = pT_r.get()
                                      op("act", lambda e: e.activation(out=pT[:, 0:W], in_=ps_[:, 0:W], func=AF.Exp, scale=MLA_SCALE), reads=[pbs], writes=[b_pT])
                                      pend.append((pT, b_pT))
                                  if kc >= LA:
                                      kk = kc - LA
                                      pT2, b_pT2 = pend[kk]
                                      op("pe", lambda e: e.matmul(po[0:65, 0:W], Va[:, kk, h, :], pT2[:, 0:W], start=(kk == 0), stop=(kk == nkc - 1)),
                                         reads=[b_Va, b_pT2], writes=[pbo])
                              op("dve", lambda e: e.tensor_copy(osb[:, 0:W], po[0:65, 0:W]), reads=[pbo], writes=[b_osb])
                              op("dve", lambda e: e.reciprocal(out=osb[64:65, 0:W], in_=osb[64:65, 0:W]), reads=[b_osb], writes=[b_osb])
                              pt, pb = psr.get()
                              op("pe", lambda e: e.matmul(pt[0:64, 0:W], selm[0:65, :], osb[0:65, 0:W], start=True, stop=True), reads=[b_osb, b_cf], writes=[pb])
                              op("act", lambda e: e.activation(out=rb[:, 0:W], in_=pt[0:64, 0:W], func=AF.Copy), reads=[pb], writes=[b_rb])
                              mixw = [b_mix[c] for c in range(t0 // 128, (t0 + W) // 128)]
                              if h % 2 == 0:
                                  op("dve", lambda e: e.tensor_tensor(out=mixT[0:64, h // 2, t0:t0 + W], in0=osb[0:64, 0:W], in1=rb[:, 0:W], op=ALU.mult),
                                     reads=[b_osb, b_rb], writes=mixw)
                              else:
                                  op("dve", lambda e: e.tensor_tensor(out=ot[:, 0:W], in0=osb[0:64, 0:W], in1=rb[:, 0:W], op=ALU.mult), reads=[b_osb, b_rb], writes=[b_ot])
                                  op("dve", lambda e: e.tensor_copy(mixT[64:128, h // 2, t0:t0 + W], ot[:, 0:W]), reads=[b_ot], writes=mixw)

                  psr = psr_keep
                  if L == 0:
                      dump("att", mixT[:, 0:2, :].rearrange("p k t -> p (k t)"), b_mix)
                      stop_at("att")
                  fw.barrier()
                  with ExitStack() as sc:
                      W2 = sb(sc, "W2", [128, 8, 1552], BF16); b_W2 = Buf()
                      fw.dma("pool", W2[:, :, 0:1536], w_in[L, :, 416:1952].rearrange("(k p) n -> p k n", p=128), writes=[b_W2])
                      fw.dma("pool", W2[:, :, 1536:1552], w_in[L, :, 2720:2736].rearrange("(k p) n -> p k n", p=128), writes=[b_W2])
                      S_run = [sb(sc, "Srun%d" % d, [128, 6, 128], F32) for d in range(2)]; b_S = [Buf(), Buf()]
                      Sbd = [sb(sc, "Sbd%d" % d, [128, 6, 128], BF16) for d in range(2)]; b_Sbd = [Buf(), Buf()]
                      S_ctx = [sb(sc, "Sctx%d" % d, [128, 6, 128], F32) for d in range(2)]; b_Sctx = [Buf(), Buf()]
                      Lacc = sb(sc, "Lacc", [128, 24], F32); b_Lacc = Buf()
                      eL = sb(sc, "eL", [128, 12], F32); b_eL = Buf()
                      cd_r = Ring([(sb(sc, "cd%d" % i, [128, CDW], BF16), Buf()) for i in range(2)])
                      xin_pre = {}
                      la_hl = sb(sc, "la_hl", [128, NCH, 48], BF16)
                      xin_r = Ring([(sb(sc, "xin%d" % i, [128, 6, 130], BF16), Buf()) for i in range(2)])
                      xact_r = Ring([(sb(sc, "xact%d" % i, [128, 6, 128], BF16), Buf()) for i in range(2)])
                      cacc_r = Ring([(sb(sc, "cacc%d" % i, [128, 128], F32), Buf()) for i in range(3)])
                      dt_r = Ring([(sb(sc, "dtt%d" % i, [128, 16], F32), Buf()) for i in range(2)])
                      scl_r = Ring([(sb(sc, "scl%d" % i, [128, 64], F32), Buf()) for i in range(2)])
                      kw_r = Ring([(sb(sc, "kw%d" % i, [128, 128], BF16), Buf()) for i in range(3)])
                      dec_r = Ring([(sb(sc, "dec%d" % i, [128, 128], F32), Buf()) for i in range(4)])
                      qkm_r = Ring([(sb(sc, "qkm%d" % i, [128, 128], F32), Buf()) for i in range(6)])
                      scT_r = Ring([(sb(sc, "scT%d" % i, [128, 128], BF16), Buf()) for i in range(5)])
                      yA_r = Ring([(sb(sc, "yAs%d" % i, [128, 768], F32), Buf()) for i in range(2)])
                      yt_r = Ring([(sb(sc, "yts%d" % i, [128, 768], F32), Buf()) for i in range(2)])
                      yf_r = Ring([(sb(sc, "yfs%d" % i, [128, 768], F32), Buf()) for i in range(1)])
                      gt_r = Ring([(sb(sc, "gts%d" % i, [128, 768], F32), Buf()) for i in range(1)])
                      mo_r = Ring([(sb(sc, "mos%d" % i, [128, 768], BF16), Buf()) for i in range(2)])
                      gst_r = Ring([(sb(sc, "gst%d" % i, [128, 32], F32), Buf()) for i in range(2)])
                      sr_t = sb(sc, "sr_t", [128, 1560], F32); b_sr = Buf()
                      dm_t = sb(sc, "dm_t", [128, 12], F32); b_dm = Buf()
                      tri_d = [triF, triB]
                      b_cdd = [Buf() for _ in range(NCH)]
                      b_yfd = [Buf() for _ in range(NCH)]

                      def hx_bufs(c):
                          return [b_hx[blk_of_chunk(c)]]

                      def xbc_bufs(c):
                          bi = blk_of_chunk(c)
                          return [b_xbc[i] for i in range(max(0, bi - 1), min(len(BLOCKS), bi + 2))]

                      def small_stats(c, d):
                          pt, pb = psr.get()
                          la_c = la_all[:, c, d * 12:(d + 1) * 12]
                          op("pe", lambda e: e.matmul(pt[:, 0:12], tri_d[d], la_c, start=True, stop=True), reads=[b_cf, b_la[c]], writes=[pb])
                          pt2, pb2 = psr.get()
                          op("pe", lambda e: e.matmul(pt2[:, 0:12], ones_f, la_c, start=True, stop=True), reads=[b_cf, b_la[c]], writes=[pb2])
                          scl, b_scl = scl_r.get()
                          op("act", lambda e: e.activation(out=scl[:, 0:12], in_=pt[:, 0:12], func=AF.Copy, scale=-1.0), reads=[pb], writes=[b_scl])
                          op("act", lambda e: e.activation(out=scl[:, 12:24], in_=pt[:, 0:12], func=AF.Exp), reads=[pb], writes=[b_scl])
                          op("act", lambda e: e.activation(out=scl[:, 24:36], in_=pt2[:, 0:12], func=AF.Exp), reads=[pb2], writes=[b_scl])
                          op("dve", lambda e: e.tensor_tensor(out=scl[:, 36:48], in0=pt2[:, 0:12], in1=scl[:, 0:12], op=ALU.add), reads=[pb2, b_scl], writes=[b_scl])
                          op("act", lambda e: e.activation(out=scl[:, 36:48], in_=scl[:, 36:48], func=AF.Exp), reads=[b_scl], writes=[b_scl])
                          op("dve", lambda e: e.tensor_copy(scl[:, 48:60], pt2[:, 0:12]), reads=[pb2], writes=[b_scl])
                          return scl, b_scl

                      def kv_pair(cd, m, d):
                          if m < 2:
                              return cd[:, CD_KTOK + m * 128:CD_KTOK + (m + 1) * 128], cd[:, CD_VTOK + m * 128:CD_VTOK + (m + 1) * 128]
                          xo = (CD_XF if d == 0 else CD_XB) + (m - 2) * 128
                          return cd[:, CD_BTOK:CD_BTOK + 128], cd[:, xo:xo + 128]

                      def chunk_state(cd, b_cd, scl, b_scl, d, m):
                          k_ap, v_ap = kv_pair(cd, m, d)
                          kw, b_kw = kw_r.get()
                          op("dve", lambda e: e.tensor_tensor(out=kw[:, :].rearrange("p (h n) -> p h n", h=2), in0=k_ap.rearrange("p (h n) -> p h n", h=2),
                                                               in1=scl[:, 36 + 2 * m:38 + 2 * m].unsqueeze(2).to_broadcast([128, 2, 64]), op=ALU.mult),
                             reads=[b_cd, b_scl], writes=[b_kw])
                          pt, pb = psr.get()
                          op("pe", lambda e: e.matmul(pt[:, 0:128], kw[:, :], v_ap, start=True, stop=True), reads=[b_kw, b_cd], writes=[pb])
                          return pt, pb

                      kwb_r = Ring([(sb(sc, "kwb%d" % i, [128, 128], BF16), Buf()) for i in range(12)])

                      def states_batch(cd, b_cd, scl, b_scl, d, slots):
                          kws = []
                          for m in range(6):
                              k_ap, v_ap = kv_pair(cd, m, d)
                              kw, b_kw = kwb_r.get()
                              op("pool", lambda e: e.tensor_tensor(out=kw[:, :].rearrange("p (h n) -> p h n", h=2), in0=k_ap.rearrange("p (h n) -> p h n", h=2),
                                                                    in1=scl[:, 36 + 2 * m:38 + 2 * m].unsqueeze(2).to_broadcast([128, 2, 64]), op=ALU.mult),
                                 reads=[b_cd, b_scl], writes=[b_kw])
                              kws.append((kw, b_kw, v_ap))
                          outs = []
                          for m in range(6):
                              kw, b_kw, v_ap = kws[m]
                              pt, pb = slots[m]
                              op("pe", lambda e: e.matmul(pt, kw[:, :], v_ap, start=True, stop=True), reads=[b_kw, b_cd], writes=[pb])
                              outs.append((pt, pb))
                          return outs

                      def upd_state(S, b_S_, scal, b_scal, so, m, add_ap, add_buf):
                          for half in range(2):
                              r0, r1 = half * 64, half * 64 + 64
                              u = 2 * m + half
                              op("dve", lambda e: e.scalar_tensor_tensor(out=S[r0:r1, m, r0:r1], in0=S[r0:r1, m, r0:r1], scalar=scal[r0:r1, so + u:so + u + 1],
                                                                          op0=ALU.mult, in1=add_ap[r0:r1, r0:r1], op1=ALU.add),
                                 reads=[b_S_, b_scal, add_buf], writes=[b_S_])

                      for d in range(2):
                          op("pool", lambda e: e.memset(S_run[d][:, :, :], 0.0), writes=[b_S[d]])
                      op("pool", lambda e: e.memset(Lacc[:, :], 0.0), writes=[b_Lacc])
                      for c in range(NCH):
                          tt = c * 128
                          cd, b_cd = cd_r.get()
                          hb = hx_bufs(c)
                          for col0, dst, scale in ((0, CD_QT, 0.125), (128, CD_QT + 128, 0.125), (256, CD_KT, 1.0), (384, CD_KT + 128, 1.0)):
                              pt, pb = psr.get()
                              fw.group("pe", [(lambda e, k=k: e.matmul(pt[:, 0:128], W2[:, k, col0:col0 + 128], hxT[:, k, tt:tt + 128], start=(k == 0), stop=(k == 7))) for k in range(8)],
                                       reads=[b_W2] + hb, writes=[pb])
                              op("act", lambda e: e.activation(out=cd[:, dst:dst + 128], in_=pt[:, 0:128], func=AF.Copy, scale=scale), reads=[pb], writes=[b_cd])
                          pt, pb = psr.get()
                          fw.group("pe", [(lambda e, k=k: e.matmul(pt[:, 0:512], hxT[:, k, tt:tt + 128], W2[:, k, 256:768], start=(k == 0), stop=(k == 7))) for k in range(8)],
                                   reads=[b_W2] + hb, writes=[pb])
                          op("dve", lambda e: e.tensor_copy(cd[:, CD_KTOK:CD_KTOK + 256], pt[:, 0:256]), reads=[pb], writes=[b_cd])
                          op("act", lambda e: e.activation(out=cd[:, CD_VTOK:CD_VTOK + 256], in_=pt[:, 256:512], func=AF.Copy), reads=[pb], writes=[b_cd])
                          pt, pb = psr.get()
                          fw.group("pe", [(lambda e, k=k: e.matmul(pt[:, 0:256], hxT[:, k, tt:tt + 128], W2[:, k, 768:1024], start=(k == 0), stop=(k == 7))) for k in range(8)],
                                   reads=[b_W2] + hb, writes=[pb])
                          op("act", lambda e: e.activation(out=cd[:, CD_SG:CD_SG + 256], in_=pt[:, 0:256], func=AF.Silu), reads=[pb], writes=[b_cd])
                          pt, pb = psr.get()
                          fw.group("pe", [(lambda e, k=k: e.matmul(pt[:, 0:512], hxT[:, k, tt:tt + 128], W2[:, k, 1024:1536], start=(k == 0), stop=(k == 7))) for k in range(8)],
                                   reads=[b_W2] + hb, writes=[pb])
                          op("act", lambda e: e.activation(out=cd[:, CD_SZ:CD_SZ + 512], in_=pt[:, 0:512], func=AF.Silu), reads=[pb], writes=[b_cd])
                          pt, pb = psr.get()
                          fw.group("pe", [(lambda e, k=k: e.matmul(pt[:, 0:16], hxT[:, k, tt:tt + 128], W2[:, k, 1536:1552], start=(k == 0), stop=(k == 7))) for k in range(8)],
                                   reads=[b_W2] + hb, writes=[pb])
                          dtt, b_dt = dt_r.get()
                          op("dve", lambda e: e.tensor_tensor(out=dtt[:, :], in0=pt[:, 0:16], in1=dtb_b, op=ALU.add), reads=[pb, b_small], writes=[b_dt])
                          op("act", lambda e: e.activation(out=dtt[:, :], in_=dtt[:, :], func=AF.Exp), reads=[b_dt], writes=[b_dt])
                          op("act", lambda e: e.activation(out=dtt[:, :], in_=dtt[:, :], func=AF.Ln, bias=1.0), reads=[b_dt], writes=[b_dt])
                          for d in range(2):
                              op("dve", lambda e: e.tensor_copy(la_all[:, c, d * 12:d * 12 + 4], laret[:, d * 4:d * 4 + 4]), reads=[b_laret], writes=[b_la[c]])
                              op("dve", lambda e: e.tensor_tensor(out=la_all[:, c, d * 12 + 4:d * 12 + 12], in0=dtt[:, d * 8:d * 8 + 8], in1=a_b[:, d * 8:d * 8 + 8], op=ALU.mult),
                                 reads=[b_dt, b_ab], writes=[b_la[c]])
                          op("dve", lambda e: e.tensor_copy(la_hl[:, c, 0:24], la_all[:, c, :]), reads=[b_la[c]], writes=[b_la[c]])
                          op("dve", lambda e: e.tensor_tensor(out=la_hl[:, c, 24:48], in0=la_all[:, c, :], in1=la_hl[:, c, 0:24], op=ALU.subtract), reads=[b_la[c]], writes=[b_la[c]])
                          xact, b_xa = xact_r.get()
                          xc = (tt + 1) if tt < TC else (tt + 3)
                          xb_ = xbc_bufs(c)
                          def load_xin(c_):
                              tt_ = c_ * 128
                              xc_ = (tt_ + 1) if tt_ < TC else (tt_ + 3)
                              xin_, b_xin_ = xin_r.get()
                              lo = 1 if c_ in (0, 2) else 0
                              hi = 129 if c_ in (1, NCH - 1) else 130
                              fw.dma("sp", xin_[:, :, lo:hi], xbcT[:, :, xc_ - 1 + lo:xc_ - 1 + hi], reads=xbc_bufs(c_), writes=[b_xin_])
                              return xin_, b_xin_
                          if c not in xin_pre:
                              xin_pre[c] = load_xin(c)
                          xin, b_xin = xin_pre.pop(c)
                          if c + 1 < NCH:
                              xin_pre[c + 1] = load_xin(c + 1)
                          xb_ = [b_xin]
                          if L == 0 and c in (0, 1):
                              dump("xin%d" % c, xin[:, :, :].rearrange("p k t -> p (k t)"), [b_xin])
                          for cc_, xcol, hcol in ((0, 0, 0), (1, 129, 1), (2, 0, 2), (NCH - 1, 129, 3)):
                              if c == cc_:
                                  op("dve", lambda e: e.tensor_copy(xin[:, :, xcol], hal[:, :, hcol]), reads=[b_hal, b_xin], writes=[b_xin])
                          for oc in range(6):
                              ca, b_ca = cacc_r.get()
                              op("dve", lambda e: e.tensor_scalar(out=ca[:, :], in0=xin[:, oc, 0:128], scalar1=convw[:, oc * 3:oc * 3 + 1], scalar2=None, op0=ALU.mult),
                                 reads=xb_ + [b_small], writes=[b_ca])
                              op("dve", lambda e: e.scalar_tensor_tensor(out=ca[:, :], in0=xin[:, oc, 1:129], scalar=convw[:, oc * 3 + 1:oc * 3 + 2], op0=ALU.mult, in1=ca[:, :], op1=ALU.add),
                                 reads=xb_ + [b_small, b_ca], writes=[b_ca])
                              op("dve", lambda e: e.scalar_tensor_tensor(out=ca[:, :], in0=xin[:, oc, 2:130], scalar=convw[:, oc * 3 + 2:oc * 3 + 3], op0=ALU.mult, in1=ca[:, :], op1=ALU.add),
                                 reads=xb_ + [b_small, b_ca], writes=[b_ca])
                              if oc < 4:
                                  op("act", lambda e: e.activation(out=xact[:, oc, :], in_=ca[:, :], func=AF.Silu, bias=convb[:, oc:oc + 1]), reads=[b_ca, b_small], writes=[b_xa])
                              else:
                                  dst = CD_BT if oc == 4 else CD_CT
                                  op("act", lambda e: e.activation(out=cd[:, dst:dst + 128], in_=ca[:, :], func=AF.Silu, bias=convb[:, oc:oc + 1]), reads=[b_ca, b_small], writes=[b_cd])
                          p2, pb2 = psr.get()
                          pbf = p2[:, 0:320].bitcast(BF16)
                          fw.group("pe", [(lambda e, oc=oc: e.transpose(pbf[:, oc * 128:(oc + 1) * 128], xact[:, oc, :], ident_b)) for oc in range(4)]
                                   + [lambda e: e.transpose(pbf[:, 512:640], cd[:, CD_BT:CD_BT + 128], ident_b)],
                                   reads=[b_xa, b_cd, b_cb], writes=[pb2])
                          op("dve", lambda e: e.tensor_copy(cd[:, CD_XS:CD_XS + 512], pbf[:, 0:512]), reads=[pb2], writes=[b_cd])
                          op("dve", lambda e: e.tensor_copy(cd[:, CD_BTOK:CD_BTOK + 128], pbf[:, 512:640]), reads=[pb2], writes=[b_cd])
                          for d in range(2):
                              xo = CD_XF if d == 0 else CD_XB
                              op("dve" if d == 0 else "pool", lambda e: e.tensor_tensor(out=cd[:, xo:xo + 512].rearrange("p (h n) -> p h n", h=8),
                                                                                       in0=cd[:, CD_XS:CD_XS + 512].rearrange("p (h n) -> p h n", h=8),
                                                                                       in1=dtt[:, d * 8:d * 8 + 8].unsqueeze(2).to_broadcast([128, 8, 64]), op=ALU.mult),
                                 reads=[b_cd, b_dt], writes=[b_cd])
                          if c == 2:
                              for d in range(2):
                                  op("dve", lambda e: e.tensor_copy(S_ctx[d][:, :, :], S_run[d][:, :, :]), reads=[b_S[d]], writes=[b_Sctx[d]])
                                  op("pool", lambda e: e.memset(S_run[d][:, :, :], 0.0), writes=[b_S[d]])
                              op("pool", lambda e: e.memset(Lacc[:, :], 0.0), writes=[b_Lacc])
                          for d in range(2):
                              scl, b_scl = small_stats(c, d)
                              if d == 1:
                                  op("act", lambda e: e.activation(out=eL[:, :], in_=Lacc[:, 12:24], func=AF.Exp), reads=[b_Lacc], writes=[b_eL])
                              slots1 = []
                              for m in range(6):
                                  p_, pb_ = psr.get()
                                  slots1.append((p_[:, 0:128], pb_))
                              st_out = states_batch(cd, b_cd, scl, b_scl, d, slots1)
                              for m in range(6):
                                  pt, pb = st_out[m]
                                  if d == 0:
                                      upd_state(S_run[0], b_S[0], scl, b_scl, 24, m, pt, pb)
                                  else:
                                      for half in range(2):
                                          r0, r1 = half * 64, half * 64 + 64
                                          u = 2 * m + half
                                          op("dve", lambda e: e.scalar_tensor_tensor(out=S_run[1][r0:r1, m, r0:r1], in0=pt[r0:r1, r0:r1], scalar=eL[r0:r1, u:u + 1],
                                                                                      op0=ALU.mult, in1=S_run[1][r0:r1, m, r0:r1], op1=ALU.add),
                                             reads=[pb, b_eL, b_S[1]], writes=[b_S[1]])
                              op("dve", lambda e: e.tensor_tensor(out=Lacc[:, d * 12:(d + 1) * 12], in0=Lacc[:, d * 12:(d + 1) * 12], in1=scl[:, 48:60], op=ALU.add),
                                 reads=[b_Lacc, b_scl], writes=[b_Lacc])
                          fw.dma("sp", cd_dram[c], cd[:, :], reads=[b_cd], writes=[b_cdd[c]], chanbuf=b_cd)
                          if L == 0 and c in (0, 2):
                              dump("cd%d" % c, cd[:, :], [b_cd])

                      if L == 0:
                          dump("la", la_all[:, :, :].rearrange("p c n -> p (c n)"), b_la)
                          dump("xbcB", xbc_dram[:, 0, 0:260], b_xbc)
                          dump("sagg0", S_run[0][:, :, :].rearrange("p c n -> p (c n)"), [b_S[0]])
                          dump("sagg1", S_run[1][:, :, :].rearrange("p c n -> p (c n)"), [b_S[1]])
                          dump("sctx0", S_ctx[0][:, :, :].rearrange("p c n -> p (c n)"), [b_Sctx[0]])
                          dump("sctx1", S_ctx[1][:, :, :].rearrange("p c n -> p (c n)"), [b_Sctx[1]])
                          stop_at("sw1")
                      b_ss = Buf(); b_sa = Buf()
                      for d in range(2):
                          fw.dma("sp", st_send[:, d * 768:(d + 1) * 768], S_run[d][:, :, :].rearrange("p m n -> p (m n)"), reads=[b_S[d]], writes=[b_ss])
                      fw.dma("sp", st_send[:, 1536:1560], Lacc[:, :], reads=[b_Lacc], writes=[b_ss])
                      fw.cc("AllGather", st_send.ap().opt(), st_all.ap().opt(), GROUPS, [b_ss], [b_sa], b_sa)
                      S0 = S_ctx
                      for d in range(2):
                          order = range(4) if d == 0 else range(3, -1, -1)
                          for r in order:
                              mcol = (2 + r) if d == 0 else (6 + r)
                              fw.dma("sp", sr_t[:, :], st_all[r * 128:(r + 1) * 128, :], reads=[b_sa], writes=[b_sr])
                              op("act", lambda e: e.activation(out=dm_t[:, :], in_=sr_t[:, 1536 + d * 12:1548 + d * 12], func=AF.Exp), reads=[b_sr], writes=[b_dm])
                              op("dve", lambda e: e.tensor_scalar(out=dm_t[:, :], in0=dm_t[:, :], scalar1=-1.0, scalar2=sel[:, mcol:mcol + 1], op0=ALU.add, op1=ALU.mult),
                                 reads=[b_dm, b_sel], writes=[b_dm])
                              op("dve", lambda e: e.tensor_scalar(out=dm_t[:, :], in0=dm_t[:, :], scalar1=1.0, scalar2=None, op0=ALU.add), reads=[b_dm], writes=[b_dm])
                              op("dve", lambda e: e.tensor_scalar(out=sr_t[:, d * 768:(d + 1) * 768], in0=sr_t[:, d * 768:(d + 1) * 768], scalar1=sel[:, mcol:mcol + 1], scalar2=None, op0=ALU.mult),
                                 reads=[b_sr, b_sel], writes=[b_sr])
                              for m in range(6):
                                  upd_state(S0[d], b_Sctx[d], dm_t, b_dm, 0, m, sr_t[:, d * 768 + m * 128:d * 768 + (m + 1) * 128], b_sr)

                      SCS = [(PS[4][:, 256:384], Buf()), (PS[4][:, 384:512], Buf()), (PS[6][:, 256:384], Buf()), (PS[6][:, 384:512], Buf()),
                             (PS[0][:, 256:384], Buf()), (PS[1][:, 256:384], Buf())]

                      def load_cd(c):
                          cd, b_cd = cd_r.get()
                          fw.dma("sp", cd[:, :], cd_dram[c], reads=[b_cdd[c]], writes=[b_cd])
                          return cd, b_cd

                      def y_step(c, d, pre, c_next):
                          cd, b_cd = pre
                          nxt = load_cd(c_next) if c_next is not None else None
                          scl, b_scl = small_stats(c, d)
                          la_c = la_all[:, c, :]
                          yA = (PS[6], PS[7]); yAb = (PSB[6], PSB[7])
                          yB = (PS[4], PS[5]); yBb = (PSB[4], PSB[5])

                          def ycols(ps2, u0, n):
                              c0 = u0 * 64
                              if c0 < 256:
                                  return ps2[0][:, c0:c0 + n]
                              return ps2[1][:, c0 - 256:c0 - 256 + n]
                          qk_cache = {}
                          pendA = []

                          def emitA(u_, scT_, b_scT_):
                              m_, half_ = u_ // 2, u_ % 2
                              _, v_pair = kv_pair(cd, m_, d)
                              yb_ = yAb[0] if u_ < 4 else yAb[1]
                              op("pe", lambda e: e.matmul(ycols(yA, u_, 64), scT_[:, :], v_pair[:, half_ * 64:half_ * 64 + 64], start=True, stop=True), reads=[b_scT_, b_cd], writes=[yb_])
                          for u in range(12):
                              m, half = u // 2, u % 2
                              r0, r1 = half * 64, half * 64 + 64
                              pm, pbm = Ring(list(zip(PS[0:4], PSB[0:4]))).items[u % 2]
                              fw.group("pe", [lambda e: e.matmul(pm[:, 0:128], la_hl[:, c, d * 12 + u:d * 12 + u + 1].to_broadcast([128, 128]), tri_bf[d], start=True, stop=False),
                                              lambda e: e.matmul(pm[:, 0:128], la_hl[:, c, 24 + d * 12 + u:24 + d * 12 + u + 1].to_broadcast([128, 128]), tri_bf[d], start=False, stop=True)],
                                       reads=[b_la[c], b_cb], writes=[pbm])
                              dec, b_dec = dec_r.get()
                              op("act", lambda e: e.activation(out=dec[:, :], in_=pm[:, 0:128], func=AF.Exp, bias=scl[:, u:u + 1]), reads=[pbm, b_scl], writes=[b_dec])
                              qkey = u if u < 4 else 4 + half
                              if qkey not in qk_cache:
                                  pq, pbq = (PS[2], PSB[2]) if (len(qk_cache) % 2 == 0) else (PS[3], PSB[3])
                                  if u < 4:
                                      k_ap = cd[r0:r1, CD_KT + m * 128:CD_KT + (m + 1) * 128]
                                      q_ap = cd[r0:r1, CD_QT + m * 128:CD_QT + (m + 1) * 128]
                                  else:
                                      k_ap = cd[r0:r1, CD_BT:CD_BT + 128]
                                      q_ap = cd[r0:r1, CD_CT:CD_CT + 128]
                                  op("pe", lambda e: e.matmul(pq[:, 0:128], k_ap, q_ap, start=True, stop=True), reads=[b_cd], writes=[pbq])
                                  qkm, b_qkm = qkm_r.get()
                                  msk = triF if d == 0 else (triBs if u < 4 else triB)
                                  op("dve", lambda e: e.tensor_tensor(out=qkm[:, :], in0=pq[:, 0:128], in1=msk, op=ALU.mult), reads=[pbq, b_cf], writes=[b_qkm])
                                  qk_cache[qkey] = (qkm, b_qkm)
                              qkm, b_qkm = qk_cache[qkey]
                              scT, b_scT = scT_r.get()
                              op("dve", lambda e: e.scalar_tensor_tensor(out=scT[:, :], in0=dec[:, :], scalar=1.0, op0=ALU.min, in1=qkm[:, :], op1=ALU.mult),
                                 reads=[b_dec, b_qkm], writes=[b_scT])
                              pendA.append((u, scT, b_scT))
                              if len(pendA) > 2:
                                  emitA(*pendA.pop(0))
                          while pendA:
                              emitA(*pendA.pop(0))
                          for m in range(6):
                              q_ap = cd[:, CD_QT + m * 128:CD_QT + (m + 1) * 128] if m < 2 else cd[:, CD_CT:CD_CT + 128]
                              yb_ = yBb[0] if m < 2 else yBb[1]
                              op("pe", lambda e: e.matmul(ycols(yB, 2 * m, 128), q_ap, Sbd[d][:, m, :], start=True, stop=True), reads=[b_cd, b_Sbd[d]], writes=[yb_])
                          st_out = states_batch(cd, b_cd, scl, b_scl, d, SCS)
                          for m in range(6):
                              pt, pb = st_out[m]
                              upd_state(S_run[d], b_S[d], scl, b_scl, 24, m, pt, pb)
                          op("act", lambda e: e.activation(out=Sbd[d][:, :, :], in_=S_run[d][:, :, :], func=AF.Copy), reads=[b_S[d]], writes=[b_Sbd[d]])
                          yAs, b_yAs = yA_r.get()
                          yts, b_yts = yt_r.get()
                          op("act", lambda e: e.activation(out=yAs[:, 0:256], in_=yA[0][:, 0:256], func=AF.Copy), reads=[yAb[0]], writes=[b_yAs])
                          op("act", lambda e: e.activation(out=yAs[:, 256:768], in_=yA[1][:, 0:512], func=AF.Copy), reads=[yAb[1]], writes=[b_yAs])
                          op("dve", lambda e: e.tensor_tensor(out=yts[:, 0:256].rearrange("p (h n) -> p h n", h=4), in0=yB[0][:, 0:256].rearrange("p (h n) -> p h n", h=4),
                                                               in1=scl[:, 12:16].unsqueeze(2).to_broadcast([128, 4, 64]), op=ALU.mult), reads=[yBb[0], b_scl], writes=[b_yts])
                          op("dve", lambda e: e.tensor_tensor(out=yts[:, 256:768].rearrange("p (h n) -> p h n", h=8), in0=yB[1][:, 0:512].rearrange("p (h n) -> p h n", h=8),
                                                               in1=scl[:, 16:24].unsqueeze(2).to_broadcast([128, 8, 64]), op=ALU.mult), reads=[yBb[1], b_scl], writes=[b_yts])
                          op("pool", lambda e: e.tensor_tensor(out=yts[:, :], in0=yts[:, :], in1=yAs[:, :], op=ALU.add), reads=[b_yts, b_yAs], writes=[b_yts])
                          return cd, b_cd, yts, b_yts, nxt

                      psr_full = psr
                      psr = Ring(list(zip(PS[0:2], PSB[0:2])))
                      for d in range(2):
                          seqs = [[0, 1], list(range(2, NCH))] if d == 0 else [[1, 0], list(range(NCH - 1, 1, -1))]
                          flat = seqs[0] + seqs[1]
                          pre = load_cd(flat[0])
                          for si, seq in enumerate(seqs):
                              if si == 0:
                                  op("pool", lambda e: e.memset(S_run[d][:, :, :], 0.0), writes=[b_S[d]])
                              else:
                                  op("dve", lambda e: e.tensor_copy(S_run[d][:, :, :], S0[d][:, :, :]), reads=[b_Sctx[d]], writes=[b_S[d]])
                              op("act", lambda e: e.activation(out=Sbd[d][:, :, :], in_=S_run[d][:, :, :], func=AF.Copy), reads=[b_S[d]], writes=[b_Sbd[d]])
                              for c in seq:
                                  fi = flat.index(c)
                                  if d == 1 and not (last and c < 2):
                                      yfs, b_yfs = yf_r.get()
                                      fw.dma("sp", yfs[:, :], yf_dram[c], reads=[b_yfd[c]], writes=[b_yfs])
                                  cd, b_cd, yts, b_yts, pre = y_step(c, d, pre, flat[fi + 1] if fi + 1 < len(flat) else None)
                                  if d == 0:
                                      fw.dma("pool", yf_dram[c], yts[:, :], reads=[b_yts], writes=[b_yfd[c]], chanbuf=b_yts)
                                      continue
                                  if last and c < 2:
                                      continue
                                  op("pool", lambda e: e.tensor_tensor(out=yts[:, :], in0=yts[:, :], in1=yfs[:, :], op=ALU.add), reads=[b_yts, b_yfs], writes=[b_yts])
                                  gt, b_gt = gt_r.get()
                                  gst, b_gst = gst_r.get()
                                  mo, b_mo = mo_r.get()
                                  yr = yts[:, 0:256].rearrange("p (h n) -> p h n", h=4)
                                  gr = gt[:, 0:256].rearrange("p (h n) -> p h n", h=4)
                                  op("dve", lambda e: e.tensor_reduce(out=gst[:, 0:4], in_=yr, op=ALU.add, axis=AX.X), reads=[b_yts], writes=[b_gst])
                                  op("act", lambda e: e.activation(out=gt[:, 0:256], in_=yts[:, 0:256], func=AF.Square), reads=[b_yts], writes=[b_gt])
                                  op("dve", lambda e: e.tensor_reduce(out=gst[:, 4:8], in_=gr, op=ALU.add, axis=AX.X), reads=[b_gt], writes=[b_gst])
                                  op("dve", lambda e: e.tensor_scalar(out=gst[:, 0:8], in0=gst[:, 0:8], scalar1=1.0 / 64, scalar2=None, op0=ALU.mult), reads=[b_gst], writes=[b_gst])
                                  op("dve", lambda e: e.tensor_tensor(out=gst[:, 8:12], in0=gst[:, 0:4], in1=gst[:, 0:4], op=ALU.mult), reads=[b_gst], writes=[b_gst])
                                  op("dve", lambda e: e.tensor_tensor(out=gst[:, 8:12], in0=gst[:, 4:8], in1=gst[:, 8:12], op=ALU.subtract), reads=[b_gst], writes=[b_gst])
                                  op("act", lambda e: e.activation(out=gst[:, 8:12], in_=gst[:, 8:12], func=AF.Sqrt, bias=EPS), reads=[b_gst], writes=[b_gst])
                                  op("dve", lambda e: e.reciprocal(out=gst[:, 8:12], in_=gst[:, 8:12]), reads=[b_gst], writes=[b_gst])
                                  op("dve", lambda e: e.tensor_tensor(out=gr, in0=yr, in1=gst[:, 0:4].unsqueeze(2).to_broadcast([128, 4, 64]), op=ALU.subtract), reads=[b_yts, b_gst], writes=[b_gt])
                                  op("dve", lambda e: e.tensor_tensor(out=gr, in0=gr, in1=gst[:, 8:12].unsqueeze(2).to_broadcast([128, 4, 64]), op=ALU.mult), reads=[b_gt, b_gst], writes=[b_gt])
                                  op("dve", lambda e: e.tensor_tensor(out=mo[:, 0:256], in0=gt[:, 0:256], in1=cd[:, CD_SG:CD_SG + 256], op=ALU.mult), reads=[b_gt, b_cd], writes=[b_mo])
                                  op("pool", lambda e: e.tensor_tensor(out=gt[:, 256:768], in0=cd[:, CD_XS:CD_XS + 512], in1=dskip_b, op=ALU.mult), reads=[b_cd, b_small], writes=[b_gt])
                                  op("pool", lambda e: e.tensor_tensor(out=gt[:, 256:768], in0=gt[:, 256:768], in1=yts[:, 256:768], op=ALU.add), reads=[b_gt, b_yts], writes=[b_gt])
                                  op("dve", lambda e: e.tensor_tensor(out=gt[:, 256:768], in0=gt[:, 256:768], in1=cd[:, CD_SZ:CD_SZ + 512], op=ALU.mult), reads=[b_gt, b_cd], writes=[b_gt])
                                  op("act", lambda e: e.activation(out=yts[:, 256:768], in_=gt[:, 256:768], func=AF.Square, accum_out=gst[:, 16:17]), reads=[b_gt], writes=[b_yts, b_gst])
                                  op("act", lambda e: e.activation(out=gst[:, 17:18], in_=gst[:, 16:17], func=AF.Sqrt, scale=1.0 / 512, bias=EPS), reads=[b_gst], writes=[b_gst])
                                  op("dve", lambda e: e.reciprocal(out=gst[:, 17:18], in_=gst[:, 17:18]), reads=[b_gst], writes=[b_gst])
                                  op("dve", lambda e: e.scalar_tensor_tensor(out=mo[:, 256:768], in0=gt[:, 256:768], scalar=gst[:, 17:18], op0=ALU.mult, in1=snw_b, op1=ALU.mult),
                                     reads=[b_gt, b_gst, b_small], writes=[b_mo])
                                  for half3 in range(2):
                                      p2, pb2 = psr.get()
                                      pbf = p2[:, 0:192].bitcast(BF16)
                                      fw.group("pe", [(lambda e, i=i: e.transpose(pbf[:, i * 128:(i + 1) * 128], mo[:, (half3 * 3 + i) * 128:(half3 * 3 + i + 1) * 128], ident_b)) for i in range(3)],
                                               reads=[b_mo, b_cb], writes=[pb2])
                                      eng = "dve"
                                      if eng == "act":
                                          op("act", lambda e: e.activation(out=mixT[:, 2 + half3 * 3:5 + half3 * 3, c * 128:(c + 1) * 128], in_=pbf[:, 0:384].rearrange("p (c t) -> p c t", c=3), func=AF.Copy),
                                             reads=[pb2], writes=[b_mix[c]])
                                      else:
                                          op("dve", lambda e: e.tensor_copy(mixT[:, 2 + half3 * 3:5 + half3 * 3, c * 128:(c + 1) * 128], pbf[:, 0:384].rearrange("p (c t) -> p c t", c=3)),
                                             reads=[pb2], writes=[b_mix[c]])
                      psr = psr_full
              if L == 0:
                  dump("mix", mixT[:, :, :].rearrange("p k t -> p (k t)"), b_mix)
                  stop_at("scan")
              fw.barrier()
              blks = [(bi, t0, W, v) for bi, (t0, W, v) in enumerate(BLOCKS) if not (last and v == 1)]
              with ExitStack() as s3:
                  xb3 = sb(s3, "xb3", [128, 8, 512], F32); b_xb3 = Buf()
                  wo = sb(s3, "wo", [128, 8, D], BF16); b_wo = Buf()
                  fw.dma("pool", wo[:, :, :], w_out[L].rearrange("(k p) n -> p k n", p=128), writes=[b_wo])
                  wk = {"sq": Ring([(sb(s3, "sq3_%d" % i, [128, 8, 512], BF16), Buf()) for i in range(1)]),
                        "rs": Ring([(sb(s3, "rs3_%d" % i, [128, 512], F32), Buf()) for i in range(2)]),
                        "tmp": Ring([(sb(s3, "tmp3_%d" % i, [128, 512], F32), Buf()) for i in range(2)])}
                  for bi, t0, W, v in blks:
                      fw.dma("sp", xb3[:, :, 0:W], x_src[:, t0:t0 + W].rearrange("(k p) t -> p k t", p=128), writes=[b_xb3])
                      mb = [b_mix[c] for c in range(t0 // 128, (t0 + W) // 128)]
                      for oc in range(8):
                          pt, pb = psr.get()
                          fw.group("pe", [(lambda e, k=k: e.matmul(pt[:, 0:W], wo[:, k, oc * 128:(oc + 1) * 128], mixT[:, k, t0:t0 + W], start=(k == 0), stop=(k == 7))) for k in range(8)],
                                   reads=[b_wo] + mb, writes=[pb])
                          op("dve", lambda e: e.scalar_tensor_tensor(out=xb3[:, oc, 0:W], in0=pt[:, 0:W], scalar=mv[:, 16 + oc, v:v + 1], op0=ALU.mult, in1=xb3[:, oc, 0:W], op1=ALU.add),
                             reads=[pb, b_mv, b_xb3], writes=[b_xb3])
                      norm_mod(xb3[:, :, 0:W], b_xb3, t0, W, v, 8, 24, wk, b_hx[bi])
                      fw.dma("sp", xres[:, t0:t0 + W].rearrange("(k p) t -> p k t", p=128), xb3[:, :, 0:W], reads=[b_xb3], writes=[b_xres], chanbuf=b_xb3)
              if L == 0:
                  dump("xmid", xres, [b_xres])
                  dump("hx2", hxT[:, :, :].rearrange("p k t -> p (k t)"), b_hx)
                  stop_at("s3a")
              lm.close()
              fw.barrier()
              with ExitStack() as s3:
                  xr = sb(s3, "xr", [128, 8, NT], F32); b_xr = [Buf() for _ in BLOCKS]
                  wk = {"sq": Ring([(sb(s3, "sq4_%d" % i, [128, 8, 512], BF16), Buf()) for i in range(1)]),
                        "rs": Ring([(sb(s3, "rs4_%d" % i, [128, 512], F32), Buf()) for i in range(1)])}
                  for bi, t0, W, v in blks:
                      fw.dma("sp", xr[:, :, t0:t0 + W], xres[:, t0:t0 + W].rearrange("(k p) t -> p k t", p=128), reads=[b_xres], writes=[b_xr[bi]])
                  is_moe = (L % 2 == 1)
                  if not is_moe:
                      units = [(None, f0, min(4, 22 - f0)) for f0 in range(0, 22, 4)]
                      wg_src, wu_src, wd_src = ffn_wg, ffn_wu, ffn_wd
                  else:
                      units = [(e_, f0, min(4, 11 - f0)) for e_ in range(8) for f0 in (0, 4, 8)]
                  wgu_r = Ring([(sb(s3, "wgu%d" % i, [128, 2, 8, 512], BF16), Buf()) for i in range(2)])
                  wd_r = Ring([(sb(s3, "wdn%d" % i, [128, 4, D], BF16), Buf()) for i in range(2)])
                  hid = sb(s3, "hid", [128, 4, 512], BF16); b_hid = Buf()
                  sg_r = Ring([(sb(s3, "sg%d" % i, [128, 512], BF16), Buf()) for i in range(2)])
                  ug_r = Ring([(sb(s3, "ug%d" % i, [128, 512], BF16), Buf()) for i in range(2)])
                  Gt = None
                  if is_moe:
                      rt = sb(s3, "rt", [128, 8, 8], BF16); b_rt = Buf()
                      fw.dma("pool", rt[:, :, :], router_in.rearrange("(k p) n -> p k n", p=128), writes=[b_rt])
                      gates = sb(s3, "gates", [128, NCH, 8], F32); b_gates = Buf()
                      G = sb(s3, "G", [128, 512], F32); b_G = Buf()
                      gb_r = Ring([(sb(s3, "gb%d" % i, [128, 128], F32), Buf()) for i in range(2)])
                      gs_r = Ring([(sb(s3, "gs%d" % i, [128, 32], F32), Buf()) for i in range(2)])
                      for bi, t0, W, v in blks:
                          for ti in range(W // 128):
                              c = (t0 // 128) + ti
                              tt = c * 128
                              pt, pb = psr.get()
                              fw.group("pe", [(lambda e, k=k: e.matmul(pt[:, 0:8], hxT[:, k, tt:tt + 128], rt[:, k, :], start=(k == 0), stop=(k == 7))) for k in range(8)],
                                       reads=[b_rt, b_hx[bi]], writes=[pb])
                              gs, b_gs = gs_r.get()
                              op("dve", lambda e: e.tensor_copy(gs[:, 0:8], pt[:, 0:8]), reads=[pb], writes=[b_gs])
                              op("dve", lambda e: e.max(out=gs[:, 8:16], in_=gs[:, 0:8]), reads=[b_gs], writes=[b_gs])
                              op("dve", lambda e: e.tensor_scalar(out=gs[:, 16:17], in0=gs[:, 8:9], scalar1=-1.0, scalar2=None, op0=ALU.mult), reads=[b_gs], writes=[b_gs])
                              op("act", lambda e: e.activation(out=gs[:, 24:32], in_=gs[:, 0:8], func=AF.Exp, bias=gs[:, 16:17]), reads=[b_gs], writes=[b_gs])
                              op("dve", lambda e: e.scalar_tensor_tensor(out=gs[:, 24:32], in0=gs[:, 0:8], scalar=gs[:, 9:10], op0=ALU.is_ge, in1=gs[:, 24:32], op1=ALU.mult), reads=[b_gs], writes=[b_gs])
                              op("dve", lambda e: e.tensor_reduce(out=gs[:, 17:18], in_=gs[:, 24:32], op=ALU.add, axis=AX.X), reads=[b_gs], writes=[b_gs])
                              op("dve", lambda e: e.reciprocal(out=gs[:, 17:18], in_=gs[:, 17:18]), reads=[b_gs], writes=[b_gs])
                              op("dve", lambda e: e.tensor_scalar(out=gates[:, c, :], in0=gs[:, 24:32], scalar1=gs[:, 17:18], scalar2=None, op0=ALU.mult), reads=[b_gs], writes=[b_gates])
                  for (e_, f0, F) in units:
                      wgu, b_wgu = wgu_r.get()
                      wdn, b_wdn = wd_r.get()
                      if e_ is None:
                          gsrc = ffn_wg[:, f0 * 128:(f0 + F) * 128]; usrc = ffn_wu[:, f0 * 128:(f0 + F) * 128]; dsrc = ffn_wd[f0 * 128:(f0 + F) * 128, :]
                      else:
                          gsrc = moe_wg[e_, :, f0 * 128:(f0 + F) * 128]; usrc = moe_wu[e_, :, f0 * 128:(f0 + F) * 128]; dsrc = moe_wd[e_, f0 * 128:(f0 + F) * 128, :]
                      fw.dma("pool", wgu[:, 0, :, 0:F * 128], gsrc.rearrange("(k p) n -> p k n", p=128), writes=[b_wgu])
                      fw.dma("pool", wgu[:, 1, :, 0:F * 128], usrc.rearrange("(k p) n -> p k n", p=128), writes=[b_wgu])
                      fw.dma("pool", wdn[:, 0:F, :], dsrc.rearrange("(f p) n -> p f n", p=128), writes=[b_wdn])
                      for bi, t0, W, v in blks:
                          if e_ is not None:
                              pt, pb = psr.get()
                              for ti in range(W // 128):
                                  c = (t0 // 128) + ti
                                  gb, b_gb = gb_r.get()
                                  op("dve", lambda e: e.tensor_scalar(out=gb[:, :], in0=ones_f, scalar1=gates[:, c, e_:e_ + 1], scalar2=None, op0=ALU.mult), reads=[b_cf, b_gates], writes=[b_gb])
                                  op("pe", lambda e: e.matmul(pt[:, ti * 128:(ti + 1) * 128], gb[:, :], ident_f, start=True, stop=True), reads=[b_gb, b_cf], writes=[pb])
                              op("act", lambda e: e.activation(out=G[:, 0:W], in_=pt[:, 0:W], func=AF.Copy), reads=[pb], writes=[b_G])
                          for f in range(F):
                              pg, pbg = psr.get()
                              pu, pbu = psr.get()
                              fw.group("pe", [(lambda e, k=k: e.matmul(pg[:, 0:W], wgu[:, 0, k, f * 128:(f + 1) * 128], hxT[:, k, t0:t0 + W], start=(k == 0), stop=(k == 7))) for k in range(8)],
                                       reads=[b_wgu, b_hx[bi]], writes=[pbg])
                              fw.group("pe", [(lambda e, k=k: e.matmul(pu[:, 0:W], wgu[:, 1, k, f * 128:(f + 1) * 128], hxT[:, k, t0:t0 + W], start=(k == 0), stop=(k == 7))) for k in range(8)],
                                       reads=[b_wgu, b_hx[bi]], writes=[pbu])
                              sg, b_sg = sg_r.get()
                              op("act", lambda e: e.activation(out=sg[:, 0:W], in_=pg[:, 0:W], func=AF.Silu), reads=[pbg], writes=[b_sg])
                              if e_ is None:
                                  op("dve", lambda e: e.tensor_tensor(out=hid[:, f, 0:W], in0=pu[:, 0:W], in1=sg[:, 0:W], op=ALU.mult), reads=[pbu, b_sg], writes=[b_hid])
                              else:
                                  ug, b_ug = ug_r.get()
                                  op("dve", lambda e: e.tensor_tensor(out=ug[:, 0:W], in0=pu[:, 0:W], in1=G[:, 0:W], op=ALU.mult), reads=[pbu, b_G], writes=[b_ug])
                                  op("pool", lambda e: e.tensor_tensor(out=hid[:, f, 0:W], in0=ug[:, 0:W], in1=sg[:, 0:W], op=ALU.mult), reads=[b_ug, b_sg], writes=[b_hid])
                          for oc in range(8):
                              pt, pb = psr.get()
                              fw.group("pe", [(lambda e, f=f: e.matmul(pt[:, 0:W], wdn[:, f, oc * 128:(oc + 1) * 128], hid[:, f, 0:W], start=(f == 0), stop=(f == F - 1))) for f in range(F)],
                                       reads=[b_wdn, b_hid], writes=[pb])
                              op("dve", lambda e: e.scalar_tensor_tensor(out=xr[:, oc, t0:t0 + W], in0=pt[:, 0:W], scalar=mv[:, 40 + oc, v:v + 1], op0=ALU.mult, in1=xr[:, oc, t0:t0 + W], op1=ALU.add),
                                 reads=[pb, b_mv, b_xr[bi]], writes=[b_xr[bi]])
                  b_out = Buf()
                  if not last:
                      for bi, t0, W, v in blks:
                          fw.dma("sp", xres[:, t0:t0 + W].rearrange("(k p) t -> p k t", p=128), xr[:, :, t0:t0 + W], reads=[b_xr[bi]], writes=[b_xres], chanbuf=b_xr[bi])
                      fw.finish([b_xres], eng="sp")
                  else:
                      for bi, t0, W, v in blks:
                          sq, b_sq = wk["sq"].get()
                          op("act", lambda e: e.activation(out=sq[:, :, 0:W], in_=xr[:, :, t0:t0 + W], func=AF.Square), reads=[b_xr[bi]], writes=[b_sq])
                          pt, pb = psr.get()
                          fw.group("pe", [(lambda e, k=k: e.matmul(pt[:, 0:W], ones_b, sq[:, k, 0:W], start=(k == 0), stop=(k == 7))) for k in range(8)], reads=[b_sq, b_cb], writes=[pb])
                          rs, b_rs = wk["rs"].get()
                          op("act", lambda e: e.activation(out=rs[:, 0:W], in_=pt[:, 0:W], func=AF.Sqrt, scale=1.0 / D, bias=EPS), reads=[pb], writes=[b_rs])
                          op("dve", lambda e: e.reciprocal(out=rs[:, 0:W], in_=rs[:, 0:W]), reads=[b_rs], writes=[b_rs])
                          for k in range(8):
                              op("dve", lambda e: e.scalar_tensor_tensor(out=xr[:, k, t0:t0 + W], in0=xr[:, k, t0:t0 + W], scalar=fnw[:, k:k + 1], op0=ALU.mult, in1=rs[:, 0:W], op1=ALU.mult),
                                 reads=[b_xr[bi], b_fnw, b_rs], writes=[b_xr[bi]])
                          fw.dma("sp", outT[:, t0 - TC:t0 - TC + W].rearrange("(k p) t -> p k t", p=128), xr[:, :, t0:t0 + W], reads=[b_xr[bi]], writes=[b_out], chanbuf=b_xr[bi])
                      fw.finish([b_out], eng="sp")
        except _Stop:
            stopped = True
        fw.finish(dbg_bufs, eng="sp")
        print("instructions:", fw.ninstr, "dma channels:", fw.nchan)
        if stopped:
            raise _StopOuter()
    return nc


def _perm_heads(a, axis):
    a = np.moveaxis(a, axis, -1)
    sh = a.shape
    a = a.reshape(sh[:-1] + (8, sh[-1] // 8))[..., PI8, :].reshape(sh)
    return np.ascontiguousarray(np.moveaxis(a, -1, axis))


def _host_inputs(inp):
    f32 = np.float32
    g = {k: np.asarray(v) for k, v in inp.items()}
    x, c, ctx, c_ctx = g["x"], g["c"], g["ctx"], g["c_ctx"]
    w_in = g["w_in"].copy()
    w_in[:, :, 1440:1952] = _perm_heads(g["w_in"][:, :, 1440:1952], 2)
    w_in[:, :, 1952:2464] = _perm_heads(g["w_in"][:, :, 1952:2464], 2)
    w_in[:, :, 2720:2728] = g["w_in"][:, :, 2720:2728][:, :, PI8]
    w_in[:, :, 2728:2736] = g["w_in"][:, :, 2728:2736][:, :, PI8]
    w_out = g["w_out"].copy()
    w_out[:, 512:1024, :] = _perm_heads(g["w_out"][:, 512:1024, :], 1)
    perm = np.concatenate([np.arange(8, 16), np.arange(0, 8), np.arange(24, 32), np.arange(16, 24)])
    w_qb = g["mla_w_qb"]
    w_qbrot = np.zeros_like(w_qb)
    for h in range(4):
        w_qbrot[:, :, h * 96 + 64:h * 96 + 96] = w_qb[:, :, h * 96 + 64 + perm]
    conv_w = g["ssd_conv_w"].copy()
    conv_w[:, :, 0:512] = _perm_heads(g["ssd_conv_w"][:, :, 0:512], 2)
    conv_b = g["ssd_conv_b"].copy()
    conv_b[:, 0:512] = _perm_heads(g["ssd_conv_b"][:, 0:512], 1)
    bc = lambda v: np.ascontiguousarray(np.broadcast_to(v[:, None, :], (v.shape[0], 128, v.shape[1]))).astype(f32)
    fm = lambda v: np.ascontiguousarray(v.reshape(-1, 128).T)
    nw = np.stack([np.concatenate([fm(g["norm1_w"][l]), fm(g["norm2_w"][l])], axis=1) for l in range(2)]).astype(f32)
    convw = np.stack([np.stack([fm(conv_w[l, t]) for t in range(3)], axis=2).reshape(128, 18) for l in range(2)]).astype(f32)
    convb = np.stack([fm(conv_b[l]) for l in range(2)]).astype(f32)
    dtb = np.concatenate([g["ssd_dt_bias"][:, 0][:, PI8], g["ssd_dt_bias"][:, 1][:, PI8]], axis=1)
    alog = np.concatenate([g["ssd_a_log"][:, 0][:, PI8], g["ssd_a_log"][:, 1][:, PI8]], axis=1)
    rdl = np.concatenate([g["ret_decay_logit"][:, 0], g["ret_decay_logit"][:, 1]], axis=1)
    dskip = np.repeat(g["ssd_d"][:, PI8], 64, axis=1)
    snw = _perm_heads(g["ssd_norm_w"], 1)
    k = np.arange(128)
    cf = np.zeros((128, 6, 128), f32)
    cf[:, 0] = np.eye(128)
    cf[:, 1] = (k[:, None] <= k[None, :])
    cf[:, 2] = (k[:, None] >= k[None, :])
    cf[:, 3] = (k[:, None] > k[None, :])
    cf[:, 4] = 1.0
    cf[64, 5, 0:64] = 1.0
    cbf = np.zeros((128, 5, 128), f32)
    cbf[:, 0] = np.eye(128)
    cbf[:, 1] = 1.0
    for m in range(32):
        cbf[perm[m], 2, m] = 1.0
    cbf[:, 3] = cf[:, 1]
    cbf[:, 4] = cf[:, 2]
    cbf = cbf.astype(ml_dtypes.bfloat16)
    inv_freq = (10000.0 ** (-np.arange(0, 16, 2, dtype=f32) / 16)).astype(f32)
    sign = np.concatenate([-np.ones(8), np.ones(8), -np.ones(8), np.ones(8)]).astype(f32)
    shared = dict(mod_w=g["mod_w"], mod_b=g["mod_b"][:, None, :], w_in=w_in, w_out=w_out, w_qb=w_qb, w_qbrot=w_qbrot,
                  w_kb=g["mla_w_kb"], w_vb=g["mla_w_vb"], nw=nw, fnw=fm(g["final_norm_w"]).astype(f32),
                  qnw_b=bc(g["mla_q_norm_w"]), kvnw_b=bc(g["mla_kv_norm_w"]), convw=convw, convb=convb,
                  dtb_b=bc(dtb), alog_b=bc(alog), rdl_b=bc(rdl), dskip_b=bc(dskip), snw_b=bc(snw),
                  router=g["moe_router"][0], ffn_wg=g["ffn_w_gate"][0], ffn_wu=g["ffn_w_up"][0], ffn_wd=g["ffn_w_down"][0],
                  moe_wg=g["moe_w_gate"][0], moe_wu=g["moe_w_up"][0], moe_wd=g["moe_w_down"][0], cf32=cf, cbf16=cbf)
    shared = {k_: np.ascontiguousarray(v, dtype=(v.dtype if v.dtype == ml_dtypes.bfloat16 else f32)) for k_, v in shared.items()}
    maps = []
    for r in range(8):
        b, j = r // 4, r % 4
        xt = np.concatenate([ctx[b], x[b, j * TL:(j + 1) * TL]], axis=0)
        ccv = np.stack([fm(c[b]), fm(c_ctx)], axis=2).astype(f32)
        tg = (j * TL + np.arange(TL)).astype(f32)
        row = np.floor(tg / 64).astype(f32)
        col = (tg - row * 64).astype(f32)
        ang_r = row[:, None] * inv_freq
        ang_c = col[:, None] * inv_freq
        ang = np.concatenate([ang_r, ang_r, ang_c, ang_c], axis=1).astype(f32)
        cos32 = np.concatenate([np.ones((TC, 32), f32), np.cos(ang).astype(f32)], axis=0)
        sin32 = np.concatenate([np.zeros((TC, 32), f32), np.sin(ang).astype(f32) * sign], axis=0)
        ropeT = np.zeros((128, 2, NT), f32)
        for base in (0, 64):
            ropeT[base:base + 32, 0] = cos32.T
            ropeT[base:base + 32, 1] = sin32.T
        sel = np.zeros((128, 16), f32)
        if j > 0:
            sel[2 * (j - 1) + 1, 0] = 1.0
        if j < 3:
            sel[2 * (j + 1), 1] = 1.0
        for r2 in range(4):
            sel[:, 2 + r2] = 1.0 if r2 < j else 0.0
            sel[:, 6 + r2] = 1.0 if r2 > j else 0.0
        m = dict(shared)
        m.update(xT=np.ascontiguousarray(xt.T.astype(f32)), cc=np.ascontiguousarray(ccv), ropeT=ropeT, sel=sel)
        maps.append(m)
    return maps


_NC_CACHE = {}


def kernel(**inputs):
    maps = _host_inputs(inputs)
    if "nc" not in _NC_CACHE:
        _NC_CACHE["nc"] = build_program()
    res = run_bass_kernel_spmd(_NC_CACHE["nc"], maps, core_ids=list(range(8)))
    out = np.zeros((2, 4 * TL, D), np.float32)
    for r in range(8):
        b, j = r // 4, r % 4
        out[b, j * TL:(j + 1) * TL, :] = np.asarray(res.results[r]["outT"]).T
    return out
```

```python
import numpy as np
import ml_dtypes
from contextlib import ExitStack
import concourse.bass as bass
import concourse.mybir as mybir
from concourse.bass_utils import run_bass_kernel_spmd

F32 = mybir.dt.float32
BF16 = mybir.dt.bfloat16
AF = mybir.ActivationFunctionType
ALU = mybir.AluOpType
AX = mybir.AxisListType

D = 1024
TC = 256
TL = 2048
NT = TC + TL
NCH = NT // 128
BLOCKS = [(0, 256, 1), (256, 512, 0), (768, 512, 0), (1280, 512, 0), (1792, 512, 0)]
EPS = 1e-6
PI8 = [0, 4, 1, 5, 2, 6, 3, 7]
MLA_SCALE = 96 ** -0.5
CD_QT, CD_KT, CD_CT, CD_BT = 0, 256, 512, 640
CD_KTOK, CD_BTOK, CD_VTOK = 768, 1024, 1152
CD_XF, CD_XB, CD_XS, CD_SG, CD_SZ = 1408, 1920, 2432, 2944, 3200
CDW = 3712


STRICT = False


class Buf:
    __slots__ = ("w", "r", "chan")

    def __init__(self):
        self.w = None
        self.r = {}
        self.chan = None


class FW:
    def __init__(self, nc, es):
        self.nc = nc
        self.es = es
        self.E = {"pe": nc.tensor, "act": nc.scalar, "dve": nc.vector, "pool": nc.gpsimd, "sp": nc.sync}
        self.sems = {}
        self.cnt = {}
        self.waited = {e: {} for e in self.E}
        for e in self.E:
            self._newsem(e)
        self.nchan = 0
        self.ninstr = 0

    def _newsem(self, key):
        h = self.es.enter_context(self.nc.semaphore("s_%s" % (str(key).replace(" ", ""),)))
        self.sems[key] = h
        self.cnt[key] = 0
        return h

    def _wait(self, eng, key, val):
        if val <= 0:
            return
        w = self.waited[eng]
        if w.get(key, 0) >= val:
            return
        w[key] = val
        self.E[eng].wait_ge(self.sems[key], val)

    def deps(self, eng, reads, writes, is_dma=False):
        for b in reads:
            if b.w is not None:
                k, v = b.w
                if k != "pe" or eng != "pe" or is_dma:
                    self._wait(eng, k, v)
        for b in writes:
            if b.w is not None:
                k, v = b.w
                if k != eng or is_dma or (STRICT and eng != "pe"):
                    self._wait(eng, k, v)
            for k, v in b.r.items():
                if k != eng or is_dma or (STRICT and eng != "pe"):
                    self._wait(eng, k, v)

    def record(self, key, val, reads, writes):
        for b in reads:
            b.r[key] = val
        for b in writes:
            b.w = (key, val)
            b.r = {}

    def op(self, eng, fn, reads=(), writes=()):
        self.deps(eng, reads, writes)
        ins = fn(self.E[eng])
        self.cnt[eng] += 1
        ins.then_inc(self.sems[eng], 1)
        self.record(eng, self.cnt[eng], reads, writes)
        self.ninstr += 1
        return ins

    def group(self, eng, fns, reads=(), writes=()):
        self.deps(eng, reads, writes)
        ins = None
        for f in fns:
            ins = f(self.E[eng])
            self.ninstr += 1
        self.cnt[eng] += 1
        ins.then_inc(self.sems[eng], 1)
        self.record(eng, self.cnt[eng], reads, writes)

    def dma(self, q, out, in_, reads=(), writes=(), chanbuf=None, **kw):
        cb = chanbuf if chanbuf is not None else (writes[0] if writes else reads[0])
        if cb.chan is None:
            cb.chan = ("ch", self.nchan)
            self.nchan += 1
            self._newsem(cb.chan)
        ck = cb.chan
        self.deps(q, reads, writes, is_dma=True)
        self._wait(q, ck, self.cnt[ck])
        ins = self.E[q].dma_start(out=out, in_=in_, **kw)
        self.cnt[ck] += 16
        ins.then_inc(self.sems[ck], 16)
        self.record(ck, self.cnt[ck], reads, writes)
        self.ninstr += 1
        return ins

    def cc(self, kind, ins_ap, outs_ap, groups, reads, writes, chanbuf):
        if chanbuf.chan is None:
            chanbuf.chan = ("ch", self.nchan)
            self.nchan += 1
            self._newsem(chanbuf.chan)
        ck = chanbuf.chan
        self.deps("pool", reads, writes, is_dma=True)
        self._wait("pool", ck, self.cnt[ck])
        ins = self.nc.gpsimd.collective_compute(kind, ALU.bypass, replica_groups=groups, ins=[ins_ap], outs=[outs_ap])
        self.cnt[ck] += 1
        ins.then_inc(self.sems[ck], 1)
        self.record(ck, self.cnt[ck], reads, writes)

    def barrier(self):
        snap = dict(self.cnt)
        for eng in self.E:
            for key, val in snap.items():
                if key != eng:
                    self._wait(eng, key, val)

    def finish(self, bufs, eng="sp"):
        for b in bufs:
            if b.w is not None:
                self._wait(eng, *b.w)
            for k, v in b.r.items():
                self._wait(eng, k, v)


class Ring:
    def __init__(self, items):
        self.items = items
        self.i = 0

    def get(self):
        it = self.items[self.i % len(self.items)]
        self.i += 1
        return it


class _Stop(Exception):
    pass


class _StopOuter(Exception):
    pass


def build_program(n_layers=2, dbg=None, stop=None, groups=None):
    box = {}
    try:
        _build_program(box, n_layers, dbg, stop, groups)
    except _StopOuter:
        pass
    return box["nc"]


def _build_program(box, n_layers, dbg, stop, groups):
    nc = bass.Bass("TRN2", target_bir_lowering=False)
    box["nc"] = nc
    dbg = dbg or {}

    def din(name, shape, dt=F32):
        return nc.dram_tensor(name, list(shape), dt, kind="ExternalInput").ap()

    xT_in = din("xT", [D, NT])
    cc_in = din("cc", [128, 8, 2])
    mod_w = din("mod_w", [2, D, 6 * D])
    mod_b = din("mod_b", [2, 1, 6 * D])
    w_in = din("w_in", [2, D, 2736])
    w_out = din("w_out", [2, D, D])
    w_qb = din("w_qb", [2, 256, 384])
    w_qbrot = din("w_qbrot", [2, 256, 384])
    w_kb = din("w_kb", [2, 128, 256])
    w_vb = din("w_vb", [2, 128, 256])
    nw_in = din("nw", [2, 128, 16])
    fnw_in = din("fnw", [128, 8])
    qnw_in = din("qnw_b", [2, 128, 256])
    kvnw_in = din("kvnw_b", [2, 128, 128])
    convw_in = din("convw", [2, 128, 18])
    convb_in = din("convb", [2, 128, 6])
    dtb_in = din("dtb_b", [2, 128, 16])
    alog_in = din("alog_b", [2, 128, 16])
    rdl_in = din("rdl_b", [2, 128, 8])
    dskip_in = din("dskip_b", [2, 128, 512])
    snw_in = din("snw_b", [2, 128, 512])
    router_in = din("router", [D, 8])
    ffn_wg = din("ffn_wg", [D, 2816])
    ffn_wu = din("ffn_wu", [D, 2816])
    ffn_wd = din("ffn_wd", [2816, D])
    moe_wg = din("moe_wg", [8, D, 1408])
    moe_wu = din("moe_wu", [8, D, 1408])
    moe_wd = din("moe_wd", [8, 1408, D])
    cf_in = din("cf32", [128, 6, 128])
    cb_in = din("cbf16", [128, 5, 128], BF16)
    rope_in = din("ropeT", [128, 2, NT])
    sel_in = din("sel", [128, 16])
    outT = nc.dram_tensor("outT", [D, TL], F32, kind="ExternalOutput").ap()
    dbg_out = {k: nc.dram_tensor("dbg_" + k, list(s), F32, kind="ExternalOutput").ap() for k, s in dbg.items()}

    xres = nc.dram_tensor("xres", [D, NT], F32).ap()
    kv_send = nc.dram_tensor("kv_send", [162, NT], BF16)
    kv_all = nc.dram_tensor("kv_all", [4 * 162, NT], BF16)
    st_send = nc.dram_tensor("st_send", [128, 1560], F32)
    st_all = nc.dram_tensor("st_all", [4 * 128, 1560], F32)
    cd_dram = nc.dram_tensor("cd_dram", [NCH, 128, CDW], BF16).ap()
    yf_dram = nc.dram_tensor("yf_dram", [NCH, 128, 768], F32).ap()
    modrow_d = nc.dram_tensor("modrow_d", [2, 6 * D], F32).ap()
    xbc_dram = nc.dram_tensor("xbc_dram", [128, 6, NT + 4], BF16).ap()
    GROUPS = groups or [[0, 1, 2, 3], [4, 5, 6, 7]]

    with ExitStack() as es:
        fw = FW(nc, es)
        op = fw.op

        sbn = [0]

        def sb(es_, name, shape, dt):
            sbn[0] += 1
            return es_.enter_context(nc.sbuf_tensor("%s_%d" % (name, sbn[0]), list(shape), dt))

        PS = [es.enter_context(nc.psum_tensor("ps%d" % i, [128, 512], F32)) for i in range(8)]
        PSB = [Buf() for _ in range(8)]
        psr = Ring(list(zip(PS, PSB)))

        cf = sb(es, "cf_sb", [128, 6, 128], F32); b_cf = Buf()
        cb = sb(es, "cb_sb", [128, 5, 128], BF16); b_cb = Buf()
        ident_f, triF, triB, triBs, ones_f = (cf[:, i, :] for i in range(5))
        selm = cf[:, 5, 0:64]
        tri_bf = [cb[:, 3, :], cb[:, 4, :]]
        ident_b, ones_b = cb[:, 0, :], cb[:, 1, :]
        perm32 = cb[0:32, 2, 0:32]
        sel = sb(es, "sel_sb", [128, 16], F32); b_sel = Buf()
        ccs = sb(es, "ccs", [128, 8, 2], F32); b_ccs = Buf()
        ccb = sb(es, "ccb", [128, 8, 2], BF16)
        mv = sb(es, "mv", [128, 48, 2], F32); b_mv = Buf()
        g12 = sb(es, "g12", [128, 16, 2], F32); b_g12 = Buf()
        nws = sb(es, "nws", [128, 16], F32); b_nws = Buf()
        fnw = sb(es, "fnw_sb", [128, 8], F32); b_fnw = Buf()
        smallv = sb(es, "smallv", [128, 256 + 128 + 18 + 6 + 16 + 16 + 8 + 512 + 512], F32); b_small = Buf()
        o_ = [0]

        def carve(n):
            a = smallv[:, o_[0]:o_[0] + n]
            o_[0] += n
            return a
        qnw_b, kvnw_b, convw, convb, dtb_b, alog_b, rdl_b, dskip_b, snw_b = (carve(n) for n in (256, 128, 18, 6, 16, 16, 8, 512, 512))
        a_b = sb(es, "a_b", [128, 16], F32); b_ab = Buf()
        laret = sb(es, "laret", [128, 8], F32); b_laret = Buf()
        la_all = sb(es, "la_all", [128, NCH, 24], F32)
        b_la = [Buf() for _ in range(NCH)]
        hxT = sb(es, "hxT", [128, 8, NT], BF16)
        b_hx = [Buf() for _ in BLOCKS]
        b_mix = [Buf() for _ in range(NCH)]
        rope = None

        fw.dma("sp", cf[:, :, :], cf_in, writes=[b_cf])
        fw.dma("sp", cb[:, :, :], cb_in, writes=[b_cb])
        fw.dma("sp", sel[:, :], sel_in, writes=[b_sel])
        fw.dma("sp", ccs[:, :, :], cc_in, writes=[b_ccs])
        fw.dma("sp", fnw[:, :], fnw_in, writes=[b_fnw])
        op("act", lambda e: e.activation(out=ccb[:, :, :], in_=ccs[:, :, :], func=AF.Silu), reads=[b_ccs], writes=[b_ccs])

        dbg_bufs = []

        def dump(name, ap, bufs):
            if name in dbg_out:
                b_ = Buf()
                dbg_bufs.append(b_)
                ncol = ap.shape[-1]
                for c0 in range(0, ncol, 1024):
                    c1 = min(ncol, c0 + 1024)
                    fw.dma("pool", dbg_out[name][:, c0:c1], ap[:, c0:c1], reads=list(bufs), writes=[b_])

        def stop_at(tag):
            if stop == tag:
                raise _Stop()

        def blk_of_chunk(c):
            return 0 if c < 2 else 1 + (c - 2) // 4

        lm = None
        stopped = False
        try:
          for L in range(n_layers):
              last = L == n_layers - 1
              x_src = xT_in if L == 0 else xres
              fw.barrier()
              offs = 0
              for src, n in ((qnw_in, 256), (kvnw_in, 128), (convw_in, 18), (convb_in, 6), (dtb_in, 16), (alog_in, 16),
                             (rdl_in, 8), (dskip_in, 512), (snw_in, 512)):
                  fw.dma("sp", smallv[:, offs:offs + n], src[L], writes=[b_small])
                  offs += n
              fw.dma("sp", nws[:, :], nw_in[L], writes=[b_nws])
              op("act", lambda e: e.activation(out=a_b[:, :], in_=alog_b, func=AF.Exp), reads=[b_small], writes=[b_ab])
              op("dve", lambda e: e.tensor_scalar(out=a_b[:, :], in0=a_b[:, :], scalar1=-1.0, scalar2=None, op0=ALU.mult), reads=[b_ab], writes=[b_ab])
              op("act", lambda e: e.activation(out=laret[:, :], in_=rdl_b, func=AF.Exp, scale=-1.0), reads=[b_small], writes=[b_laret])
              op("act", lambda e: e.activation(out=laret[:, :], in_=laret[:, :], func=AF.Ln, bias=1.0), reads=[b_laret], writes=[b_laret])
              op("dve", lambda e: e.tensor_scalar(out=laret[:, :], in0=laret[:, :], scalar1=-1.0, scalar2=None, op0=ALU.mult), reads=[b_laret], writes=[b_laret])

              with ExitStack() as ph:
                  mw = [sb(ph, "mw%d" % i, [128, 8, 512], BF16) for i in range(2)]
                  b_mw = [Buf(), Buf()]
                  mrow = sb(ph, "mrow", [2, 6 * D], F32); b_mrow = Buf()
                  mbias = sb(ph, "mbias", [2, 6 * D], F32); b_mbias = Buf()
                  for v in range(2):
                      fw.dma("sp", mbias[v:v + 1, :], mod_b[L], writes=[b_mbias])
                  for j in range(12):
                      t, bt = mw[j % 2], b_mw[j % 2]
                      fw.dma("pool", t[:, :, :], mod_w[L, :, j * 512:(j + 1) * 512].rearrange("(k p) n -> p k n", p=128), writes=[bt])
                      pt, pb = psr.get()
                      fw.group("pe", [(lambda e, k=k: e.matmul(pt[0:2, :], ccb[:, k, :], t[:, k, :], start=(k == 0), stop=(k == 7))) for k in range(8)],
                               reads=[b_ccs, bt], writes=[pb])
                      op("dve", lambda e: e.tensor_tensor(out=mrow[:, j * 512:(j + 1) * 512], in0=pt[0:2, :], in1=mbias[:, j * 512:(j + 1) * 512], op=ALU.add),
                         reads=[pb, b_mbias], writes=[b_mrow])
                  pt, pb = psr.get()
                  fw.group("pe", [(lambda e, j=j: e.transpose(pt[:, 2 * j:2 * j + 2], mrow[0:2, j * 128:(j + 1) * 128], ident_f[0:2, 0:2])) for j in range(48)],
                           reads=[b_mrow, b_cf], writes=[pb])
                  op("dve", lambda e: e.tensor_copy(mv[:, :, :].rearrange("p j v -> p (j v)"), pt[:, 0:96]), reads=[pb], writes=[b_mv])
                  for n_, (sc0, nw0) in enumerate(((8, 0), (32, 8))):
                      op("dve", lambda e: e.tensor_scalar(out=g12[:, n_ * 8:(n_ + 1) * 8, :], in0=mv[:, sc0:sc0 + 8, :], scalar1=1.0, scalar2=None, op0=ALU.add),
                         reads=[b_mv], writes=[b_g12])
                      op("dve", lambda e: e.tensor_tensor(out=g12[:, n_ * 8:(n_ + 1) * 8, :], in0=g12[:, n_ * 8:(n_ + 1) * 8, :],
                                                           in1=nws[:, nw0:nw0 + 8].unsqueeze(2).to_broadcast([128, 8, 2]), op=ALU.mult),
                         reads=[b_g12, b_nws], writes=[b_g12])

              if L == 0:
                  dump("mv", mv[:, :, :].rearrange("p j v -> p (j v)"), [b_mv])
                  dump("g12", g12[:, :, :].rearrange("p j v -> p (j v)"), [b_g12])
                  stop_at("mod")
              fw.barrier()
              def norm_mod(xb_ap, xb_buf, t0, W, v, gofs, shofs, wk, out_buf):
                  sq, b_sq = wk["sq"].get()
                  op("act", lambda e: e.activation(out=sq[:, :, 0:W], in_=xb_ap, func=AF.Square), reads=[xb_buf], writes=[b_sq])
                  pt, pb = psr.get()
                  fw.group("pe", [(lambda e, k=k: e.matmul(pt[:, 0:W], ones_b, sq[:, k, 0:W], start=(k == 0), stop=(k == 7))) for k in range(8)],
                           reads=[b_sq, b_cb], writes=[pb])
                  rs, b_rs = wk["rs"].get()
                  op("act", lambda e: e.activation(out=rs[:, 0:W], in_=pt[:, 0:W], func=AF.Sqrt, scale=1.0 / D, bias=EPS), reads=[pb], writes=[b_rs])
                  op("dve", lambda e: e.reciprocal(out=rs[:, 0:W], in_=rs[:, 0:W]), reads=[b_rs], writes=[b_rs])
                  for k in range(8):
                      tmp, b_tmp = wk["tmp"].get()
                      op("dve", lambda e: e.tensor_tensor(out=tmp[:, 0:W], in0=xb_ap[:, k, :], in1=rs[:, 0:W], op=ALU.mult), reads=[xb_buf, b_rs], writes=[b_tmp])
                      op("act", lambda e: e.activation(out=hxT[:, k, t0:t0 + W], in_=tmp[:, 0:W], func=AF.Identity,
                                                        scale=g12[:, gofs + k, v:v + 1], bias=mv[:, shofs + k, v:v + 1]),
                         reads=[b_tmp, b_g12, b_mv], writes=[out_buf])

              lm = ExitStack()
              mixT = sb(lm, "mixT", [128, 8, NT], BF16)
              b_xres = Buf()
              with ExitStack() as mx:
                  xbcT = xbc_dram
                  qT = sb(mx, "qT", [96, 4, NT], BF16); b_qT = [Buf() for _ in BLOCKS]
                  hal = sb(mx, "hal", [128, 6, 4], BF16); b_hal = Buf()
                  b_xbc = [Buf() for _ in BLOCKS]
                  b_halo = Buf()
                  XO = lambda t: t + 1 if t < TC else t + 3
                  with ExitStack() as s1:
                      W1a = sb(s1, "W1a", [128, 8, 416], BF16); b_W1a = Buf()
                      W1x = sb(s1, "W1x", [128, 8, 768], BF16); b_W1x = Buf()
                      wqb = sb(s1, "wqb", [128, 2, 384], BF16); b_wqb = Buf()
                      wqr = sb(s1, "wqr", [128, 2, 384], BF16); b_wqr = Buf()
                      ropes_t = sb(s1, "ropes", [128, 2, 512], F32); b_rope = Buf()
                      xst = sb(s1, "xst", [128, 6, 512], BF16); b_xst = Buf()
                      bcol_r = Ring([(sb(s1, "bcol%d" % i, [128, 136], BF16), Buf()) for i in range(2)])
                      kvl = sb(s1, "kvl", [128, 2, NT], BF16); b_kvl = Buf()
                      xb_r = Ring([(sb(s1, "xblk%d" % i, [128, 8, 512], F32), Buf()) for i in range(1)])
                      wk = {"sq": Ring([(sb(s1, "sq%d" % i, [128, 8, 512], BF16), Buf()) for i in range(1)]),
                            "rs": Ring([(sb(s1, "rs%d" % i, [128, 512], F32), Buf()) for i in range(1)]),
                            "tmp": Ring([(sb(s1, "tmp%d" % i, [128, 512], F32), Buf()) for i in range(2)])}
                      tk_r = Ring([(sb(s1, "tk%d" % i, [128, 416], BF16), Buf()) for i in range(2)])
                      st_r = Ring([(sb(s1, "st%d" % i, [128, 4], F32), Buf()) for i in range(2)])
                      cqT = sb(s1, "cqT", [128, 2, 512], BF16); b_cqT = Buf()
                      kpT = sb(s1, "kpT", [32, 512], BF16); b_kpT = Buf()
                      junk = sb(s1, "junk", [128, 256], F32); b_junk = Buf()
                      rt_r = Ring([(sb(s1, "rt%d" % i, [128, 512], F32), Buf()) for i in range(2)])
                      fw.dma("pool", W1a[:, :, :], w_in[L, :, 0:416].rearrange("(k p) n -> p k n", p=128), writes=[b_W1a])
                      fw.dma("pool", W1x[:, :, :], w_in[L, :, 1952:2720].rearrange("(k p) n -> p k n", p=128), writes=[b_W1x])
                      fw.dma("pool", wqb[:, :, :], w_qb[L].rearrange("(k p) n -> p k n", p=128), writes=[b_wqb])
                      fw.dma("pool", wqr[:, :, :], w_qbrot[L].rearrange("(k p) n -> p k n", p=128), writes=[b_wqr])
                      b_send = Buf()
                      zrow = sb(s1, "zrow", [2, 1536], BF16); b_zrow = Buf()
                      op("pool", lambda e: e.memset(zrow[:, :], 0.0), writes=[b_zrow])
                      fw.dma("sp", kv_send[160:162, 768:NT], zrow[:, :], reads=[b_zrow], writes=[b_send], chanbuf=b_zrow)
                      for bi, (t0, W, v) in enumerate(BLOCKS):
                          fw.dma("sp", ropes_t[:, :, 0:W], rope_in[:, :, t0:t0 + W], writes=[b_rope])
                          ropes = ropes_t[:, :, :]
                          xb, b_xb = xb_r.get()
                          fw.dma("sp", xb[:, :, 0:W], x_src[:, t0:t0 + W].rearrange("(k p) t -> p k t", p=128), writes=[b_xb])
                          norm_mod(xb[:, :, 0:W], b_xb, t0, W, v, 0, 0, wk, b_hx[bi])
                          if bi == 0:
                              stop_at("s1a")
                          for oc in range(6):
                              pt, pb = psr.get()
                              fw.group("pe", [(lambda e, k=k: e.matmul(pt[:, 0:W], W1x[:, k, oc * 128:(oc + 1) * 128], hxT[:, k, t0:t0 + W], start=(k == 0), stop=(k == 7))) for k in range(8)],
                                       reads=[b_W1x, b_hx[bi]], writes=[pb])
                              op("act", lambda e: e.activation(out=xst[:, oc, 0:W], in_=pt[:, 0:W], func=AF.Copy), reads=[pb], writes=[b_xst])
                          fw.dma("sp", xbcT[:, :, XO(t0):XO(t0) + W], xst[:, :, 0:W], reads=[b_xst], writes=[b_xbc[bi]], chanbuf=b_xst)
                          if bi in (1, 4):
                              ccol = 0 if bi == 1 else W - 1
                              rrow = 160 if bi == 1 else 161
                              bcol, b_bcol = bcol_r.get()
                              op("dve", lambda e: e.tensor_copy(bcol[:, 0:6], xst[:, :, ccol]), reads=[b_xst], writes=[b_bcol])
                              p2, pb2 = psr.get()
                              pbf = p2[:, 0:64].bitcast(BF16)
                              op("pe", lambda e: e.transpose(pbf[0:6, 0:128], bcol[:, 0:6], ident_b), reads=[b_bcol, b_cb], writes=[pb2])
                              op("dve", lambda e: e.tensor_copy(bcol[0:6, 8:136], pbf[0:6, 0:128]), reads=[pb2], writes=[b_bcol])
                              fw.dma("sp", kv_send[rrow:rrow + 1, 0:768].rearrange("o (c p) -> (o c) p", p=128), bcol[0:6, 8:136], reads=[b_bcol], writes=[b_send], chanbuf=b_bcol)
                          if bi == 0:
                              stop_at("s1b")
                          for ti in range(W // 128):
                              tt = t0 + ti * 128
                              pt, pb = psr.get()
                              fw.group("pe", [(lambda e, k=k: e.matmul(pt[:, 0:416], hxT[:, k, tt:tt + 128], W1a[:, k, :], start=(k == 0), stop=(k == 7))) for k in range(8)],
                                       reads=[b_W1a, b_hx[bi]], writes=[pb])
                              if bi == 0 and ti == 0:
                                  stop_at("t1")
                              stt_, b_st = st_r.get()
                              op("act", lambda e: e.activation(out=junk[:, 0:256], in_=pt[:, 0:256], func=AF.Square, accum_out=stt_[:, 0:1]), reads=[pb], writes=[b_junk, b_st])
                              op("act", lambda e: e.activation(out=junk[:, 0:128], in_=pt[:, 256:384], func=AF.Square, accum_out=stt_[:, 1:2]), reads=[pb], writes=[b_junk, b_st])
                              op("act", lambda e: e.activation(out=stt_[:, 2:3], in_=stt_[:, 0:1], func=AF.Sqrt, scale=1.0 / 256, bias=EPS), reads=[b_st], writes=[b_st])
                              op("act", lambda e: e.activation(out=stt_[:, 3:4], in_=stt_[:, 1:2], func=AF.Sqrt, scale=1.0 / 128, bias=EPS), reads=[b_st], writes=[b_st])
                              op("dve", lambda e: e.reciprocal(out=stt_[:, 2:4], in_=stt_[:, 2:4]), reads=[b_st], writes=[b_st])
                              if bi == 0 and ti == 0:
                                  stop_at("t2")
                              tk, b_tk = tk_r.get()
                              op("dve", lambda e: e.scalar_tensor_tensor(out=tk[:, 0:256], in0=pt[:, 0:256], scalar=stt_[:, 2:3], op0=ALU.mult, in1=qnw_b, op1=ALU.mult),
                                 reads=[pb, b_st, b_small], writes=[b_tk])
                              op("dve", lambda e: e.scalar_tensor_tensor(out=tk[:, 256:384], in0=pt[:, 256:384], scalar=stt_[:, 3:4], op0=ALU.mult, in1=kvnw_b, op1=ALU.mult),
                                 reads=[pb, b_st, b_small], writes=[b_tk])
                              op("act", lambda e: e.activation(out=tk[:, 384:416], in_=pt[:, 384:416], func=AF.Copy), reads=[pb], writes=[b_tk])
                              if bi == 0 and ti == 0:
                                  stop_at("t3")
                              p2, pb2 = psr.get()
                              pbf = p2[:, 0:256].bitcast(BF16)
                              fw.group("pe", [lambda e: e.transpose(pbf[:, 0:128], tk[:, 0:128], ident_b),
                                              lambda e: e.transpose(pbf[:, 128:256], tk[:, 128:256], ident_b),
                                              lambda e: e.transpose(pbf[:, 256:384], tk[:, 256:384], ident_b),
                                              lambda e: e.transpose(pbf[0:32, 384:512], tk[:, 384:416], ident_b)],
                                       reads=[b_tk, b_cb], writes=[pb2])
                              if bi == 0 and ti == 0:
                                  stop_at("t4")
                              op("dve", lambda e: e.tensor_copy(cqT[:, :, ti * 128:(ti + 1) * 128], pbf[:, 0:256].rearrange("p (c t) -> p c t", c=2)), reads=[pb2], writes=[b_cqT])
                              if bi == 0 and ti == 0:
                                  stop_at("t5")
                              op("dve", lambda e: e.tensor_copy(kvl[:, 0, tt:tt + 128], pbf[:, 256:384]), reads=[pb2], writes=[b_kvl])
                              if bi == 0 and ti == 0:
                                  stop_at("t6")
                              op("dve", lambda e: e.tensor_copy(kpT[:, ti * 128:(ti + 1) * 128], pbf[0:32, 384:512]), reads=[pb2], writes=[b_kpT])
                          if bi == 0:
                              stop_at("s1c")
                          pt, pb = psr.get()
                          op("pe", lambda e: e.matmul(pt[0:32, 0:W], perm32, kpT[:, 0:W], start=True, stop=True), reads=[b_cb, b_kpT], writes=[pb])
                          r1, b_r1 = rt_r.get()
                          r2, b_r2 = rt_r.get()
                          op("dve", lambda e: e.tensor_tensor(out=r1[0:32, 0:W], in0=pt[0:32, 0:W], in1=ropes_t[0:32, 1, 0:W], op=ALU.mult), reads=[pb, b_rope], writes=[b_r1])
                          op("pool", lambda e: e.tensor_tensor(out=r2[0:32, 0:W], in0=kpT[:, 0:W], in1=ropes_t[0:32, 0, 0:W], op=ALU.mult), reads=[b_kpT, b_rope], writes=[b_r2])
                          op("dve", lambda e: e.tensor_tensor(out=kvl[0:32, 1, t0:t0 + W], in0=r1[0:32, 0:W], in1=r2[0:32, 0:W], op=ALU.add), reads=[b_r1, b_r2], writes=[b_kvl])
                          if bi == 0:
                              stop_at("s1d")
                          for h in range(4):
                              pq, pbq = psr.get()
                              pr, pbr = psr.get()
                              fw.group("pe", [(lambda e, k=k: e.matmul(pq[0:96, 0:W], wqb[:, k, h * 96:(h + 1) * 96], cqT[:, k, 0:W], start=(k == 0), stop=(k == 1))) for k in range(2)],
                                       reads=[b_wqb, b_cqT], writes=[pbq])
                              fw.group("pe", [(lambda e, k=k: e.matmul(pr[0:96, 0:W], wqr[:, k, h * 96:(h + 1) * 96], cqT[:, k, 0:W], start=(k == 0), stop=(k == 1))) for k in range(2)],
                                       reads=[b_wqr, b_cqT], writes=[pbr])
                              op("act", lambda e: e.activation(out=qT[0:64, h, t0:t0 + W], in_=pq[0:64, 0:W], func=AF.Copy), reads=[pbq], writes=[b_qT[bi]])
                              r1, b_r1 = rt_r.get()
                              r2, b_r2 = rt_r.get()
                              op("dve", lambda e: e.tensor_tensor(out=r1[64:96, 0:W], in0=pr[64:96, 0:W], in1=ropes_t[64:96, 1, 0:W], op=ALU.mult), reads=[pbr, b_rope], writes=[b_r1])
                              op("dve", lambda e: e.tensor_tensor(out=r2[64:96, 0:W], in0=pq[64:96, 0:W], in1=ropes_t[64:96, 0, 0:W], op=ALU.mult), reads=[pbq, b_rope], writes=[b_r2])
                              if bi == 0 and h == 0:
                                  stop_at("q0")
                              op("pool", lambda e: e.tensor_tensor(out=qT[64:96, h, t0:t0 + W], in0=r1[64:96, 0:W], in1=r2[64:96, 0:W], op=ALU.add), reads=[b_r1, b_r2], writes=[b_qT[bi]])
                              if bi == 0 and h == 0:
                                  stop_at("q1")
                          if bi == 0:
                              stop_at("b0")
                          if bi == 1:
                              stop_at("b1")
                      fw.dma("sp", kv_send[0:128, :], kvl[:, 0, :], reads=[b_kvl], writes=[b_send])
                      fw.dma("sp", kv_send[128:160, :], kvl[0:32, 1, :], reads=[b_kvl], writes=[b_send])
                      if L == 0:
                          dump("hx", hxT[:, :, :].rearrange("p k t -> p (k t)"), b_hx)
                          dump("qT", qT[:, :, :].rearrange("p k t -> p (k t)"), b_qT)
                          dump("kvl", kvl[:, :, :].rearrange("p k t -> p (k t)"), [b_kvl])
                          dump("xbcA", xbc_dram[:, 0, 0:260], b_xbc)
                          stop_at("s1")
                      b_all = Buf()
                      fw.cc("AllGather", kv_send.ap().opt(), kv_all.ap().opt(), GROUPS, [b_send], [b_all], b_all)
                  fw.barrier()
                  with ExitStack() as hh:
                      bd = sb(hh, "bd", [8, 768], BF16); b_bd = Buf()
                      selb = sb(hh, "selb", [8, 2], BF16); b_selb = Buf()
                      for r in range(4):
                          fw.dma("sp", bd[2 * r:2 * r + 2, :], kv_all[r * 162 + 160:r * 162 + 162, 0:768], reads=[b_all], writes=[b_bd])
                      op("dve", lambda e: e.tensor_copy(selb[:, :], sel[0:8, 0:2]), reads=[b_sel], writes=[b_selb])
                      op("dve", lambda e: e.memset(hal[:, :, :], 0.0), writes=[b_hal])
                      for oc in range(6):
                          pt, pb = psr.get()
                          op("pe", lambda e: e.matmul(pt[:, 0:2], bd[:, oc * 128:(oc + 1) * 128], selb[:, :], start=True, stop=True), reads=[b_bd, b_selb], writes=[pb])
                          op("dve", lambda e: e.tensor_copy(hal[:, oc, 2:4], pt[:, 0:2]), reads=[pb], writes=[b_hal])

                  fw.barrier()
                  with ExitStack() as at:
                      NK = TC + 4 * TL
                      ckT = sb(at, "ckT", [128, NK], BF16); b_ck = Buf()
                      KT = sb(at, "KT", [96, NK], BF16); b_KTp = Buf(); b_KTn = Buf()
                      Va = sb(at, "Va", [128, NK // 128, 4, 65], BF16); b_Va = Buf()
                      wkb = sb(at, "wkb", [128, 256], BF16); b_wkb = Buf()
                      wvb = sb(at, "wvb", [128, 256], BF16); b_wvb = Buf()
                      pT_r = Ring([(sb(at, "pT%d" % i, [128, 512], BF16), Buf()) for i in range(5)])
                      s_ring = Ring(list(zip(PS[0:4], PSB[0:4])))
                      o_ring = Ring(list(zip(PS[4:6], PSB[4:6])))
                      psr_keep = psr
                      psr = Ring(list(zip(PS[6:8], PSB[6:8])))
                      osb = sb(at, "osb", [65, 512], F32); b_osb = Buf()
                      rb = sb(at, "rb", [64, 512], F32); b_rb = Buf()
                      ot = sb(at, "ot", [64, 512], BF16); b_ot = Buf()
                      fw.dma("pool", wkb[:, :], w_kb[L], writes=[b_wkb])
                      fw.dma("pool", wvb[:, :], w_vb[L], writes=[b_wvb])
                      fw.dma("sp", ckT[:, 0:TC], kv_all[0:128, 0:TC], reads=[b_all], writes=[b_ck])
                      fw.dma("sp", KT[64:96, 0:TC], kv_all[128:160, 0:TC], reads=[b_all], writes=[b_KTp])
                      for r in range(4):
                          fw.dma("sp", ckT[:, TC + r * TL:TC + (r + 1) * TL], kv_all[r * 162:r * 162 + 128, TC:NT], reads=[b_all], writes=[b_ck])
                          fw.dma("sp", KT[64:96, TC + r * TL:TC + (r + 1) * TL], kv_all[r * 162 + 128:r * 162 + 160, TC:NT], reads=[b_all], writes=[b_KTp])
                      op("pool", lambda e: e.memset(Va[:, :, :, 64:65], 1.0), writes=[b_Va])
                      for kc in range(NK // 128):
                          pt, pb = psr.get()
                          op("pe", lambda e: e.matmul(pt[:, 0:256], ckT[:, kc * 128:(kc + 1) * 128], wvb[:, :], start=True, stop=True), reads=[b_ck, b_wvb], writes=[pb])
                          eng = "act" if kc % 2 == 0 else "dve"
                          if eng == "act":
                              op("act", lambda e: e.activation(out=Va[:, kc, :, 0:64], in_=pt[:, 0:256].rearrange("p (h d) -> p h d", h=4), func=AF.Copy), reads=[pb], writes=[b_Va])
                          else:
                              op("dve", lambda e: e.tensor_copy(Va[:, kc, :, 0:64], pt[:, 0:256].rearrange("p (h d) -> p h d", h=4)), reads=[pb], writes=[b_Va])
                      for h in range(4):
                          for kb in range((NK + 511) // 512):
                              k0 = kb * 512
                              kw_ = min(512, NK - k0)
                              pt, pb = psr.get()
                              op("pe", lambda e: e.matmul(pt[0:64, 0:kw_], wkb[:, h * 64:(h + 1) * 64], ckT[:, k0:k0 + kw_], start=True, stop=True), reads=[b_wkb, b_ck], writes=[pb])
                              if kb % 2 == 0:
                                  op("act", lambda e: e.activation(out=KT[0:64, k0:k0 + kw_], in_=pt[0:64, 0:kw_], func=AF.Copy), reads=[pb], writes=[b_KTn])
                              else:
                                  op("dve", lambda e: e.tensor_copy(KT[0:64, k0:k0 + kw_], pt[0:64, 0:kw_]), reads=[pb], writes=[b_KTn])
                          for bi, (t0, W, v) in enumerate(BLOCKS):
                              if v == 1 and last:
                                  continue
                              nkc = 2 if v == 1 else NK // 128
                              po, pbo = o_ring.get()
                              LA = 2
                              pend = []
                              for kc in range(nkc + LA):
                                  if kc < nkc:
                                      ps_, pbs = s_ring.get()
                                      op("pe", lambda e: e.matmul(ps_[:, 0:W], KT[0:96, kc * 128:(kc + 1) * 128], qT[0:96, h, t0:t0 + W], start=True, stop=True),
                                         reads=[b_KTn, b_KTp, b_qT[bi]], writes=[pbs])
                                      pT, b_pT = pT_r.get()
                                      op("act", lambda e: e.activation(out=pT[:, 0:W], in_=ps_[:, 0:W], func=AF.Exp, scale=MLA_SCALE), reads=[pbs], writes=[b_pT])
                                      pend.append((pT, b_pT))
                                  if kc >= LA:
                                      kk = kc - LA
                                      pT2, b_pT2 = pend[kk]
                                      op("pe", lambda e: e.matmul(po[0:65, 0:W], Va[:, kk, h, :], pT2[:, 0:W], start=(kk == 0), stop=(kk == nkc - 1)),
                                         reads=[b_Va, b_pT2], writes=[pbo])
                              op("dve", lambda e: e.tensor_copy(osb[:, 0:W], po[0:65, 0:W]), reads=[pbo], writes=[b_osb])
                              op("dve", lambda e: e.reciprocal(out=osb[64:65, 0:W], in_=osb[64:65, 0:W]), reads=[b_osb], writes=[b_osb])
                              pt, pb = psr.get()
                              op("pe", lambda e: e.matmul(pt[0:64, 0:W], selm[0:65, :], osb[0:65, 0:W], start=True, stop=True), reads=[b_osb, b_cf], writes=[pb])
                              op("act", lambda e: e.activation(out=rb[:, 0:W], in_=pt[0:64, 0:W], func=AF.Copy), reads=[pb], writes=[b_rb])
                              mixw = [b_mix[c] for c in range(t0 // 128, (t0 + W) // 128)]
                              if h % 2 == 0:
                                  op("dve", lambda e: e.tensor_tensor(out=mixT[0:64, h // 2, t0:t0 + W], in0=osb[0:64, 0:W], in1=rb[:, 0:W], op=ALU.mult),
                                     reads=[b_osb, b_rb], writes=mixw)
                              else:
                                  op("dve", lambda e: e.tensor_tensor(out=ot[:, 0:W], in0=osb[0:64, 0:W], in1=rb[:, 0:W], op=ALU.mult), reads=[b_osb, b_rb], writes=[b_ot])
                                  op("dve", lambda e: e.tensor_copy(mixT[64:128, h // 2, t0:t0 + W], ot[:, 0:W]), reads=[b_ot], writes=mixw)

                  psr = psr_keep
                  if L == 0:
                      dump("att", mixT[:, 0:2, :].rearrange("p k t -> p (k t)"), b_mix)
                      stop_at("att")
                  fw.barrier()
                  with ExitStack() as sc:
                      W2 = sb(sc, "W2", [128, 8, 1552], BF16); b_W2 = Buf()
                      fw.dma("pool", W2[:, :, 0:1536], w_in[L, :, 416:1952].rearrange("(k p) n -> p k n", p=128), writes=[b_W2])
                      fw.dma("pool", W2[:, :, 1536:1552], w_in[L, :, 2720:2736].rearrange("(k p) n -> p k n", p=128), writes=[b_W2])
                      S_run = [sb(sc, "Srun%d" % d, [128, 6, 128], F32) for d in range(2)]; b_S = [Buf(), Buf()]
                      Sbd = [sb(sc, "Sbd%d" % d, [128, 6, 128], BF16) for d in range(2)]; b_Sbd = [Buf(), Buf()]
                      S_ctx = [sb(sc, "Sctx%d" % d, [128, 6, 128], F32) for d in range(2)]; b_Sctx = [Buf(), Buf()]
                      Lacc = sb(sc, "Lacc", [128, 24], F32); b_Lacc = Buf()
                      eL = sb(sc, "eL", [128, 12], F32); b_eL = Buf()
                      cd_r = Ring([(sb(sc, "cd%d" % i, [128, CDW], BF16), Buf()) for i in range(2)])
                      xin_pre = {}
                      la_hl = sb(sc, "la_hl", [128, NCH, 48], BF16)
                      xin_r = Ring([(sb(sc, "xin%d" % i, [128, 6, 130], BF16), Buf()) for i in range(2)])
                      xact_r = Ring([(sb(sc, "xact%d" % i, [128, 6, 128], BF16), Buf()) for i in range(2)])
                      cacc_r = Ring([(sb(sc, "cacc%d" % i, [128, 128], F32), Buf()) for i in range(3)])
                      dt_r = Ring([(sb(sc, "dtt%d" % i, [128, 16], F32), Buf()) for i in range(2)])
                      scl_r = Ring([(sb(sc, "scl%d" % i, [128, 64], F32), Buf()) for i in range(2)])
                      kw_r = Ring([(sb(sc, "kw%d" % i, [128, 128], BF16), Buf()) for i in range(3)])
                      dec_r = Ring([(sb(sc, "dec%d" % i, [128, 128], F32), Buf()) for i in range(4)])
                      qkm_r = Ring([(sb(sc, "qkm%d" % i, [128, 128], F32), Buf()) for i in range(6)])
                      scT_r = Ring([(sb(sc, "scT%d" % i, [128, 128], BF16), Buf()) for i in range(5)])
                      yA_r = Ring([(sb(sc, "yAs%d" % i, [128, 768], F32), Buf()) for i in range(2)])
                      yt_r = Ring([(sb(sc, "yts%d" % i, [128, 768], F32), Buf()) for i in range(2)])
                      yf_r = Ring([(sb(sc, "yfs%d" % i, [128, 768], F32), Buf()) for i in range(1)])
                      gt_r = Ring([(sb(sc, "gts%d" % i, [128, 768], F32), Buf()) for i in range(1)])
                      mo_r = Ring([(sb(sc, "mos%d" % i, [128, 768], BF16), Buf()) for i in range(2)])
                      gst_r = Ring([(sb(sc, "gst%d" % i, [128, 32], F32), Buf()) for i in range(2)])
                      sr_t = sb(sc, "sr_t", [128, 1560], F32); b_sr = Buf()
                      dm_t = sb(sc, "dm_t", [128, 12], F32); b_dm = Buf()
                      tri_d = [triF, triB]
                      b_cdd = [Buf() for _ in range(NCH)]
                      b_yfd = [Buf() for _ in range(NCH)]

                      def hx_bufs(c):
                          return [b_hx[blk_of_chunk(c)]]

                      def xbc_bufs(c):
                          bi = blk_of_chunk(c)
                          return [b_xbc[i] for i in range(max(0, bi - 1), min(len(BLOCKS), bi + 2))]

                      def small_stats(c, d):
                          pt, pb = psr.get()
                          la_c = la_all[:, c, d * 12:(d + 1) * 12]
                          op("pe", lambda e: e.matmul(pt[:, 0:12], tri_d[d], la_c, start=True, stop=True), reads=[b_cf, b_la[c]], writes=[pb])
                          pt2, pb2 = psr.get()
                          op("pe", lambda e: e.matmul(pt2[:, 0:12], ones_f, la_c, start=True, stop=True), reads=[b_cf, b_la[c]], writes=[pb2])
                          scl, b_scl = scl_r.get()
                          op("act", lambda e: e.activation(out=scl[:, 0:12], in_=pt[:, 0:12], func=AF.Copy, scale=-1.0), reads=[pb], writes=[b_scl])
                          op("act", lambda e: e.activation(out=scl[:, 12:24], in_=pt[:, 0:12], func=AF.Exp), reads=[pb], writes=[b_scl])
                          op("act", lambda e: e.activation(out=scl[:, 24:36], in_=pt2[:, 0:12], func=AF.Exp), reads=[pb2], writes=[b_scl])
                          op("dve", lambda e: e.tensor_tensor(out=scl[:, 36:48], in0=pt2[:, 0:12], in1=scl[:, 0:12], op=ALU.add), reads=[pb2, b_scl], writes=[b_scl])
                          op("act", lambda e: e.activation(out=scl[:, 36:48], in_=scl[:, 36:48], func=AF.Exp), reads=[b_scl], writes=[b_scl])
                          op("dve", lambda e: e.tensor_copy(scl[:, 48:60], pt2[:, 0:12]), reads=[pb2], writes=[b_scl])
                          return scl, b_scl

                      def kv_pair(cd, m, d):
                          if m < 2:
                              return cd[:, CD_KTOK + m * 128:CD_KTOK + (m + 1) * 128], cd[:, CD_VTOK + m * 128:CD_VTOK + (m + 1) * 128]
                          xo = (CD_XF if d == 0 else CD_XB) + (m - 2) * 128
                          return cd[:, CD_BTOK:CD_BTOK + 128], cd[:, xo:xo + 128]

                      def chunk_state(cd, b_cd, scl, b_scl, d, m):
                          k_ap, v_ap = kv_pair(cd, m, d)
                          kw, b_kw = kw_r.get()
                          op("dve", lambda e: e.tensor_tensor(out=kw[:, :].rearrange("p (h n) -> p h n", h=2), in0=k_ap.rearrange("p (h n) -> p h n", h=2),
                                                               in1=scl[:, 36 + 2 * m:38 + 2 * m].unsqueeze(2).to_broadcast([128, 2, 64]), op=ALU.mult),
                             reads=[b_cd, b_scl], writes=[b_kw])
                          pt, pb = psr.get()
                          op("pe", lambda e: e.matmul(pt[:, 0:128], kw[:, :], v_ap, start=True, stop=True), reads=[b_kw, b_cd], writes=[pb])
                          return pt, pb

                      kwb_r = Ring([(sb(sc, "kwb%d" % i, [128, 128], BF16), Buf()) for i in range(12)])

                      def states_batch(cd, b_cd, scl, b_scl, d, slots):
                          kws = []
                          for m in range(6):
                              k_ap, v_ap = kv_pair(cd, m, d)
                              kw, b_kw = kwb_r.get()
                              op("pool", lambda e: e.tensor_tensor(out=kw[:, :].rearrange("p (h n) -> p h n", h=2), in0=k_ap.rearrange("p (h n) -> p h n", h=2),
                                                                    in1=scl[:, 36 + 2 * m:38 + 2 * m].unsqueeze(2).to_broadcast([128, 2, 64]), op=ALU.mult),
                                 reads=[b_cd, b_scl], writes=[b_kw])
                              kws.append((kw, b_kw, v_ap))
                          outs = []
                          for m in range(6):
                              kw, b_kw, v_ap = kws[m]
                              pt, pb = slots[m]
                              op("pe", lambda e: e.matmul(pt, kw[:, :], v_ap, start=True, stop=True), reads=[b_kw, b_cd], writes=[pb])
                              outs.append((pt, pb))
                          return outs

                      def upd_state(S, b_S_, scal, b_scal, so, m, add_ap, add_buf):
                          for half in range(2):
                              r0, r1 = half * 64, half * 64 + 64
                              u = 2 * m + half
                              op("dve", lambda e: e.scalar_tensor_tensor(out=S[r0:r1, m, r0:r1], in0=S[r0:r1, m, r0:r1], scalar=scal[r0:r1, so + u:so + u + 1],
                                                                          op0=ALU.mult, in1=add_ap[r0:r1, r0:r1], op1=ALU.add),
                                 reads=[b_S_, b_scal, add_buf], writes=[b_S_])

                      for d in range(2):
                          op("pool", lambda e: e.memset(S_run[d][:, :, :], 0.0), writes=[b_S[d]])
                      op("pool", lambda e: e.memset(Lacc[:, :], 0.0), writes=[b_Lacc])
                      for c in range(NCH):
                          tt = c * 128
                          cd, b_cd = cd_r.get()
                          hb = hx_bufs(c)
                          xact, b_xa = xact_r.get()
                          xc = (tt + 1) if tt < TC else (tt + 3)
                          xb_ = xbc_bufs(c)
                          def load_xin(c_):
                              tt_ = c_ * 128
                              xc_ = (tt_ + 1) if tt_ < TC else (tt_ + 3)
                              xin_, b_xin_ = xin_r.get()
                              lo = 1 if c_ in (0, 2) else 0
                              hi = 129 if c_ in (1, NCH - 1) else 130
                              fw.dma("sp", xin_[:, :, lo:hi], xbcT[:, :, xc_ - 1 + lo:xc_ - 1 + hi], reads=xbc_bufs(c_), writes=[b_xin_])
                              return xin_, b_xin_
                          if c not in xin_pre:
                              xin_pre[c] = load_xin(c)
                          xin, b_xin = xin_pre.pop(c)
                          if c + 1 < NCH:
                              xin_pre[c + 1] = load_xin(c + 1)
                          xb_ = [b_xin]
                          if L == 0 and c in (0, 1):
                              dump("xin%d" % c, xin[:, :, :].rearrange("p k t -> p (k t)"), [b_xin])
                          for cc_, xcol, hcol in ((0, 0, 0), (1, 129, 1), (2, 0, 2), (NCH - 1, 129, 3)):
                              if c == cc_:
                                  op("dve", lambda e: e.tensor_copy(xin[:, :, xcol], hal[:, :, hcol]), reads=[b_hal, b_xin], writes=[b_xin])
                          for oc in range(6):
                              ca, b_ca = cacc_r.get()
                              op("dve", lambda e: e.tensor_scalar(out=ca[:, :], in0=xin[:, oc, 0:128], scalar1=convw[:, oc * 3:oc * 3 + 1], scalar2=None, op0=ALU.mult),
                                 reads=xb_ + [b_small], writes=[b_ca])
                              op("dve", lambda e: e.scalar_tensor_tensor(out=ca[:, :], in0=xin[:, oc, 1:129], scalar=convw[:, oc * 3 + 1:oc * 3 + 2], op0=ALU.mult, in1=ca[:, :], op1=ALU.add),
                                 reads=xb_ + [b_small, b_ca], writes=[b_ca])
                              op("dve", lambda e: e.scalar_tensor_tensor(out=ca[:, :], in0=xin[:, oc, 2:130], scalar=convw[:, oc * 3 + 2:oc * 3 + 3], op0=ALU.mult, in1=ca[:, :], op1=ALU.add),
                                 reads=xb_ + [b_small, b_ca], writes=[b_ca])
                              if oc < 4:
                                  op("act", lambda e: e.activation(out=xact[:, oc, :], in_=ca[:, :], func=AF.Silu, bias=convb[:, oc:oc + 1]), reads=[b_ca, b_small], writes=[b_xa])
                              else:
                                  dst = CD_BT if oc == 4 else CD_CT
                                  op("act", lambda e: e.activation(out=cd[:, dst:dst + 128], in_=ca[:, :], func=AF.Silu, bias=convb[:, oc:oc + 1]), reads=[b_ca, b_small], writes=[b_cd])
                          for col0, dst, scale in ((0, CD_QT, 0.125), (128, CD_QT + 128, 0.125), (256, CD_KT, 1.0), (384, CD_KT + 128, 1.0)):
                              pt, pb = psr.get()
                              fw.group("pe", [(lambda e, k=k: e.matmul(pt[:, 0:128], W2[:, k, col0:col0 + 128], hxT[:, k, tt:tt + 128], start=(k == 0), stop=(k == 7))) for k in range(8)],
                                       reads=[b_W2] + hb, writes=[pb])
                              op("act", lambda e: e.activation(out=cd[:, dst:dst + 128], in_=pt[:, 0:128], func=AF.Copy, scale=scale), reads=[pb], writes=[b_cd])
                          pt, pb = psr.get()
                          fw.group("pe", [(lambda e, k=k: e.matmul(pt[:, 0:512], hxT[:, k, tt:tt + 128], W2[:, k, 256:768], start=(k == 0), stop=(k == 7))) for k in range(8)],
                                   reads=[b_W2] + hb, writes=[pb])
                          op("dve", lambda e: e.tensor_copy(cd[:, CD_KTOK:CD_KTOK + 256], pt[:, 0:256]), reads=[pb], writes=[b_cd])
                          op("act", lambda e: e.activation(out=cd[:, CD_VTOK:CD_VTOK + 256], in_=pt[:, 256:512], func=AF.Copy), reads=[pb], writes=[b_cd])
                          pt, pb = psr.get()
                          fw.group("pe", [(lambda e, k=k: e.matmul(pt[:, 0:256], hxT[:, k, tt:tt + 128], W2[:, k, 768:1024], start=(k == 0), stop=(k == 7))) for k in range(8)],
                                   reads=[b_W2] + hb, writes=[pb])
                          op("act", lambda e: e.activation(out=cd[:, CD_SG:CD_SG + 256], in_=pt[:, 0:256], func=AF.Silu), reads=[pb], writes=[b_cd])
                          pt, pb = psr.get()
                          fw.group("pe", [(lambda e, k=k: e.matmul(pt[:, 0:512], hxT[:, k, tt:tt + 128], W2[:, k, 1024:1536], start=(k == 0), stop=(k == 7))) for k in range(8)],
                                   reads=[b_W2] + hb, writes=[pb])
                          op("act", lambda e: e.activation(out=cd[:, CD_SZ:CD_SZ + 512], in_=pt[:, 0:512], func=AF.Silu), reads=[pb], writes=[b_cd])
                          pt, pb = psr.get()
                          fw.group("pe", [(lambda e, k=k: e.matmul(pt[:, 0:16], hxT[:, k, tt:tt + 128], W2[:, k, 1536:1552], start=(k == 0), stop=(k == 7))) for k in range(8)],
                                   reads=[b_W2] + hb, writes=[pb])
                          dtt, b_dt = dt_r.get()
                          op("dve", lambda e: e.tensor_tensor(out=dtt[:, :], in0=pt[:, 0:16], in1=dtb_b, op=ALU.add), reads=[pb, b_small], writes=[b_dt])
                          op("act", lambda e: e.activation(out=dtt[:, :], in_=dtt[:, :], func=AF.Exp), reads=[b_dt], writes=[b_dt])
                          op("act", lambda e: e.activation(out=dtt[:, :], in_=dtt[:, :], func=AF.Ln, bias=1.0), reads=[b_dt], writes=[b_dt])
                          for d in range(2):
                              op("dve", lambda e: e.tensor_copy(la_all[:, c, d * 12:d * 12 + 4], laret[:, d * 4:d * 4 + 4]), reads=[b_laret], writes=[b_la[c]])
                              op("dve", lambda e: e.tensor_tensor(out=la_all[:, c, d * 12 + 4:d * 12 + 12], in0=dtt[:, d * 8:d * 8 + 8], in1=a_b[:, d * 8:d * 8 + 8], op=ALU.mult),
                                 reads=[b_dt, b_ab], writes=[b_la[c]])
                          op("dve", lambda e: e.tensor_copy(la_hl[:, c, 0:24], la_all[:, c, :]), reads=[b_la[c]], writes=[b_la[c]])
                          op("dve", lambda e: e.tensor_tensor(out=la_hl[:, c, 24:48], in0=la_all[:, c, :], in1=la_hl[:, c, 0:24], op=ALU.subtract), reads=[b_la[c]], writes=[b_la[c]])
                          p2, pb2 = psr.get()
                          pbf = p2[:, 0:320].bitcast(BF16)
                          fw.group("pe", [(lambda e, oc=oc: e.transpose(pbf[:, oc * 128:(oc + 1) * 128], xact[:, oc, :], ident_b)) for oc in range(4)]
                                   + [lambda e: e.transpose(pbf[:, 512:640], cd[:, CD_BT:CD_BT + 128], ident_b)],
                                   reads=[b_xa, b_cd, b_cb], writes=[pb2])
                          op("dve", lambda e: e.tensor_copy(cd[:, CD_XS:CD_XS + 512], pbf[:, 0:512]), reads=[pb2], writes=[b_cd])
                          op("dve", lambda e: e.tensor_copy(cd[:, CD_BTOK:CD_BTOK + 128], pbf[:, 512:640]), reads=[pb2], writes=[b_cd])
                          for d in range(2):
                              xo = CD_XF if d == 0 else CD_XB
                              op("dve" if d == 0 else "pool", lambda e: e.tensor_tensor(out=cd[:, xo:xo + 512].rearrange("p (h n) -> p h n", h=8),
                                                                                       in0=cd[:, CD_XS:CD_XS + 512].rearrange("p (h n) -> p h n", h=8),
                                                                                       in1=dtt[:, d * 8:d * 8 + 8].unsqueeze(2).to_broadcast([128, 8, 64]), op=ALU.mult),
                                 reads=[b_cd, b_dt], writes=[b_cd])
                          if c == 2:
                              for d in range(2):
                                  op("dve", lambda e: e.tensor_copy(S_ctx[d][:, :, :], S_run[d][:, :, :]), reads=[b_S[d]], writes=[b_Sctx[d]])
                                  op("pool", lambda e: e.memset(S_run[d][:, :, :], 0.0), writes=[b_S[d]])
                              op("pool", lambda e: e.memset(Lacc[:, :], 0.0), writes=[b_Lacc])
                          for d in range(2):
                              scl, b_scl = small_stats(c, d)
                              if d == 1:
                                  op("act", lambda e: e.activation(out=eL[:, :], in_=Lacc[:, 12:24], func=AF.Exp), reads=[b_Lacc], writes=[b_eL])
                              slots1 = []
                              for m in range(6):
                                  p_, pb_ = psr.get()
                                  slots1.append((p_[:, 0:128], pb_))
                              st_out = states_batch(cd, b_cd, scl, b_scl, d, slots1)
                              for m in range(6):
                                  pt, pb = st_out[m]
                                  if d == 0:
                                      upd_state(S_run[0], b_S[0], scl, b_scl, 24, m, pt, pb)
                                  else:
                                      for half in range(2):
                                          r0, r1 = half * 64, half * 64 + 64
                                          u = 2 * m + half
                                          op("dve", lambda e: e.scalar_tensor_tensor(out=S_run[1][r0:r1, m, r0:r1], in0=pt[r0:r1, r0:r1], scalar=eL[r0:r1, u:u + 1],
                                                                                      op0=ALU.mult, in1=S_run[1][r0:r1, m, r0:r1], op1=ALU.add),
                                             reads=[pb, b_eL, b_S[1]], writes=[b_S[1]])
                              op("dve", lambda e: e.tensor_tensor(out=Lacc[:, d * 12:(d + 1) * 12], in0=Lacc[:, d * 12:(d + 1) * 12], in1=scl[:, 48:60], op=ALU.add),
                                 reads=[b_Lacc, b_scl], writes=[b_Lacc])
                          fw.dma("sp", cd_dram[c], cd[:, :], reads=[b_cd], writes=[b_cdd[c]], chanbuf=b_cd)
                          if L == 0 and c in (0, 2):
                              dump("cd%d" % c, cd[:, :], [b_cd])

                      if L == 0:
                          dump("la", la_all[:, :, :].rearrange("p c n -> p (c n)"), b_la)
                          dump("xbcB", xbc_dram[:, 0, 0:260], b_xbc)
                          dump("sagg0", S_run[0][:, :, :].rearrange("p c n -> p (c n)"), [b_S[0]])
                          dump("sagg1", S_run[1][:, :, :].rearrange("p c n -> p (c n)"), [b_S[1]])
                          dump("sctx0", S_ctx[0][:, :, :].rearrange("p c n -> p (c n)"), [b_Sctx[0]])
                          dump("sctx1", S_ctx[1][:, :, :].rearrange("p c n -> p (c n)"), [b_Sctx[1]])
                          stop_at("sw1")
                      b_ss = Buf(); b_sa = Buf()
                      for d in range(2):
                          fw.dma("sp", st_send[:, d * 768:(d + 1) * 768], S_run[d][:, :, :].rearrange("p m n -> p (m n)"), reads=[b_S[d]], writes=[b_ss])
                      fw.dma("sp", st_send[:, 1536:1560], Lacc[:, :], reads=[b_Lacc], writes=[b_ss])
                      fw.cc("AllGather", st_send.ap().opt(), st_all.ap().opt(), GROUPS, [b_ss], [b_sa], b_sa)
                      S0 = S_ctx
                      for d in range(2):
                          order = range(4) if d == 0 else range(3, -1, -1)
                          for r in order:
                              mcol = (2 + r) if d == 0 else (6 + r)
                              fw.dma("sp", sr_t[:, :], st_all[r * 128:(r + 1) * 128, :], reads=[b_sa], writes=[b_sr])
                              op("act", lambda e: e.activation(out=dm_t[:, :], in_=sr_t[:, 1536 + d * 12:1548 + d * 12], func=AF.Exp), reads=[b_sr], writes=[b_dm])
                              op("dve", lambda e: e.tensor_scalar(out=dm_t[:, :], in0=dm_t[:, :], scalar1=-1.0, scalar2=sel[:, mcol:mcol + 1], op0=ALU.add, op1=ALU.mult),
                                 reads=[b_dm, b_sel], writes=[b_dm])
                              op("dve", lambda e: e.tensor_scalar(out=dm_t[:, :], in0=dm_t[:, :], scalar1=1.0, scalar2=None, op0=ALU.add), reads=[b_dm], writes=[b_dm])
                              op("dve", lambda e: e.tensor_scalar(out=sr_t[:, d * 768:(d + 1) * 768], in0=sr_t[:, d * 768:(d + 1) * 768], scalar1=sel[:, mcol:mcol + 1], scalar2=None, op0=ALU.mult),
                                 reads=[b_sr, b_sel], writes=[b_sr])
                              for m in range(6):
                                  upd_state(S0[d], b_Sctx[d], dm_t, b_dm, 0, m, sr_t[:, d * 768 + m * 128:d * 768 + (m + 1) * 128], b_sr)

                      SCS = [(PS[4][:, 256:384], Buf()), (PS[4][:, 384:512], Buf()), (PS[6][:, 256:384], Buf()), (PS[6][:, 384:512], Buf()),
                             (PS[0][:, 256:384], Buf()), (PS[1][:, 256:384], Buf())]

                      def load_cd(c):
                          cd, b_cd = cd_r.get()
                          fw.dma("sp", cd[:, :], cd_dram[c], reads=[b_cdd[c]], writes=[b_cd])
                          return cd, b_cd

                      def y_step(c, d, pre, c_next):
                          cd, b_cd = pre
                          nxt = load_cd(c_next) if c_next is not None else None
                          scl, b_scl = small_stats(c, d)
                          la_c = la_all[:, c, :]
                          yA = (PS[6], PS[7]); yAb = (PSB[6], PSB[7])
                          yB = (PS[4], PS[5]); yBb = (PSB[4], PSB[5])

                          def ycols(ps2, u0, n):
                              c0 = u0 * 64
                              if c0 < 256:
                                  return ps2[0][:, c0:c0 + n]
                              return ps2[1][:, c0 - 256:c0 - 256 + n]
                          qk_cache = {}
                          pendA = []

                          def emitA(u_, scT_, b_scT_):
                              m_, half_ = u_ // 2, u_ % 2
                              _, v_pair = kv_pair(cd, m_, d)
                              yb_ = yAb[0] if u_ < 4 else yAb[1]
                              op("pe", lambda e: e.matmul(ycols(yA, u_, 64), scT_[:, :], v_pair[:, half_ * 64:half_ * 64 + 64], start=True, stop=True), reads=[b_scT_, b_cd], writes=[yb_])
                          for u in range(12):
                              m, half = u // 2, u % 2
                              r0, r1 = half * 64, half * 64 + 64
                              pm, pbm = Ring(list(zip(PS[0:4], PSB[0:4]))).items[u % 2]
                              fw.group("pe", [lambda e: e.matmul(pm[:, 0:128], la_hl[:, c, d * 12 + u:d * 12 + u + 1].to_broadcast([128, 128]), tri_bf[d], start=True, stop=False),
                                              lambda e: e.matmul(pm[:, 0:128], la_hl[:, c, 24 + d * 12 + u:24 + d * 12 + u + 1].to_broadcast([128, 128]), tri_bf[d], start=False, stop=True)],
                                       reads=[b_la[c], b_cb], writes=[pbm])
                              dec, b_dec = dec_r.get()
                              op("act", lambda e: e.activation(out=dec[:, :], in_=pm[:, 0:128], func=AF.Exp, bias=scl[:, u:u + 1]), reads=[pbm, b_scl], writes=[b_dec])
                              qkey = u if u < 4 else 4 + half
                              if qkey not in qk_cache:
                                  pq, pbq = (PS[2], PSB[2]) if (len(qk_cache) % 2 == 0) else (PS[3], PSB[3])
                                  if u < 4:
                                      k_ap = cd[r0:r1, CD_KT + m * 128:CD_KT + (m + 1) * 128]
                                      q_ap = cd[r0:r1, CD_QT + m * 128:CD_QT + (m + 1) * 128]
                                  else:
                                      k_ap = cd[r0:r1, CD_BT:CD_BT + 128]
                                      q_ap = cd[r0:r1, CD_CT:CD_CT + 128]
                                  op("pe", lambda e: e.matmul(pq[:, 0:128], k_ap, q_ap, start=True, stop=True), reads=[b_cd], writes=[pbq])
                                  qkm, b_qkm = qkm_r.get()
                                  msk = triF if d == 0 else (triBs if u < 4 else triB)
                                  op("dve", lambda e: e.tensor_tensor(out=qkm[:, :], in0=pq[:, 0:128], in1=msk, op=ALU.mult), reads=[pbq, b_cf], writes=[b_qkm])
                                  qk_cache[qkey] = (qkm, b_qkm)
                              qkm, b_qkm = qk_cache[qkey]
                              scT, b_scT = scT_r.get()
                              op("dve", lambda e: e.scalar_tensor_tensor(out=scT[:, :], in0=dec[:, :], scalar=1.0, op0=ALU.min, in1=qkm[:, :], op1=ALU.mult),
                                 reads=[b_dec, b_qkm], writes=[b_scT])
                              pendA.append((u, scT, b_scT))
                              if len(pendA) > 2:
                                  emitA(*pendA.pop(0))
                          while pendA:
                              emitA(*pendA.pop(0))
                          for m in range(6):
                              q_ap = cd[:, CD_QT + m * 128:CD_QT + (m + 1) * 128] if m < 2 else cd[:, CD_CT:CD_CT + 128]
                              yb_ = yBb[0] if m < 2 else yBb[1]
                              op("pe", lambda e: e.matmul(ycols(yB, 2 * m, 128), q_ap, Sbd[d][:, m, :], start=True, stop=True), reads=[b_cd, b_Sbd[d]], writes=[yb_])
                          st_out = states_batch(cd, b_cd, scl, b_scl, d, SCS)
                          for m in range(6):
                              pt, pb = st_out[m]
                              upd_state(S_run[d], b_S[d], scl, b_scl, 24, m, pt, pb)
                          op("act", lambda e: e.activation(out=Sbd[d][:, :, :], in_=S_run[d][:, :, :], func=AF.Copy), reads=[b_S[d]], writes=[b_Sbd[d]])
                          yAs, b_yAs = yA_r.get()
                          yts, b_yts = yt_r.get()
                          op("act", lambda e: e.activation(out=yAs[:, 0:256], in_=yA[0][:, 0:256], func=AF.Copy), reads=[yAb[0]], writes=[b_yAs])
                          op("act", lambda e: e.activation(out=yAs[:, 256:768], in_=yA[1][:, 0:512], func=AF.Copy), reads=[yAb[1]], writes=[b_yAs])
                          op("dve", lambda e: e.tensor_tensor(out=yts[:, 0:256].rearrange("p (h n) -> p h n", h=4), in0=yB[0][:, 0:256].rearrange("p (h n) -> p h n", h=4),
                                                               in1=scl[:, 12:16].unsqueeze(2).to_broadcast([128, 4, 64]), op=ALU.mult), reads=[yBb[0], b_scl], writes=[b_yts])
                          op("dve", lambda e: e.tensor_tensor(out=yts[:, 256:768].rearrange("p (h n) -> p h n", h=8), in0=yB[1][:, 0:512].rearrange("p (h n) -> p h n", h=8),
                                                               in1=scl[:, 16:24].unsqueeze(2).to_broadcast([128, 8, 64]), op=ALU.mult), reads=[yBb[1], b_scl], writes=[b_yts])
                          op("pool", lambda e: e.tensor_tensor(out=yts[:, :], in0=yts[:, :], in1=yAs[:, :], op=ALU.add), reads=[b_yts, b_yAs], writes=[b_yts])
                          return cd, b_cd, yts, b_yts, nxt

                      psr_full = psr
                      psr = Ring(list(zip(PS[0:2], PSB[0:2])))
                      for d in range(2):
                          seqs = [[0, 1], list(range(2, NCH))] if d == 0 else [[1, 0], list(range(NCH - 1, 1, -1))]
                          flat = seqs[0] + seqs[1]
                          pre = load_cd(flat[0])
                          for si, seq in enumerate(seqs):
                              if si == 0:
                                  op("pool", lambda e: e.memset(S_run[d][:, :, :], 0.0), writes=[b_S[d]])
                              else:
                                  op("dve", lambda e: e.tensor_copy(S_run[d][:, :, :], S0[d][:, :, :]), reads=[b_Sctx[d]], writes=[b_S[d]])
                              op("act", lambda e: e.activation(out=Sbd[d][:, :, :], in_=S_run[d][:, :, :], func=AF.Copy), reads=[b_S[d]], writes=[b_Sbd[d]])
                              for c in seq:
                                  fi = flat.index(c)
                                  if d == 1 and not (last and c < 2):
                                      yfs, b_yfs = yf_r.get()
                                      fw.dma("sp", yfs[:, :], yf_dram[c], reads=[b_yfd[c]], writes=[b_yfs])
                                  cd, b_cd, yts, b_yts, pre = y_step(c, d, pre, flat[fi + 1] if fi + 1 < len(flat) else None)
                                  if d == 0:
                                      fw.dma("pool", yf_dram[c], yts[:, :], reads=[b_yts], writes=[b_yfd[c]], chanbuf=b_yts)
                                      continue
                                  if last and c < 2:
                                      continue
                                  op("pool", lambda e: e.tensor_tensor(out=yts[:, :], in0=yts[:, :], in1=yfs[:, :], op=ALU.add), reads=[b_yts, b_yfs], writes=[b_yts])
                                  gt, b_gt = gt_r.get()
                                  gst, b_gst = gst_r.get()
                                  mo, b_mo = mo_r.get()
                                  yr = yts[:, 0:256].rearrange("p (h n) -> p h n", h=4)
                                  gr = gt[:, 0:256].rearrange("p (h n) -> p h n", h=4)
                                  op("dve", lambda e: e.tensor_reduce(out=gst[:, 0:4], in_=yr, op=ALU.add, axis=AX.X), reads=[b_yts], writes=[b_gst])
                                  op("act", lambda e: e.activation(out=gt[:, 0:256], in_=yts[:, 0:256], func=AF.Square), reads=[b_yts], writes=[b_gt])
                                  op("dve", lambda e: e.tensor_reduce(out=gst[:, 4:8], in_=gr, op=ALU.add, axis=AX.X), reads=[b_gt], writes=[b_gst])
                                  op("dve", lambda e: e.tensor_scalar(out=gst[:, 0:8], in0=gst[:, 0:8], scalar1=1.0 / 64, scalar2=None, op0=ALU.mult), reads=[b_gst], writes=[b_gst])
                                  op("dve", lambda e: e.tensor_tensor(out=gst[:, 8:12], in0=gst[:, 0:4], in1=gst[:, 0:4], op=ALU.mult), reads=[b_gst], writes=[b_gst])
                                  op("dve", lambda e: e.tensor_tensor(out=gst[:, 8:12], in0=gst[:, 4:8], in1=gst[:, 8:12], op=ALU.subtract), reads=[b_gst], writes=[b_gst])
                                  op("act", lambda e: e.activation(out=gst[:, 8:12], in_=gst[:, 8:12], func=AF.Sqrt, bias=EPS), reads=[b_gst], writes=[b_gst])
                                  op("dve", lambda e: e.reciprocal(out=gst[:, 8:12], in_=gst[:, 8:12]), reads=[b_gst], writes=[b_gst])
                                  op("dve", lambda e: e.tensor_tensor(out=gr, in0=yr, in1=gst[:, 0:4].unsqueeze(2).to_broadcast([128, 4, 64]), op=ALU.subtract), reads=[b_yts, b_gst], writes=[b_gt])
                                  op("dve", lambda e: e.tensor_tensor(out=gr, in0=gr, in1=gst[:, 8:12].unsqueeze(2).to_broadcast([128, 4, 64]), op=ALU.mult), reads=[b_gt, b_gst], writes=[b_gt])
                                  op("dve", lambda e: e.tensor_tensor(out=mo[:, 0:256], in0=gt[:, 0:256], in1=cd[:, CD_SG:CD_SG + 256], op=ALU.mult), reads=[b_gt, b_cd], writes=[b_mo])
                                  op("pool", lambda e: e.tensor_tensor(out=gt[:, 256:768], in0=cd[:, CD_XS:CD_XS + 512], in1=dskip_b, op=ALU.mult), reads=[b_cd, b_small], writes=[b_gt])
                                  op("pool", lambda e: e.tensor_tensor(out=gt[:, 256:768], in0=gt[:, 256:768], in1=yts[:, 256:768], op=ALU.add), reads=[b_gt, b_yts], writes=[b_gt])
                                  op("dve", lambda e: e.tensor_tensor(out=gt[:, 256:768], in0=gt[:, 256:768], in1=cd[:, CD_SZ:CD_SZ + 512], op=ALU.mult), reads=[b_gt, b_cd], writes=[b_gt])
                                  op("act", lambda e: e.activation(out=yts[:, 256:768], in_=gt[:, 256:768], func=AF.Square, accum_out=gst[:, 16:17]), reads=[b_gt], writes=[b_yts, b_gst])
                                  op("act", lambda e: e.activation(out=gst[:, 17:18], in_=gst[:, 16:17], func=AF.Sqrt, scale=1.0 / 512, bias=EPS), reads=[b_gst], writes=[b_gst])
                                  op("dve", lambda e: e.reciprocal(out=gst[:, 17:18], in_=gst[:, 17:18]), reads=[b_gst], writes=[b_gst])
                                  op("dve", lambda e: e.scalar_tensor_tensor(out=mo[:, 256:768], in0=gt[:, 256:768], scalar=gst[:, 17:18], op0=ALU.mult, in1=snw_b, op1=ALU.mult),
                                     reads=[b_gt, b_gst, b_small], writes=[b_mo])
                                  for half3 in range(2):
                                      p2, pb2 = psr.get()
                                      pbf = p2[:, 0:192].bitcast(BF16)
                                      fw.group("pe", [(lambda e, i=i: e.transpose(pbf[:, i * 128:(i + 1) * 128], mo[:, (half3 * 3 + i) * 128:(half3 * 3 + i + 1) * 128], ident_b)) for i in range(3)],
                                               reads=[b_mo, b_cb], writes=[pb2])
                                      eng = "dve"
                                      if eng == "act":
                                          op("act", lambda e: e.activation(out=mixT[:, 2 + half3 * 3:5 + half3 * 3, c * 128:(c + 1) * 128], in_=pbf[:, 0:384].rearrange("p (c t) -> p c t", c=3), func=AF.Copy),
                                             reads=[pb2], writes=[b_mix[c]])
                                      else:
                                          op("dve", lambda e: e.tensor_copy(mixT[:, 2 + half3 * 3:5 + half3 * 3, c * 128:(c + 1) * 128], pbf[:, 0:384].rearrange("p (c t) -> p c t", c=3)),
                                             reads=[pb2], writes=[b_mix[c]])
                      psr = psr_full
              if L == 0:
                  dump("mix", mixT[:, :, :].rearrange("p k t -> p (k t)"), b_mix)
                  stop_at("scan")
              fw.barrier()
              blks = [(bi, t0, W, v) for bi, (t0, W, v) in enumerate(BLOCKS) if not (last and v == 1)]
              with ExitStack() as s3:
                  xb3 = sb(s3, "xb3", [128, 8, 512], F32); b_xb3 = Buf()
                  wo = sb(s3, "wo", [128, 8, D], BF16); b_wo = Buf()
                  fw.dma("pool", wo[:, :, :], w_out[L].rearrange("(k p) n -> p k n", p=128), writes=[b_wo])
                  wk = {"sq": Ring([(sb(s3, "sq3_%d" % i, [128, 8, 512], BF16), Buf()) for i in range(1)]),
                        "rs": Ring([(sb(s3, "rs3_%d" % i, [128, 512], F32), Buf()) for i in range(2)]),
                        "tmp": Ring([(sb(s3, "tmp3_%d" % i, [128, 512], F32), Buf()) for i in range(2)])}
                  for bi, t0, W, v in blks:
                      fw.dma("sp", xb3[:, :, 0:W], x_src[:, t0:t0 + W].rearrange("(k p) t -> p k t", p=128), writes=[b_xb3])
                      mb = [b_mix[c] for c in range(t0 // 128, (t0 + W) // 128)]
                      for oc in range(8):
                          pt, pb = psr.get()
                          fw.group("pe", [(lambda e, k=k: e.matmul(pt[:, 0:W], wo[:, k, oc * 128:(oc + 1) * 128], mixT[:, k, t0:t0 + W], start=(k == 0), stop=(k == 7))) for k in range(8)],
                                   reads=[b_wo] + mb, writes=[pb])
                          op("dve", lambda e: e.scalar_tensor_tensor(out=xb3[:, oc, 0:W], in0=pt[:, 0:W], scalar=mv[:, 16 + oc, v:v + 1], op0=ALU.mult, in1=xb3[:, oc, 0:W], op1=ALU.add),
                             reads=[pb, b_mv, b_xb3], writes=[b_xb3])
                      norm_mod(xb3[:, :, 0:W], b_xb3, t0, W, v, 8, 24, wk, b_hx[bi])
                      fw.dma("sp", xres[:, t0:t0 + W].rearrange("(k p) t -> p k t", p=128), xb3[:, :, 0:W], reads=[b_xb3], writes=[b_xres], chanbuf=b_xb3)
              if L == 0:
                  dump("xmid", xres, [b_xres])
                  dump("hx2", hxT[:, :, :].rearrange("p k t -> p (k t)"), b_hx)
                  stop_at("s3a")
              lm.close()
              fw.barrier()
              with ExitStack() as s3:
                  xr = sb(s3, "xr", [128, 8, NT], F32); b_xr = [Buf() for _ in BLOCKS]
                  wk = {"sq": Ring([(sb(s3, "sq4_%d" % i, [128, 8, 512], BF16), Buf()) for i in range(1)]),
                        "rs": Ring([(sb(s3, "rs4_%d" % i, [128, 512], F32), Buf()) for i in range(1)])}
                  for bi, t0, W, v in blks:
                      fw.dma("sp", xr[:, :, t0:t0 + W], xres[:, t0:t0 + W].rearrange("(k p) t -> p k t", p=128), reads=[b_xres], writes=[b_xr[bi]])
                  is_moe = (L % 2 == 1)
                  if not is_moe:
                      units = [(None, f0, min(4, 22 - f0)) for f0 in range(0, 22, 4)]
                      wg_src, wu_src, wd_src = ffn_wg, ffn_wu, ffn_wd
                  else:
                      units = [(e_, f0, min(4, 11 - f0)) for e_ in range(8) for f0 in (0, 4, 8)]
                  wgu_r = Ring([(sb(s3, "wgu%d" % i, [128, 2, 8, 512], BF16), Buf()) for i in range(2)])
                  wd_r = Ring([(sb(s3, "wdn%d" % i, [128, 4, D], BF16), Buf()) for i in range(2)])
                  hid = sb(s3, "hid", [128, 4, 512], BF16); b_hid = Buf()
                  sg_r = Ring([(sb(s3, "sg%d" % i, [128, 512], BF16), Buf()) for i in range(2)])
                  ug_r = Ring([(sb(s3, "ug%d" % i, [128, 512], BF16), Buf()) for i in range(2)])
                  Gt = None
                  if is_moe:
                      rt = sb(s3, "rt", [128, 8, 8], BF16); b_rt = Buf()
                      fw.dma("pool", rt[:, :, :], router_in.rearrange("(k p) n -> p k n", p=128), writes=[b_rt])
                      gates = sb(s3, "gates", [128, NCH, 8], F32); b_gates = Buf()
                      G = sb(s3, "G", [128, 512], F32); b_G = Buf()
                      gb_r = Ring([(sb(s3, "gb%d" % i, [128, 128], F32), Buf()) for i in range(2)])
                      gs_r = Ring([(sb(s3, "gs%d" % i, [128, 32], F32), Buf()) for i in range(2)])
                      for bi, t0, W, v in blks:
                          for ti in range(W // 128):
                              c = (t0 // 128) + ti
                              tt = c * 128
                              pt, pb = psr.get()
                              fw.group("pe", [(lambda e, k=k: e.matmul(pt[:, 0:8], hxT[:, k, tt:tt + 128], rt[:, k, :], start=(k == 0), stop=(k == 7))) for k in range(8)],
                                       reads=[b_rt, b_hx[bi]], writes=[pb])
                              gs, b_gs = gs_r.get()
                              op("dve", lambda e: e.tensor_copy(gs[:, 0:8], pt[:, 0:8]), reads=[pb], writes=[b_gs])
                              op("dve", lambda e: e.max(out=gs[:, 8:16], in_=gs[:, 0:8]), reads=[b_gs], writes=[b_gs])
                              op("dve", lambda e: e.tensor_scalar(out=gs[:, 16:17], in0=gs[:, 8:9], scalar1=-1.0, scalar2=None, op0=ALU.mult), reads=[b_gs], writes=[b_gs])
                              op("act", lambda e: e.activation(out=gs[:, 24:32], in_=gs[:, 0:8], func=AF.Exp, bias=gs[:, 16:17]), reads=[b_gs], writes=[b_gs])
                              op("dve", lambda e: e.scalar_tensor_tensor(out=gs[:, 24:32], in0=gs[:, 0:8], scalar=gs[:, 9:10], op0=ALU.is_ge, in1=gs[:, 24:32], op1=ALU.mult), reads=[b_gs], writes=[b_gs])
                              op("dve", lambda e: e.tensor_reduce(out=gs[:, 17:18], in_=gs[:, 24:32], op=ALU.add, axis=AX.X), reads=[b_gs], writes=[b_gs])
                              op("dve", lambda e: e.reciprocal(out=gs[:, 17:18], in_=gs[:, 17:18]), reads=[b_gs], writes=[b_gs])
                              op("dve", lambda e: e.tensor_scalar(out=gates[:, c, :], in0=gs[:, 24:32], scalar1=gs[:, 17:18], scalar2=None, op0=ALU.mult), reads=[b_gs], writes=[b_gates])
                  for (e_, f0, F) in units:
                      wgu, b_wgu = wgu_r.get()
                      wdn, b_wdn = wd_r.get()
                      if e_ is None:
                          gsrc = ffn_wg[:, f0 * 128:(f0 + F) * 128]; usrc = ffn_wu[:, f0 * 128:(f0 + F) * 128]; dsrc = ffn_wd[f0 * 128:(f0 + F) * 128, :]
                      else:
                          gsrc = moe_wg[e_, :, f0 * 128:(f0 + F) * 128]; usrc = moe_wu[e_, :, f0 * 128:(f0 + F) * 128]; dsrc = moe_wd[e_, f0 * 128:(f0 + F) * 128, :]
                      fw.dma("pool", wgu[:, 0, :, 0:F * 128], gsrc.rearrange("(k p) n -> p k n", p=128), writes=[b_wgu])
                      fw.dma("pool", wgu[:, 1, :, 0:F * 128], usrc.rearrange("(k p) n -> p k n", p=128), writes=[b_wgu])
                      fw.dma("pool", wdn[:, 0:F, :], dsrc.rearrange("(f p) n -> p f n", p=128), writes=[b_wdn])
                      for bi, t0, W, v in blks:
                          if e_ is not None:
                              pt, pb = psr.get()
                              for ti in range(W // 128):
                                  c = (t0 // 128) + ti
                                  gb, b_gb = gb_r.get()
                                  op("dve", lambda e: e.tensor_scalar(out=gb[:, :], in0=ones_f, scalar1=gates[:, c, e_:e_ + 1], scalar2=None, op0=ALU.mult), reads=[b_cf, b_gates], writes=[b_gb])
                                  op("pe", lambda e: e.matmul(pt[:, ti * 128:(ti + 1) * 128], gb[:, :], ident_f, start=True, stop=True), reads=[b_gb, b_cf], writes=[pb])
                              op("act", lambda e: e.activation(out=G[:, 0:W], in_=pt[:, 0:W], func=AF.Copy), reads=[pb], writes=[b_G])
                          for f in range(F):
                              pg, pbg = psr.get()
                              pu, pbu = psr.get()
                              fw.group("pe", [(lambda e, k=k: e.matmul(pg[:, 0:W], wgu[:, 0, k, f * 128:(f + 1) * 128], hxT[:, k, t0:t0 + W], start=(k == 0), stop=(k == 7))) for k in range(8)],
                                       reads=[b_wgu, b_hx[bi]], writes=[pbg])
                              fw.group("pe", [(lambda e, k=k: e.matmul(pu[:, 0:W], wgu[:, 1, k, f * 128:(f + 1) * 128], hxT[:, k, t0:t0 + W], start=(k == 0), stop=(k == 7))) for k in range(8)],
                                       reads=[b_wgu, b_hx[bi]], writes=[pbu])
                              sg, b_sg = sg_r.get()
                              op("act", lambda e: e.activation(out=sg[:, 0:W], in_=pg[:, 0:W], func=AF.Silu), reads=[pbg], writes=[b_sg])
                              if e_ is None:
                                  op("dve", lambda e: e.tensor_tensor(out=hid[:, f, 0:W], in0=pu[:, 0:W], in1=sg[:, 0:W], op=ALU.mult), reads=[pbu, b_sg], writes=[b_hid])
                              else:
                                  ug, b_ug = ug_r.get()
                                  op("dve", lambda e: e.tensor_tensor(out=ug[:, 0:W], in0=pu[:, 0:W], in1=G[:, 0:W], op=ALU.mult), reads=[pbu, b_G], writes=[b_ug])
                                  op("pool", lambda e: e.tensor_tensor(out=hid[:, f, 0:W], in0=ug[:, 0:W], in1=sg[:, 0:W], op=ALU.mult), reads=[b_ug, b_sg], writes=[b_hid])
                          for oc in range(8):
                              pt, pb = psr.get()
                              fw.group("pe", [(lambda e, f=f: e.matmul(pt[:, 0:W], wdn[:, f, oc * 128:(oc + 1) * 128], hid[:, f, 0:W], start=(f == 0), stop=(f == F - 1))) for f in range(F)],
                                       reads=[b_wdn, b_hid], writes=[pb])
                              op("dve", lambda e: e.scalar_tensor_tensor(out=xr[:, oc, t0:t0 + W], in0=pt[:, 0:W], scalar=mv[:, 40 + oc, v:v + 1], op0=ALU.mult, in1=xr[:, oc, t0:t0 + W], op1=ALU.add),
                                 reads=[pb, b_mv, b_xr[bi]], writes=[b_xr[bi]])
                  b_out = Buf()
                  if not last:
                      for bi, t0, W, v in blks:
                          fw.dma("sp", xres[:, t0:t0 + W].rearrange("(k p) t -> p k t", p=128), xr[:, :, t0:t0 + W], reads=[b_xr[bi]], writes=[b_xres], chanbuf=b_xr[bi])
                      fw.finish([b_xres], eng="sp")
                  else:
                      for bi, t0, W, v in blks:
                          sq, b_sq = wk["sq"].get()
                          op("act", lambda e: e.activation(out=sq[:, :, 0:W], in_=xr[:, :, t0:t0 + W], func=AF.Square), reads=[b_xr[bi]], writes=[b_sq])
                          pt, pb = psr.get()
                          fw.group("pe", [(lambda e, k=k: e.matmul(pt[:, 0:W], ones_b, sq[:, k, 0:W], start=(k == 0), stop=(k == 7))) for k in range(8)], reads=[b_sq, b_cb], writes=[pb])
                          rs, b_rs = wk["rs"].get()
                          op("act", lambda e: e.activation(out=rs[:, 0:W], in_=pt[:, 0:W], func=AF.Sqrt, scale=1.0 / D, bias=EPS), reads=[pb], writes=[b_rs])
                          op("dve", lambda e: e.reciprocal(out=rs[:, 0:W], in_=rs[:, 0:W]), reads=[b_rs], writes=[b_rs])
                          for k in range(8):
                              op("dve", lambda e: e.scalar_tensor_tensor(out=xr[:, k, t0:t0 + W], in0=xr[:, k, t0:t0 + W], scalar=fnw[:, k:k + 1], op0=ALU.mult, in1=rs[:, 0:W], op1=ALU.mult),
                                 reads=[b_xr[bi], b_fnw, b_rs], writes=[b_xr[bi]])
                          fw.dma("sp", outT[:, t0 - TC:t0 - TC + W].rearrange("(k p) t -> p k t", p=128), xr[:, :, t0:t0 + W], reads=[b_xr[bi]], writes=[b_out], chanbuf=b_xr[bi])
                      fw.finish([b_out], eng="sp")
        except _Stop:
            stopped = True
        fw.finish(dbg_bufs, eng="sp")
        print("instructions:", fw.ninstr, "dma channels:", fw.nchan)
        if stopped:
            raise _StopOuter()
    return nc


def _perm_heads(a, axis):
    a = np.moveaxis(a, axis, -1)
    sh = a.shape
    a = a.reshape(sh[:-1] + (8, sh[-1] // 8))[..., PI8, :].reshape(sh)
    return np.ascontiguousarray(np.moveaxis(a, -1, axis))


def _host_inputs(inp):
    f32 = np.float32
    g = {k: np.asarray(v) for k, v in inp.items()}
    x, c, ctx, c_ctx = g["x"], g["c"], g["ctx"], g["c_ctx"]
    w_in = g["w_in"].copy()
    w_in[:, :, 1440:1952] = _perm_heads(g["w_in"][:, :, 1440:1952], 2)
    w_in[:, :, 1952:2464] = _perm_heads(g["w_in"][:, :, 1952:2464], 2)
    w_in[:, :, 2720:2728] = g["w_in"][:, :, 2720:2728][:, :, PI8]
    w_in[:, :, 2728:2736] = g["w_in"][:, :, 2728:2736][:, :, PI8]
    w_out = g["w_out"].copy()
    w_out[:, 512:1024, :] = _perm_heads(g["w_out"][:, 512:1024, :], 1)
    perm = np.concatenate([np.arange(8, 16), np.arange(0, 8), np.arange(24, 32), np.arange(16, 24)])
    w_qb = g["mla_w_qb"]
    w_qbrot = np.zeros_like(w_qb)
    for h in range(4):
        w_qbrot[:, :, h * 96 + 64:h * 96 + 96] = w_qb[:, :, h * 96 + 64 + perm]
    conv_w = g["ssd_conv_w"].copy()
    conv_w[:, :, 0:512] = _perm_heads(g["ssd_conv_w"][:, :, 0:512], 2)
    conv_b = g["ssd_conv_b"].copy()
    conv_b[:, 0:512] = _perm_heads(g["ssd_conv_b"][:, 0:512], 1)
    bc = lambda v: np.ascontiguousarray(np.broadcast_to(v[:, None, :], (v.shape[0], 128, v.shape[1]))).astype(f32)
    fm = lambda v: np.ascontiguousarray(v.reshape(-1, 128).T)
    nw = np.stack([np.concatenate([fm(g["norm1_w"][l]), fm(g["norm2_w"][l])], axis=1) for l in range(2)]).astype(f32)
    convw = np.stack([np.stack([fm(conv_w[l, t]) for t in range(3)], axis=2).reshape(128, 18) for l in range(2)]).astype(f32)
    convb = np.stack([fm(conv_b[l]) for l in range(2)]).astype(f32)
    dtb = np.concatenate([g["ssd_dt_bias"][:, 0][:, PI8], g["ssd_dt_bias"][:, 1][:, PI8]], axis=1)
    alog = np.concatenate([g["ssd_a_log"][:, 0][:, PI8], g["ssd_a_log"][:, 1][:, PI8]], axis=1)
    rdl = np.concatenate([g["ret_decay_logit"][:, 0], g["ret_decay_logit"][:, 1]], axis=1)
    dskip = np.repeat(g["ssd_d"][:, PI8], 64, axis=1)
    snw = _perm_heads(g["ssd_norm_w"], 1)
    k = np.arange(128)
    cf = np.zeros((128, 6, 128), f32)
    cf[:, 0] = np.eye(128)
    cf[:, 1] = (k[:, None] <= k[None, :])
    cf[:, 2] = (k[:, None] >= k[None, :])
    cf[:, 3] = (k[:, None] > k[None, :])
    cf[:, 4] = 1.0
    cf[64, 5, 0:64] = 1.0
    cbf = np.zeros((128, 5, 128), f32)
    cbf[:, 0] = np.eye(128)
    cbf[:, 1] = 1.0
    for m in range(32):
        cbf[perm[m], 2, m] = 1.0
    cbf[:, 3] = cf[:, 1]
    cbf[:, 4] = cf[:, 2]
    cbf = cbf.astype(ml_dtypes.bfloat16)
    inv_freq = (10000.0 ** (-np.arange(0, 16, 2, dtype=f32) / 16)).astype(f32)
    sign = np.concatenate([-np.ones(8), np.ones(8), -np.ones(8), np.ones(8)]).astype(f32)
    shared = dict(mod_w=g["mod_w"], mod_b=g["mod_b"][:, None, :], w_in=w_in, w_out=w_out, w_qb=w_qb, w_qbrot=w_qbrot,
                  w_kb=g["mla_w_kb"], w_vb=g["mla_w_vb"], nw=nw, fnw=fm(g["final_norm_w"]).astype(f32),
                  qnw_b=bc(g["mla_q_norm_w"]), kvnw_b=bc(g["mla_kv_norm_w"]), convw=convw, convb=convb,
                  dtb_b=bc(dtb), alog_b=bc(alog), rdl_b=bc(rdl), dskip_b=bc(dskip), snw_b=bc(snw),
                  router=g["moe_router"][0], ffn_wg=g["ffn_w_gate"][0], ffn_wu=g["ffn_w_up"][0], ffn_wd=g["ffn_w_down"][0],
                  moe_wg=g["moe_w_gate"][0], moe_wu=g["moe_w_up"][0], moe_wd=g["moe_w_down"][0], cf32=cf, cbf16=cbf)
    shared = {k_: np.ascontiguousarray(v, dtype=(v.dtype if v.dtype == ml_dtypes.bfloat16 else f32)) for k_, v in shared.items()}
    maps = []
    for r in range(8):
        b, j = r // 4, r % 4
        xt = np.concatenate([ctx[b], x[b, j * TL:(j + 1) * TL]], axis=0)
        ccv = np.stack([fm(c[b]), fm(c_ctx)], axis=2).astype(f32)
        tg = (j * TL + np.arange(TL)).astype(f32)
        row = np.floor(tg / 64).astype(f32)
        col = (tg - row * 64).astype(f32)
        ang_r = row[:, None] * inv_freq
        ang_c = col[:, None] * inv_freq
        ang = np.concatenate([ang_r, ang_r, ang_c, ang_c], axis=1).astype(f32)
        cos32 = np.concatenate([np.ones((TC, 32), f32), np.cos(ang).astype(f32)], axis=0)
        sin32 = np.concatenate([np.zeros((TC, 32), f32), np.sin(ang).astype(f32) * sign], axis=0)
        ropeT = np.zeros((128, 2, NT), f32)
        for base in (0, 64):
            ropeT[base:base + 32, 0] = cos32.T
            ropeT[base:base + 32, 1] = sin32.T
        sel = np.zeros((128, 16), f32)
        if j > 0:
            sel[2 * (j - 1) + 1, 0] = 1.0
        if j < 3:
            sel[2 * (j + 1), 1] = 1.0
        for r2 in range(4):
            sel[:, 2 + r2] = 1.0 if r2 < j else 0.0
            sel[:, 6 + r2] = 1.0 if r2 > j else 0.0
        m = dict(shared)
        m.update(xT=np.ascontiguousarray(xt.T.astype(f32)), cc=np.ascontiguousarray(ccv), ropeT=ropeT, sel=sel)
        maps.append(m)
    return maps


_NC_CACHE = {}


def kernel(**inputs):
    maps = _host_inputs(inputs)
    if "nc" not in _NC_CACHE:
        _NC_CACHE["nc"] = build_program()
    res = run_bass_kernel_spmd(_NC_CACHE["nc"], maps, core_ids=list(range(8)))
    out = np.zeros((2, 4 * TL, D), np.float32)
    for r in range(8):
        b, j = r // 4, r % 4
        out[b, j * TL:(j + 1) * TL, :] = np.asarray(res.results[r]["outT"]).T
    return out
```
